# Optimizing a Trainium2 kernel written in Bass

```python
import math
import jax, jax.numpy as jnp
from jax import lax
import numpy as np

D_MODEL = 2048
BATCH = 4
SEQ = 2048
DEPTH = 1
DEC_BATCH = 128
DEC_SEQ = 8
PAST_LEN = 16384
PAGE_SIZE = 128

POOL_WIDTH = D_MODEL // 2
POOL_WINDOWS = (2, 4, 8, 16)
N_POOL_GROUPS = 4
POOL_GROUP = POOL_WIDTH // N_POOL_GROUPS
POOL_BUF = 15
MLSTM_HEADS = 4
MLSTM_WIDTH = D_MODEL
HEAD_DIM = MLSTM_WIDTH // MLSTM_HEADS
MLSTM_CHUNK = 64
N_EXPERT_GROUPS = 4
EXPERTS_PER_GROUP = 8
N_EXPERTS = N_EXPERT_GROUPS * EXPERTS_PER_GROUP
TOP_K_IN_GROUP = 2
EXPERT_FF = D_MODEL // 4
IN_COLS = POOL_WIDTH + 4 * MLSTM_WIDTH + 2 * MLSTM_HEADS + 2 * D_MODEL
RMS_EPS = 1e-6

kernel_name = "hybrid_pool_mlstm_hmoe_step"


def _rmsnorm(x, g):
    xf = x.astype(jnp.float32)
    xf = xf * lax.rsqrt(jnp.mean(xf * xf, axis=-1, keepdims=True) + RMS_EPS)
    return (xf * g.astype(jnp.float32)).astype(x.dtype)


def _split_in(z):
    sizes = (POOL_WIDTH, MLSTM_WIDTH, MLSTM_WIDTH, MLSTM_WIDTH, MLSTM_WIDTH,
             MLSTM_HEADS, MLSTM_HEADS, D_MODEL)
    idx = [int(i) for i in np.cumsum(sizes)]
    return jnp.split(z, idx, axis=-1)


def _pool_mixer(u, buf, pos0, w_pool, pool_scale):
    B, S, P = u.shape
    full = jnp.concatenate([buf.astype(jnp.float32), u.astype(jnp.float32)], axis=1)
    csum = jnp.concatenate([jnp.zeros((B, 1, P), jnp.float32), jnp.cumsum(full, axis=1)], axis=1)
    uf = u.astype(jnp.float32)
    outs = []
    for g, w in enumerate(POOL_WINDOWS):
        cs = csum[..., g * POOL_GROUP:(g + 1) * POOL_GROUP]
        wsum = cs[:, POOL_BUF + 1:POOL_BUF + 1 + S] - cs[:, POOL_BUF + 1 - w:POOL_BUF + 1 - w + S]
        count = jnp.minimum(pos0 + jnp.arange(S, dtype=jnp.int32) + 1, w).astype(jnp.float32)[None, :, None]
        outs.append(wsum / count - uf[..., g * POOL_GROUP:(g + 1) * POOL_GROUP])
    pooled = jnp.stack(outs, axis=2)
    mixed = jnp.einsum('bsgc,gcd->bsgd', pooled, w_pool.astype(jnp.float32)).reshape(B, S, P)
    out = (mixed * pool_scale.astype(jnp.float32)).astype(u.dtype)
    return out, full[:, -POOL_BUF:].astype(u.dtype)


def _mlstm_chunk(carry, inp):
    C, n, m = carry
    q, k, v, ig, lf = inp
    L = q.shape[2]
    b = jnp.cumsum(lf, axis=-1)
    causal = jnp.tril(jnp.ones((L, L), dtype=bool))
    dmat = jnp.where(causal, b[..., :, None] - b[..., None, :] + ig[..., None, :], -jnp.inf)
    inter = b + m[..., None]
    m_new = jnp.maximum(inter, jnp.max(dmat, axis=-1))
    w_intra = jnp.exp(dmat - m_new[..., None])
    w_inter = jnp.exp(inter - m_new)
    s = jnp.einsum('bhtd,bhsd->bhts', q, k) * w_intra
    num = jnp.einsum('bhts,bhsv->bhtv', s, v) + w_inter[..., None] * jnp.einsum('bhtd,bhdv->bhtv', q, C)
    den = jnp.sum(s, axis=-1) + w_inter * jnp.einsum('bhtd,bhd->bht', q, n)
    h = num / jnp.maximum(jnp.abs(den), jnp.exp(-m_new))[..., None]
    m_last = m_new[..., -1]
    wl_inter = jnp.exp(b[..., -1] + m - m_last)
    wl = jnp.exp(b[..., -1:] - b + ig - m_last[..., None])
    C_new = wl_inter[..., None, None] * C + jnp.einsum('bhs,bhsd,bhsv->bhdv', wl, k, v)
    n_new = wl_inter[..., None] * n + jnp.einsum('bhs,bhsd->bhd', wl, k)
    return (C_new, n_new, m_last), h


def _mlstm(q, k, v, ig, lf, C0, n0, m0):
    B, H, S, d = q.shape
    L = math.gcd(S, MLSTM_CHUNK)
    nc = S // L

    def to_blocks(a):
        return jnp.moveaxis(a.reshape(a.shape[:2] + (nc, L) + a.shape[3:]), 2, 0)

    carry0 = (C0.astype(jnp.float32), n0.astype(jnp.float32), m0.astype(jnp.float32))
    (C, n, m), h = lax.scan(_mlstm_chunk, carry0, tuple(to_blocks(a) for a in (q, k, v, ig, lf)))
    h = jnp.moveaxis(h, 0, 2).reshape(B, H, S, d)
    return h, C, n, m


def _hier_moe(x, w_rg, b_rg, w_re, b_re, w_eg, w_eu, w_ed):
    B, S, D = x.shape
    xt = x.reshape(B * S, D)
    T = xt.shape[0]
    g_logits = (xt @ w_rg).astype(jnp.float32) + b_rg.astype(jnp.float32)
    g_prob = jax.nn.softmax(g_logits, axis=-1)
    g_idx = jnp.argmax(g_logits, axis=-1)
    g_val = jnp.take_along_axis(g_prob, g_idx[:, None], axis=-1)
    e_logits = ((xt @ w_re).astype(jnp.float32) + b_re.astype(jnp.float32)).reshape(T, N_EXPERT_GROUPS, EXPERTS_PER_GROUP)
    in_group = e_logits[jnp.arange(T), g_idx]
    top_val, top_idx = lax.top_k(in_group, TOP_K_IN_GROUP)
    top_w = jax.nn.softmax(top_val, axis=-1) * g_val
    expert_id = g_idx[:, None] * EXPERTS_PER_GROUP + top_idx
    combine = jnp.einsum('tk,tke->te', top_w, jax.nn.one_hot(expert_id, N_EXPERTS, dtype=jnp.float32))
    hg = jnp.einsum('td,edf->tef', xt, w_eg)
    hu = jnp.einsum('td,edf->tef', xt, w_eu)
    h = jax.nn.silu(hg) * hu * combine[..., None].astype(x.dtype)
    return jnp.einsum('tef,efd->td', h, w_ed).reshape(B, S, D)


def _layer(x, buf, C0, n0, m0, pos0, g_mix, w_in, b_if, w_pool, pool_scale, w_proj_a, w_proj_b,
           g_head, w_out, g_ffn, w_rg, b_rg, w_re, b_re, w_eg, w_eu, w_ed):
    B, S, _ = x.shape
    hN = _rmsnorm(x, g_mix)
    z = hN @ w_in
    u, q, k, v, o, ig, fg, ga, gb = _split_in(z)
    a, new_buf = _pool_mixer(u, buf, pos0, w_pool, pool_scale)
    def heads(t):
        return jnp.transpose(t.reshape(B, S, MLSTM_HEADS, HEAD_DIM), (0, 2, 1, 3)).astype(jnp.float32)
    qh, kh, vh = heads(q), heads(k) * (HEAD_DIM ** -0.5), heads(v)
    ig_pre = jnp.transpose(ig.astype(jnp.float32) + b_if[:MLSTM_HEADS].astype(jnp.float32), (0, 2, 1))
    lf = jax.nn.log_sigmoid(jnp.transpose(fg.astype(jnp.float32) + b_if[MLSTM_HEADS:].astype(jnp.float32), (0, 2, 1)))
    h, C, n, m = _mlstm(qh, kh, vh, ig_pre, lf, C0, n0, m0)
    h = jnp.transpose(h, (0, 2, 1, 3))
    h = h * lax.rsqrt(jnp.mean(h * h, axis=-1, keepdims=True) + RMS_EPS)
    h = h * g_head.astype(jnp.float32).reshape(MLSTM_HEADS, HEAD_DIM)
    hb = (h.reshape(B, S, MLSTM_WIDTH) * jax.nn.sigmoid(o.astype(jnp.float32))).astype(x.dtype)
    mix = jax.nn.sigmoid(ga) * (a @ w_proj_a) + jax.nn.sigmoid(gb) * (hb @ w_proj_b)
    x = x + mix @ w_out
    x = x + _hier_moe(_rmsnorm(x, g_ffn), w_rg, b_rg, w_re, b_re, w_eg, w_eu, w_ed)
    return x, new_buf, C, n, m


def setup_inputs(seed: int = 0) -> dict:
    key = jax.random.key(seed)
    ks = jax.random.split(key, 32)
    f32 = jnp.float32
    nrm = lambda k, s, sc: jax.random.normal(k, s, f32) * sc
    H = MLSTM_HEADS
    b_i = nrm(ks[0], (DEPTH, H), 0.1)
    b_f = jnp.broadcast_to(jnp.linspace(3.0, 6.0, H, dtype=f32), (DEPTH, H)) + nrm(ks[1], (DEPTH, H), 0.1)
    return {
        "x_prompt": nrm(ks[2], (BATCH, SEQ, D_MODEL), 1.0),
        "x_sample": nrm(ks[3], (DEC_BATCH, DEC_SEQ, D_MODEL), 1.0),
        "state_pool": nrm(ks[4], (DEPTH, DEC_BATCH, POOL_BUF, POOL_WIDTH), 1.0),
        "state_C": nrm(ks[5], (DEPTH, DEC_BATCH, H, HEAD_DIM, HEAD_DIM), 0.05),
        "state_n": nrm(ks[6], (DEPTH, DEC_BATCH, H, HEAD_DIM), 0.05),
        "state_m": nrm(ks[7], (DEPTH, DEC_BATCH, H), 1.0),
        "g_mix": 1.0 + nrm(ks[8], (DEPTH, D_MODEL), 0.02),
        "w_in": nrm(ks[9], (DEPTH, D_MODEL, IN_COLS), D_MODEL ** -0.5),
        "b_if": jnp.concatenate([b_i, b_f], axis=-1),
        "w_pool": nrm(ks[10], (DEPTH, N_POOL_GROUPS, POOL_GROUP, POOL_GROUP), POOL_GROUP ** -0.5),
        "pool_scale": 1.0 + nrm(ks[11], (DEPTH, POOL_WIDTH), 0.02),
        "w_proj_a": nrm(ks[12], (DEPTH, POOL_WIDTH, D_MODEL), POOL_WIDTH ** -0.5),
        "w_proj_b": nrm(ks[13], (DEPTH, MLSTM_WIDTH, D_MODEL), MLSTM_WIDTH ** -0.5),
        "g_head": 1.0 + nrm(ks[14], (DEPTH, MLSTM_WIDTH), 0.02),
        "w_out": nrm(ks[15], (DEPTH, D_MODEL, D_MODEL), D_MODEL ** -0.5),
        "g_ffn": 1.0 + nrm(ks[16], (DEPTH, D_MODEL), 0.02),
        "w_router_group": nrm(ks[17], (DEPTH, D_MODEL, N_EXPERT_GROUPS), D_MODEL ** -0.5),
        "b_router_group": nrm(ks[18], (DEPTH, N_EXPERT_GROUPS), 0.01),
        "w_router_expert": nrm(ks[19], (DEPTH, D_MODEL, N_EXPERTS), D_MODEL ** -0.5),
        "b_router_expert": nrm(ks[20], (DEPTH, N_EXPERTS), 0.01),
        "w_exp_gate": nrm(ks[21], (DEPTH, N_EXPERTS, D_MODEL, EXPERT_FF), D_MODEL ** -0.5),
        "w_exp_up": nrm(ks[22], (DEPTH, N_EXPERTS, D_MODEL, EXPERT_FF), D_MODEL ** -0.5),
        "w_exp_down": nrm(ks[23], (DEPTH, N_EXPERTS, EXPERT_FF, D_MODEL), EXPERT_FF ** -0.5),
        "g_final": 1.0 + nrm(ks[24], (D_MODEL,), 0.02),
    }


def reference(x_prompt, x_sample, state_pool, state_C, state_n, state_m, g_mix, w_in, b_if, w_pool,
              pool_scale, w_proj_a, w_proj_b, g_head, w_out, g_ffn, w_router_group, b_router_group,
              w_router_expert, b_router_expert, w_exp_gate, w_exp_up, w_exp_down, g_final):
    yp, ys = x_prompt, x_sample
    B = x_prompt.shape[0]
    pool_p, C_p, n_p, m_p = [], [], [], []
    pool_s, C_s, n_s, m_s = [], [], [], []
    for l in range(DEPTH):
        params = (g_mix[l], w_in[l], b_if[l], w_pool[l], pool_scale[l], w_proj_a[l], w_proj_b[l],
                  g_head[l], w_out[l], g_ffn[l], w_router_group[l], b_router_group[l],
                  w_router_expert[l], b_router_expert[l], w_exp_gate[l], w_exp_up[l], w_exp_down[l])
        buf0 = jnp.zeros((B, POOL_BUF, POOL_WIDTH), yp.dtype)
        C0 = jnp.zeros((B, MLSTM_HEADS, HEAD_DIM, HEAD_DIM), jnp.float32)
        n0 = jnp.zeros((B, MLSTM_HEADS, HEAD_DIM), jnp.float32)
        m0 = jnp.zeros((B, MLSTM_HEADS), jnp.float32)
        yp, bp, cp, nq, mq = _layer(yp, buf0, C0, n0, m0, 0, *params)
        ys, bs, cs, ns, ms = _layer(ys, state_pool[l], state_C[l], state_n[l], state_m[l], PAST_LEN, *params)
        pool_p.append(bp); C_p.append(cp); n_p.append(nq); m_p.append(mq)
        pool_s.append(bs); C_s.append(cs); n_s.append(ns); m_s.append(ms)
    y_prompt = _rmsnorm(yp, g_final)
    y_sample = _rmsnorm(ys, g_final)
    return (y_prompt, y_sample, jnp.stack(pool_p), jnp.stack(C_p), jnp.stack(n_p), jnp.stack(m_p),
            jnp.stack(pool_s), jnp.stack(C_s), jnp.stack(n_s), jnp.stack(m_s))
```

```python
import contextlib
import numpy as np
import concourse.bass as bass
import concourse.mybir as mybir
from concourse.bass_utils import run_bass_kernel_spmd

F32 = mybir.dt.float32
BF16 = mybir.dt.bfloat16
AF = mybir.ActivationFunctionType
ALU = mybir.AluOpType
AX = mybir.AxisListType

ENGS = ("pe", "act", "dve", "pool", "sp")
NDMA = 8


class Prog:
    def __init__(self, nc):
        self.nc = nc
        self.lists = {e: [] for e in ENGS}
        self.cnt = {e: 0 for e in ENGS}
        self.dcnt = {e: 0 for e in ENGS}
        self.waited = {e: {} for e in ENGS}
        self.lastw = {}
        self.readers = {}
        self.out_tokens = []

    def _need(self, eng, tok, waits):
        if tok is None:
            return
        sk, v = tok
        if self.waited[eng].get(sk, 0) >= v:
            return
        if sk == "pe" and eng == "pe":
            return
        self.waited[eng][sk] = v
        waits.append((sk, v))

    def _deps(self, eng, reads, writes):
        toks = {}

        def add(t):
            if t is not None and toks.get(t[0], 0) < t[1]:
                toks[t[0]] = t[1]
        for k in reads:
            add(self.lastw.get(k))
        for k in writes:
            add(self.lastw.get(k))
            for t in self.readers.get(k, ()):
                add(t)
        waits = []
        for sk, v in toks.items():
            self._need(eng, (sk, v), waits)
        return waits

    def _commit(self, tok, reads, writes):
        for k in reads:
            self.readers.setdefault(k, []).append(tok)
        for k in writes:
            self.lastw[k] = tok
            self.readers[k] = []

    @staticmethod
    def _norm(reads, writes):
        def nk(k):
            if isinstance(k, str) and k.startswith("pss"):
                return "pss"
            return k
        def is_ps(k):
            return k in ("pss", "pacc") or (isinstance(k, tuple) and k[0] in ("psb", "pst"))
        r2, w2 = [], []
        for k in reads:
            k = nk(k)
            (w2 if is_ps(k) else r2).append(k)
        for k in writes:
            w2.append(nk(k))
        return r2, w2

    def op(self, eng, fn, reads=(), writes=()):
        reads, writes = self._norm(reads, writes)
        waits = self._deps(eng, reads, writes)
        self.cnt[eng] += 1
        tok = (eng, self.cnt[eng])
        self.lists[eng].append(("op", fn, waits, None))
        self._commit(tok, reads, writes)
        return tok

    def dma(self, q, fn, reads=(), writes=(), is_output=False):
        waits = self._deps(q, reads, writes)
        j = self.dcnt[q]
        self.dcnt[q] += 1
        sk = ("dma", q, j % NDMA)
        val = 16 * (j // NDMA + 1)
        if val > 16:
            self._need(q, (sk, val - 16), waits)
        tok = (sk, val)
        if q == "pool":
            if getattr(self, "_last_pool_tok", None) is not None:
                self._need(q, self._last_pool_tok, waits)
            self._last_pool_tok = tok
        self.lists[q].append(("dma", fn, waits, sk))
        self._commit(tok, reads, writes)
        if is_output:
            self.out_tokens.append(tok)
        return tok

    def barrier_all(self):
        toks = [(e, self.cnt[e]) for e in ENGS if self.cnt[e] > 0]
        for q in ENGS:
            for s in range(min(NDMA, self.dcnt[q])):
                n_uses = (self.dcnt[q] - 1 - s) // NDMA + 1
                toks.append((("dma", q, s), 16 * n_uses))
        for e in ENGS:
            waits = []
            for t in toks:
                if t[0] == e:
                    continue
                self._need(e, t, waits)
            if waits:
                self.lists[e].append(("wait", None, waits, None))
        self.lastw.clear()
        self.readers.clear()

    def finish(self):
        waits = []
        for t in self.out_tokens:
            self._need("sp", t, waits)
        self.lists["sp"].append(("wait", None, waits, None))

    def emit(self):
        nc = self.nc
        with contextlib.ExitStack() as st:
            sems = {}
            for e in ENGS:
                sems[e] = st.enter_context(nc.semaphore("s_" + e))
                for s in range(NDMA):
                    sems[("dma", e, s)] = st.enter_context(nc.semaphore("d_%s_%d" % (e, s)))
            block = st.enter_context(nc.Block())

            def run(engname):
                def body(eng):
                    for kind, fn, waits, sk in self.lists[engname]:
                        for (wk, v) in waits:
                            eng.wait_ge(sems[wk], v)
                        if kind == "op":
                            fn(eng).then_inc(sems[engname], 1)
                        elif kind == "dma":
                            fn(eng).then_inc(sems[sk], 16)
                return body

            block.tensor(run("pe"))
            block.scalar(run("act"))
            block.vector(run("dve"))
            block.gpsimd(run("pool"))
            block.sync(run("sp"))


D = 2048
KC = 16
NPRE = 1024
NOWN = 1152
TPRE = 8
TOWN = 9
H = 4
HD = 512
PW = 1024
U0, Q0, K0, V0, O0, IG0, FG0, GA0, GB0, INC = 0, 1024, 3072, 5120, 7168, 9216, 9220, 9224, 11272, 13320
NE = 32
EFF = 512
EPS = 1e-6
NEG = -30000.0
KSCALE = HD ** -0.5
TGS = [(0, 512), (512, 512), (1024, 128)]


class Bump:
    TOTAL = 204800

    def __init__(self, arena, start):
        self.arena = arena
        self.top = start
        self.peak = start

    def view(self, off, shape, dt):
        esz = 2 if dt == BF16 else 4
        n = 1
        for s in shape[1:]:
            n *= s
        nbytes = n * esz
        assert off % 4 == 0 and nbytes % 4 == 0 and off + nbytes <= self.TOTAL, (off, shape, nbytes)
        v = self.arena[0:shape[0], off // 4:(off + nbytes) // 4]
        if dt == BF16:
            v = v.bitcast(BF16)
        if len(shape) == 3:
            v = v.rearrange("p (a b) -> p a b", a=shape[1])
        elif len(shape) == 4:
            v = v.rearrange("p (a b c) -> p a b c", a=shape[1], b=shape[2])
        return v

    def alloc(self, shape, dt):
        esz = 2 if dt == BF16 else 4
        n = 1
        for s in shape[1:]:
            n *= s
        nbytes = (n * esz + 3) // 4 * 4
        shape2 = list(shape)
        if nbytes != n * esz:
            assert len(shape) == 2
            shape2 = [shape[0], nbytes // esz]
        off = (self.top + 63) // 64 * 64
        self.top = off + nbytes
        self.peak = max(self.peak, self.top)
        assert self.top <= self.TOTAL, ("SBUF arena overflow", self.top, shape)
        v = self.view(off, shape2, dt)
        if shape2 != list(shape):
            v = v[:, 0:shape[1]]
        return v


class Scope:
    def __init__(self, bump):
        self.bump = bump

    def __enter__(self):
        self.mark = self.bump.top
        return self

    def __exit__(self, *a):
        self.bump.top = self.mark
        return False


class Ring:
    def __init__(self, st, nc, name, shape, dt, n, psum=False, bump=None):
        if psum:
            self.t = [st.enter_context(nc.psum_tensor("%s%d" % (name, i), shape, dt)) for i in range(n)]
        else:
            self.t = [bump.alloc(shape, dt) for i in range(n)]
        self.k = [(name, i) for i in range(n)]
        self.i = 0

    def next(self):
        j = self.i % len(self.t)
        self.i += 1
        return self.t[j], self.k[j]


_DBG = {}


def build(debug=None, stop=None):
    nc = bass.Bass("TRN2", target_bir_lowering=False)
    P = Prog(nc)
    debug = debug or ()

    def maybe_stop(tag):
        if stop == tag:
            P.barrier_all()
            P.finish()
            P.emit()
            _DBG['P'] = P
            return True
        return False

    def din(name, shape):
        return nc.dram_tensor(name, shape, F32, kind="ExternalInput").ap()

    def dout(name, shape):
        return nc.dram_tensor(name, shape, F32, kind="ExternalOutput").ap()

    xo = din("xo", [NOWN, D]); xp = din("xp", [NPRE, D])
    flag_d = din("flag", [128, 1]); rc_d = din("rc", [64])
    spool = din("spool", [16, 15, PW]); sC = din("sC", [16, H, HD, HD]); sn = din("sn", [64, HD]); sm = din("sm", [16, H])
    g_mix = din("g_mix", [D]); w_in = din("w_in", [D, INC]); b_if = din("b_if", [8])
    w_pool = din("w_pool", [4, 256, 256]); pool_scale = din("pool_scale", [PW])
    w_pa = din("w_pa", [PW, D]); w_pb = din("w_pb", [D, D]); g_head = din("g_head", [D]); w_out = din("w_out", [D, D])
    g_ffn = din("g_ffn", [D]); w_rg = din("w_rg", [D, 4]); b_rg = din("b_rg", [4]); w_re = din("w_re", [D, NE]); b_re = din("b_re", [NE])
    w_eg = din("w_eg", [NE, D, EFF]); w_eu = din("w_eu", [NE, D, EFF]); w_ed = din("w_ed", [NE, EFF, D]); g_final = din("g_final", [D])

    y_d = dout("y", [NOWN, D]); poolp_d = dout("pool_p", [15, PW]); Cp_d = dout("C_p", [H, HD, HD]); np_d = dout("n_p", [H, HD]); mp_d = dout("m_p", [H, 1])
    pools_d = dout("pool_s", [16, 15, PW]); Cs_d = dout("C_s", [16, H, HD, HD]); ns_d = dout("n_s", [64, HD]); ms_d = dout("m_s", [16, H])
    cpre_d = dout("scr_cpre", [H, 128, 4 * HD])
    npre_d = dout("scr_npre", [H, 128, 4])

    def kchunks(ap2d):
        return ap2d.rearrange("(c p) n -> p c n", p=128)

    dv, pl, ac, pe = "dve", "pool", "act", "pe"

    with contextlib.ExitStack() as st0:
        Y0, M0, A0, AEND = 0, 73728, 110592, 129024
        arena = st0.enter_context(nc.sbuf_tensor("arena", [128, Bump.TOTAL // 4], F32))
        bump = Bump(arena, AEND)
        view = bump.view

        def sb(name, shape, dt=F32, st=None):
            return bump.alloc(shape, dt)

        hNTo = view(Y0, [128, KC, NOWN], BF16)
        hbT = view(Y0 + 36864, [128, KC, NOWN], BF16)
        yacc = view(Y0, [128, TOWN, D], F32)
        hNTp = view(M0, [128, KC, NPRE], BF16)
        mixT = view(M0, [128, KC, NOWN], BF16)
        hFT = view(M0, [128, KC, NOWN], BF16)
        aT = view(A0, [128, 8, NOWN], BF16)

        PSB = Ring(st0, nc, "psb", [128, 512], F32, 4, psum=True)
        pss = st0.enter_context(nc.psum_tensor("pss", [128, 512], F32))
        pacc = st0.enter_context(nc.psum_tensor("pacc", [128, 512], F32))
        PST = Ring(st0, nc, "pst", [128, 1024], BF16, 2, psum=True)

        identb = sb("identb", [128, 128], BF16); identf = sb("identf", [128, 128])
        maskP = sb("maskP", [128, 128]); maskS = sb("maskS", [128, 128])
        E16 = sb("E16", [16, 128]); blk = sb("blk", [128, 16])
        sel = sb("sel", [4, 4, 128]); ones_b = sb("ones_b", [128, 1], BF16)
        flag = sb("flag", [128, 1]); rcb = sb("rcb", [128, 4, 16])
        gbc = sb("gbc", [128, D])

        QA = Bump.TOTAL // 16
        for qi, eng_ in enumerate([dv, pl, dv, pl]):
            P.op(eng_, lambda e, qi=qi: e.memset(arena[:, qi * QA:(qi + 1) * QA], 0.0), writes=[("arena0", qi)])
        P.barrier_all()
        P.op(pl, lambda e: e.memset(identf[:], 1.0), writes=["identf"])
        P.op(pl, lambda e: e.affine_select(out=identf[:], in_=identf[:], pattern=[[-1, 128]], compare_op=ALU.is_equal, fill=0.0, base=0, channel_multiplier=1), reads=["identf"], writes=["identf"])
        P.op(dv, lambda e: e.tensor_copy(out=identb[:], in_=identf[:]), reads=["identf"], writes=["identb"])
        P.op(pl, lambda e: e.memset(maskP[:], 0.0), writes=["maskP"])
        P.op(pl, lambda e: e.affine_select(out=maskP[:], in_=maskP[:], pattern=[[1, 128]], compare_op=ALU.is_ge, fill=NEG, base=0, channel_multiplier=-1), reads=["maskP"], writes=["maskP"])
        P.op(pl, lambda e: e.memset(E16[:], 1.0), writes=["E16"])
        P.op(pl, lambda e: e.affine_select(out=E16[:], in_=E16[:], pattern=[[1, 128]], compare_op=ALU.is_ge, fill=0.0, base=0, channel_multiplier=-8), reads=["E16"], writes=["E16"])
        P.op(pl, lambda e: e.affine_select(out=E16[:], in_=E16[:], pattern=[[-1, 128]], compare_op=ALU.is_ge, fill=0.0, base=7, channel_multiplier=8), reads=["E16"], writes=["E16"])
        P.op(pe, lambda e: e.matmul(pss[:, 0:128], lhsT=E16[:], rhs=E16[:], start=True, stop=True), reads=["E16"], writes=["pss"])
        P.op(dv, lambda e: e.tensor_scalar(out=maskS[:], in0=pss[:, 0:128], scalar1=-1.0, scalar2=-NEG, op0=ALU.add, op1=ALU.mult), reads=["pss"], writes=["maskS"])
        P.op(dv, lambda e: e.tensor_tensor(out=maskS[:], in0=maskS[:], in1=maskP[:], op=ALU.add), reads=["maskS", "maskP"], writes=["maskS"])
        P.op(pe, lambda e: e.transpose(out=pss[:, 128:144], in_=E16[:], identity=identf[0:16, 0:16]), reads=["E16", "identf", "maskS"], writes=["pss"])
        P.op(dv, lambda e: e.tensor_copy(out=blk[:], in_=pss[:, 128:144]), reads=["pss"], writes=["blk"])
        P.op(pl, lambda e: e.memset(sel[:], 1.0), writes=["sel"])
        P.op(pl, lambda e: e.affine_select(out=sel[:], in_=sel[:], pattern=[[-1, 4], [0, 128]], compare_op=ALU.is_equal, fill=0.0, base=0, channel_multiplier=1), reads=["sel"], writes=["sel"])
        P.op(pl, lambda e: e.memset(ones_b[:], 1.0), writes=["ones_b"])
        P.dma("sp", lambda e: e.dma_start(out=flag[:], in_=flag_d), writes=["flag"])
        P.dma("sp", lambda e: e.dma_start(out=rcb[:].rearrange("p a b -> p (a b)"), in_=rc_d.partition_broadcast(128)), writes=["rcb"])
        P.dma("sp", lambda e: e.dma_start(out=gbc[:], in_=g_mix.partition_broadcast(128)), writes=["gbc"])

        def dbg(name, ap_sb, shape, cast=False):
            if name not in debug:
                return
            d = dout("dbg_" + name, shape)
            P.barrier_all()
            P.dma("pool" if cast else "sp", lambda e: e.dma_start(out=d, in_=ap_sb), is_output=True)
            P.barrier_all()

        def rmsnorm_T(src, dstT, dst_key, tok0, xt_ring, hn_ring, small):
            if isinstance(src, tuple):
                _, xt_ap, xk = src
            else:
                xt, xk = xt_ring.next()
                P.dma("sp", lambda e: e.dma_start(out=xt[:], in_=src), writes=[xk])
                xt_ap = xt[:]
            hn, hk = hn_ring.next()
            ssq, junk = small
            P.op(ac, lambda e: e.activation(out=junk[:], in_=xt_ap, func=AF.Square, accum_out=ssq[:, 0:1]), reads=[xk], writes=["junk", "ssq"])
            P.op(ac, lambda e: e.activation(out=ssq[:, 1:2], in_=ssq[:, 0:1], func=AF.Sqrt, scale=1.0 / D, bias=EPS), reads=["ssq"], writes=["ssq"])
            P.op(dv, lambda e: e.reciprocal(out=ssq[:, 2:3], in_=ssq[:, 1:2]), reads=["ssq"], writes=["ssq"])
            P.op(dv, lambda e: e.scalar_tensor_tensor(out=hn[:], in0=xt_ap, scalar=ssq[:, 2:3], in1=gbc[:], op0=ALU.mult, op1=ALU.mult), reads=[xk, "ssq", "gbc"], writes=[hk])
            for half in range(2):
                pt, pk = PST.next()
                for c in range(8):
                    cc = half * 8 + c
                    P.op(pe, lambda e, c=c, cc=cc, pt=pt: e.transpose(out=pt[:, c * 128:(c + 1) * 128], in_=hn[:, cc * 128:(cc + 1) * 128], identity=identb[:]), reads=[hk, "identb"], writes=[pk])
                if half == 0:
                    P.op(ac, lambda e, pt=pt, half=half: e.activation(out=dstT[:, half * 8:(half + 1) * 8, tok0:tok0 + 128], in_=pt[:].rearrange("p (c t) -> p c t", c=8), func=AF.Copy), reads=[pk], writes=[(dst_key, tok0 // 128, half)])
                else:
                    P.op(dv, lambda e, pt=pt, half=half: e.tensor_copy(out=dstT[:, half * 8:(half + 1) * 8, tok0:tok0 + 128], in_=pt[:].rearrange("p (c t) -> p c t", c=8)), reads=[pk], writes=[(dst_key, tok0 // 128, half)])

        def wload(dst, key, src_ap, slow=False):
            P.barrier_all()
            P.dma("pool", lambda e: e.dma_start(out=dst, in_=src_ap, allow_slow_non_contiguous=slow), writes=[key])
            P.barrier_all()

        with Scope(bump) as stMid:
            colq = sb("colq", [128, TPRE + TOWN, 20], F32, stMid)
            WLI = sb("WLI", [4, 32], F32, stMid); wliS = sb("wliS", [128, 32, 4], F32, stMid)
            alpha = sb("alpha", [4, NOWN], F32, stMid)
            uhist = sb("uhist", [128, 8, 16], F32, stMid)
            with Scope(bump) as stA:
                xt_ring = Ring(stA, nc, "xt", [128, D], F32, 2, bump=bump)
                hn_ring = Ring(stA, nc, "hn", [128, D], BF16, 2, bump=bump)
                ssq = sb("ssqA", [128, 4], F32, stA); junk = sb("junkA", [128, D], BF16, stA)
                for i in range(TPRE):
                    rmsnorm_T(xp[i * 128:(i + 1) * 128, :], hNTp, "hNTp", i * 128, xt_ring, hn_ring, (ssq, junk))
                for i in range(TOWN):
                    rmsnorm_T(xo[i * 128:(i + 1) * 128, :], hNTo, "hNTo", i * 128, xt_ring, hn_ring, (ssq, junk))
                P.barrier_all()
                if maybe_stop("A"):
                    return nc
            NT = NPRE + NOWN
            with Scope(bump) as stB:
                R = [sb("row%d" % i, [4, NT], F32, stB) for i in range(6)]
                ig, lf, mrow, brow, arow, beta = R
                Wig = sb("Wig", [128, KC, 4], BF16, stB); Wfg = sb("Wfg", [128, KC, 4], BF16, stB)
                bi = sb("bi", [4, 2], F32, stB); nbf = sb("nbf", [4, 1], F32, stB)
                smT = sb("smT", [4, 16], F32, stB); zrow = sb("zrow", [4, 128], F32, stB)
                mprev = sb("mprev", [4, 16], F32, stB); tmp4 = sb("tmp4", [4, 16], F32, stB)
                with nc.allow_non_contiguous_dma(reason="tiny gate loads"):
                    wload(Wig[:], "Wig", kchunks(w_in[:, IG0:IG0 + 4]), True)
                    wload(Wfg[:], "Wfg", kchunks(w_in[:, FG0:FG0 + 4]), True)
                    P.dma("sp", lambda e: e.dma_start(out=bi[:], in_=b_if.rearrange("(two h) -> h two", two=2), allow_slow_non_contiguous=True), writes=["bi"])
                    P.dma("sp", lambda e: e.dma_start(out=smT[:], in_=sm.rearrange("j h -> h j"), allow_slow_non_contiguous=True), writes=["smT"])
                P.op(dv, lambda e: e.tensor_scalar(out=nbf[:], in0=bi[:, 1:2], scalar1=-1.0, scalar2=None, op0=ALU.mult), reads=["bi"], writes=["nbf"])
                P.op(dv, lambda e: e.memset(zrow[:], 0.0), writes=["zrow"])
                if maybe_stop("B1"):
                    return nc
                groups = [(hNTp, 0, 512, 0), (hNTp, 512, 512, 512)] + [(hNTo, t0, n, NPRE + t0) for (t0, n) in TGS]
                for (src, t0, n, col0) in groups:
                    pg, pgk = PSB.next(); pf, pfk = PSB.next()
                    for c in range(KC):
                        P.op(pe, lambda e, c=c, pg=pg, src=src, t0=t0, n=n: e.matmul(pg[0:4, 0:n], lhsT=Wig[:, c, :], rhs=src[:, c, t0:t0 + n], start=(c == 0), stop=(c == KC - 1)), reads=["Wig"], writes=[pgk])
                    for c in range(KC):
                        P.op(pe, lambda e, c=c, pf=pf, src=src, t0=t0, n=n: e.matmul(pf[0:4, 0:n], lhsT=Wfg[:, c, :], rhs=src[:, c, t0:t0 + n], start=(c == 0), stop=(c == KC - 1)), reads=["Wfg"], writes=[pfk])
                    P.op(ac, lambda e, pg=pg, col0=col0, n=n: e.activation(out=ig[:, col0:col0 + n], in_=pg[0:4, 0:n], func=AF.Identity, bias=bi[:, 0:1], scale=1.0), reads=[pgk, "bi"], writes=["ig"])
                    P.op(ac, lambda e, pf=pf, col0=col0, n=n: e.activation(out=lf[:, col0:col0 + n], in_=pf[0:4, 0:n], func=AF.Exp, bias=nbf[:, 0:1], scale=-1.0), reads=[pfk, "nbf"], writes=["lf"])
                P.op(ac, lambda e: e.activation(out=lf[:], in_=lf[:], func=AF.Ln, bias=1.0, scale=1.0), reads=["lf"], writes=["lf"])
                P.op(dv, lambda e: e.tensor_scalar(out=lf[:], in0=lf[:], scalar1=-1.0, scalar2=None, op0=ALU.mult), reads=["lf"], writes=["lf"])
                if maybe_stop("B2"):
                    return nc
                P.op(dv, lambda e: e.tensor_tensor_scan(out=mrow[:, 0:NPRE], data0=lf[:, 0:NPRE], data1=ig[:, 0:NPRE], initial=0.0, op0=ALU.add, op1=ALU.max), reads=["lf", "ig"], writes=["mrow"])
                P.op(dv, lambda e: e.memset(mprev[:], 0.0), writes=["mprev"])
                P.op(dv, lambda e: e.tensor_tensor(out=mprev[:, 8:9], in0=mrow[:, NPRE - 1:NPRE], in1=flag[0:4, :], op=ALU.mult), reads=["mrow", "flag", "mprev"], writes=["mprev"])
                P.op(dv, lambda e: e.tensor_tensor_scan(out=mrow[:, NPRE:2048], data0=lf[:, NPRE:2048], data1=ig[:, NPRE:2048], initial=mprev[:, 8:9], op0=ALU.add, op1=ALU.max), reads=["lf", "ig", "mprev", "mrow"], writes=["mrow"])
                for j in range(16):
                    a = 2048 + 8 * j
                    P.op(dv, lambda e, a=a, j=j: e.tensor_tensor_scan(out=mrow[:, a:a + 8], data0=lf[:, a:a + 8], data1=ig[:, a:a + 8], initial=smT[:, j:j + 1], op0=ALU.add, op1=ALU.max), reads=["lf", "ig", "smT", "mrow"], writes=["mrow"])
                    P.op(dv, lambda e, a=a: e.tensor_tensor_scan(out=brow[:, a:a + 8], data0=lf[:, a:a + 8], data1=zrow[:, 0:8], initial=0.0, op0=ALU.add, op1=ALU.add), reads=["lf", "zrow", "brow"], writes=["brow"])
                for i in range(16):
                    a = 128 * i
                    P.op(dv, lambda e, a=a: e.tensor_tensor_scan(out=brow[:, a:a + 128], data0=lf[:, a:a + 128], data1=zrow[:], initial=0.0, op0=ALU.add, op1=ALU.add), reads=["lf", "zrow", "brow"], writes=["brow"])
                P.op(dv, lambda e: e.tensor_copy(out=mprev[:, 1:8], in_=mrow[:, 127:127 + 7 * 128:128]), reads=["mrow", "mprev"], writes=["mprev"])
                P.op(dv, lambda e: e.tensor_copy(out=mprev[:, 9:16], in_=mrow[:, NPRE + 127:NPRE + 127 + 7 * 128:128]), reads=["mrow", "mprev"], writes=["mprev"])
                if maybe_stop("B3"):
                    return nc
                P.dma("sp", lambda e: e.dma_start(out=mp_d, in_=mrow[:, 2047:2048]), reads=["mrow"], is_output=True)
                with nc.allow_non_contiguous_dma(reason="tiny m out"):
                    P.dma("sp", lambda e: e.dma_start(out=ms_d.rearrange("j h -> h j"), in_=mrow[:, 2048 + 7:NT:8], allow_slow_non_contiguous=True), reads=["mrow"], is_output=True)
                P.op(dv, lambda e: e.tensor_tensor(out=arow[:], in0=brow[:], in1=mrow[:], op=ALU.subtract), reads=["brow", "mrow"], writes=["arow"])
                if maybe_stop("B4"):
                    return nc
                P.op(dv, lambda e: e.tensor_tensor(out=beta[:], in0=ig[:], in1=brow[:], op=ALU.subtract), reads=["ig", "brow"], writes=["beta"])
                P.op(dv, lambda e: e.tensor_copy(out=alpha[:], in_=arow[:, NPRE:NT]), reads=["arow"], writes=["alpha"])
                emneg, winter, wl = brow, lf, ig
                P.op(ac, lambda e: e.activation(out=emneg[:], in_=mrow[:], func=AF.Exp, scale=-1.0), reads=["mrow", "brow", "beta", "arow"], writes=["brow"])
                for i in range(16):
                    a = 128 * i
                    P.op(ac, lambda e, a=a, i=i: e.activation(out=winter[:, a:a + 128], in_=arow[:, a:a + 128], func=AF.Exp, bias=mprev[:, i:i + 1], scale=1.0), reads=["arow", "mprev", "lf", "mrow"], writes=["lf"])
                    P.op(ac, lambda e, a=a: e.activation(out=wl[:, a:a + 128], in_=beta[:, a:a + 128], func=AF.Exp, bias=arow[:, a + 127:a + 128], scale=1.0), reads=["beta", "arow", "ig"], writes=["ig"])
                for j in range(16):
                    a = 2048 + 8 * j
                    P.op(ac, lambda e, a=a, j=j: e.activation(out=winter[:, a:a + 8], in_=arow[:, a:a + 8], func=AF.Exp, bias=smT[:, j:j + 1], scale=1.0), reads=["arow", "smT", "lf"], writes=["lf"])
                    P.op(ac, lambda e, a=a: e.activation(out=wl[:, a:a + 8], in_=beta[:, a:a + 8], func=AF.Exp, bias=arow[:, a + 7:a + 8], scale=1.0), reads=["beta", "arow", "ig"], writes=["ig"])
                P.op(dv, lambda e: e.tensor_tensor(out=tmp4[:], in0=arow[:, 127:2048:128], in1=mprev[:], op=ALU.add), reads=["arow", "mprev"], writes=["tmp4"])
                P.op(ac, lambda e: e.activation(out=WLI[:, 0:16], in_=tmp4[:], func=AF.Exp), reads=["tmp4"], writes=["WLI"])
                P.op(dv, lambda e: e.tensor_tensor(out=tmp4[:], in0=arow[:, 2048 + 7:NT:8], in1=smT[:], op=ALU.add), reads=["arow", "smT", "tmp4", "WLI"], writes=["tmp4"])
                P.op(ac, lambda e: e.activation(out=WLI[:, 16:32], in_=tmp4[:], func=AF.Exp), reads=["tmp4", "WLI"], writes=["WLI"])
                if maybe_stop("B5"):
                    return nc
                for i in range(TPRE + TOWN):
                    a = 128 * i
                    for qi, (rowt, rk) in enumerate([(beta, "beta"), (winter, "lf"), (emneg, "brow"), (wl, "ig"), (arow, "arow")]):
                        P.op(pe, lambda e, rowt=rowt, a=a, qi=qi: e.transpose(out=pss[:, 256 + 4 * qi:256 + 4 * qi + 4], in_=rowt[:, a:a + 128], identity=identf[0:4, 0:4]), reads=[rk, "identf"], writes=["pssq"])
                    P.op(dv, lambda e, i=i: e.tensor_copy(out=colq[:, i, :], in_=pss[:, 256:276]), reads=["pssq"], writes=[("colq", i)])
                    if i == 0 and maybe_stop("B5a"):
                        return nc
                    if i == 7 and maybe_stop("B5b"):
                        return nc
                    if i == 16 and maybe_stop("B5c"):
                        return nc
                rowc_r = Ring(stB, nc, "rowc", [4, 128], F32, 2, bump=bump)
                for i in range(32):
                    rowc, rck = rowc_r.next()
                    P.op(ac, lambda e, rowc=rowc, i=i: e.activation(out=rowc[:], in_=zrow[:], func=AF.Identity, bias=WLI[:, i:i + 1], scale=0.0), reads=["WLI", "zrow"], writes=[rck])
                    P.op(pe, lambda e, rowc=rowc, i=i: e.transpose(out=pss[:, 288 + 4 * i:288 + 4 * i + 4], in_=rowc[:], identity=identf[0:4, 0:4]), reads=[rck], writes=["pssw"])
                P.op(dv, lambda e: e.tensor_copy(out=wliS[:].rearrange("p i h -> p (i h)"), in_=pss[:, 288:416]), reads=["pssw"], writes=["wliS"])
                if maybe_stop("B6"):
                    return nc
                wuB = [view(Y0 + 36864 + k * 4096, [128, KC, 128], BF16) for k in range(2)]
                for fc in range(8):
                    wu, wk = wuB[fc % 2], ("wuB", fc % 2)
                    wload(wu, wk, kchunks(w_in[:, U0 + fc * 128:U0 + (fc + 1) * 128]))
                    pu, puk = PSB.next()
                    for c in range(KC):
                        P.op(pe, lambda e, c=c, wu=wu, pu=pu: e.matmul(pu[:, 0:16], lhsT=wu[:, c, :], rhs=hNTp[:, c, NPRE - 16:NPRE], start=(c == 0), stop=(c == KC - 1)), reads=[wk], writes=[puk])
                    P.op(ac, lambda e, fc=fc, pu=pu: e.activation(out=uhist[:, fc, :], in_=pu[:, 0:16], func=AF.Copy), reads=[puk], writes=[("uhist", fc)])
                if "rows" in debug:
                    for nm, t in [("ig_wl", ig), ("lf_winter", lf), ("mrow", mrow), ("b_emneg", brow), ("arow", arow), ("beta", beta)]:
                        dbg_ap = t[:]
                        d = dout("dbg_" + nm, [4, NT])
                        P.barrier_all()
                        P.dma("sp", lambda e, d=d, dbg_ap=dbg_ap: e.dma_start(out=d, in_=dbg_ap), is_output=True)
                P.barrier_all()
                if maybe_stop("B"):
                    return nc
            dbg("hNTo", hNTo, [128, KC, NOWN])
            dbg("colq", colq[:], [128, TPRE + TOWN, 20])
            with Scope(bump) as stC0:
                WkC0 = [view(Y0 + 36864 + k * 16384, [128, KC, HD], BF16) for k in range(2)]
                Wv_r = Ring(stC0, nc, "WvC0_", [128, KC, HD], BF16, 2, bump=bump)
                pad0 = sb("pad0", [128, 1024], F32, stC0)
                Cst = sb("Cst0", [128, 4, HD], F32, stC0); nst = sb("nst0", [128, 4], F32, stC0)
                kt_r = Ring(stC0, nc, "ktC0_", [128, HD], BF16, 2, bump=bump); v_r = Ring(stC0, nc, "vC0_", [128, HD], BF16, 2, bump=bump)
                wlib = sb("wlib0", [128, 2], F32, stC0)

                WvA = view(A0, [128, KC, HD], BF16)
                for h in range(H):
                    Wk, wkk = WkC0[1], ("WkC0", 1)
                    Wv, wvk = WvA, "WvA"
                    wload(Wk, wkk, kchunks(w_in[:, K0 + h * HD:K0 + (h + 1) * HD]))
                    wload(Wv, wvk, kchunks(w_in[:, V0 + h * HD:V0 + (h + 1) * HD]))
                    P.op(dv, lambda e: e.memset(Cst[:], 0.0), reads=["Cst"], writes=["Cst"])
                    P.op(dv, lambda e: e.memset(nst[:], 0.0), reads=["nst"], writes=["nst"])
                    for i in range(TPRE):
                        pk_, pkk = PSB.next(); pv_, pvk = PSB.next()
                        for c in range(KC):
                            P.op(pe, lambda e, c=c, pk_=pk_, i=i, Wk=Wk: e.matmul(pk_[:], lhsT=hNTp[:, c, i * 128:(i + 1) * 128], rhs=Wk[:, c, :], start=(c == 0), stop=(c == KC - 1)), reads=[wkk], writes=[pkk])
                        for c in range(KC):
                            P.op(pe, lambda e, c=c, pv_=pv_, i=i, Wv=Wv: e.matmul(pv_[:], lhsT=hNTp[:, c, i * 128:(i + 1) * 128], rhs=Wv[:, c, :], start=(c == 0), stop=(c == KC - 1)), reads=[wvk], writes=[pvk])
                        kt, ktk = kt_r.next(); vt, vk = v_r.next()
                        P.op(dv, lambda e, kt=kt, pk_=pk_, i=i, h=h: e.tensor_scalar(out=kt[:], in0=pk_[:], scalar1=colq[:, i, 12 + h:13 + h], scalar2=KSCALE, op0=ALU.mult, op1=ALU.mult), reads=[pkk], writes=[ktk])
                        P.op(ac, lambda e, vt=vt, pv_=pv_: e.activation(out=vt[:], in_=pv_[:], func=AF.Copy), reads=[pvk], writes=[vk])
                        if h == 0 and i == 0:
                            dbg("c0v", vt[:], [128, HD], cast=True)
                            dbg("c0kt", kt[:], [128, HD], cast=True)
                            dbg("c0wv", Wv[:, 0, :], [128, HD], cast=True)
                            dbg("c0wv15", Wv[:, 15, :], [128, HD], cast=True)
                            dbg("c0hn", hNTp[:, 0, 0:128], [128, 128], cast=True)
                        for c2 in range(4):
                            pkv, pkvk = PSB.next()
                            P.op(pe, lambda e, c2=c2, pkv=pkv, kt=kt, vt=vt: e.matmul(pkv[:], lhsT=kt[:, c2 * 128:(c2 + 1) * 128], rhs=vt[:], start=True, stop=True), reads=[ktk, vk], writes=[pkvk])
                            P.op(dv, lambda e, c2=c2, pkv=pkv, h=h, i=i: e.scalar_tensor_tensor(out=Cst[:, c2, :], in0=Cst[:, c2, :], scalar=wliS[:, i, h:h + 1], in1=pkv[:], op0=ALU.mult, op1=ALU.add), reads=["Cst", pkvk], writes=["Cst"])
                        for c2 in range(4):
                            P.op(pe, lambda e, c2=c2, kt=kt: e.matmul(pss[:, 8 + c2:9 + c2], lhsT=kt[:, c2 * 128:(c2 + 1) * 128], rhs=ones_b[:], start=True, stop=True), reads=[ktk], writes=["pss1"])
                        P.op(dv, lambda e, h=h, i=i: e.scalar_tensor_tensor(out=nst[:], in0=nst[:], scalar=wliS[:, i, h:h + 1], in1=pss[:, 8:12], op0=ALU.mult, op1=ALU.add), reads=["nst", "pss1"], writes=["nst"])
                        P.barrier_all()
                        if h == 0 and i == 0:
                            dbg("c0cst", Cst[:], [128, 4, HD])
                        if h == 1 and i == 0:
                            dbg("wvA", Wv, [128, KC, HD], cast=True)
                        if h == 1 and i == 7:
                            dbg("wvB", Wv, [128, KC, HD], cast=True)
                            dbg("c0cst7b", Cst[:], [128, 4, HD])
                        if h == 0 and i == 7:
                            dbg("c0cst7", Cst[:], [128, 4, HD])
                            dbg("c0v7", vt[:], [128, HD], cast=True)
                            dbg("c0kt7", kt[:], [128, HD], cast=True)
                        if h == 0 and i == 3:
                            dbg("c0cst3", Cst[:], [128, 4, HD])
                    P.op(dv, lambda e: e.tensor_scalar(out=Cst[:], in0=Cst[:], scalar1=flag[:, 0:1], scalar2=None, op0=ALU.mult), reads=["Cst"], writes=["Cst"])
                    P.op(dv, lambda e: e.tensor_scalar(out=nst[:], in0=nst[:], scalar1=flag[:, 0:1], scalar2=None, op0=ALU.mult), reads=["nst"], writes=["nst"])
                    if h == 0:
                        dbg("c0cstF", Cst[:], [128, 4, HD])
                        dbg("wliS", wliS[:], [128, 32, 4])
                        dbg("flag", flag[:], [128, 1])
                    P.dma("sp", lambda e, h=h: e.dma_start(out=cpre_d[h], in_=Cst[:].rearrange("p c v -> p (c v)")), reads=["Cst"], writes=[("cpre", h)])
                    P.dma("sp", lambda e, h=h: e.dma_start(out=npre_d[h], in_=nst[:]), reads=["nst"], writes=[("npre", h)])
                P.barrier_all()
                if maybe_stop("C0"):
                    return nc
            with Scope(bump) as stC:
                Wq = view(M0, [128, KC, HD], BF16); Wk = view(M0 + 16384, [128, KC, HD], BF16); Wv = view(M0 + 32768, [128, KC, HD], BF16)
                Cst = sb("Cst", [128, 4, HD], F32, stC); Cb = sb("Cb", [128, 4, HD], BF16, stC)
                nst = sb("nst", [128, 4], F32, stC); nb = sb("nb", [128, 4], BF16, stC)
                ghb = sb("ghb", [128, HD], F32, stC)
                s_qT = sb("s_qT", [128, H, 4, 128], BF16, stC); s_kt = sb("s_kt", [128, H, HD], BF16, stC)
                s_v = sb("s_v", [128, H, HD], BF16, stC); s_num = sb("s_num", [128, H, HD], F32, stC); s_den = sb("s_den", [128, H], F32, stC)
                qT_r = Ring(stC, nc, "qT", [128, 4, 128], BF16, 2, bump=bump); kT_r = Ring(stC, nc, "kT", [128, 4, 128], BF16, 2, bump=bump)
                kt_r = Ring(stC, nc, "kt", [128, HD], BF16, 2, bump=bump); v_r = Ring(stC, nc, "vt", [128, HD], BF16, 2, bump=bump)
                Mt_r = Ring(stC, nc, "Mt", [128, 128], F32, 2, bump=bump)
                Wt_r = Ring(stC, nc, "Wt", [128, 128], F32, 2, bump=bump); PT_r = Ring(stC, nc, "PT", [128, 128], BF16, 2, bump=bump)
                num_r = Ring(stC, nc, "num", [128, HD], F32, 1, bump=bump); tmpn_r = Ring(stC, nc, "tmpn", [128, HD], F32, 1, bump=bump)
                hbt_r = Ring(stC, nc, "hbt", [128, HD], BF16, 2, bump=bump)
                sm_r = Ring(stC, nc, "smalls", [128, 16], F32, 2, bump=bump)
                junkC = sb("junkC", [128, HD], BF16, stC)

                def finish_tile(h, i, num_ap, numk, den_ap, small, smk):
                    P.op(dv, lambda e: e.tensor_scalar(out=small[:, 4:5], in0=den_ap, scalar1=-1.0, scalar2=None, op0=ALU.mult), reads=[smk], writes=[smk])
                    P.op(dv, lambda e: e.tensor_tensor(out=small[:, 4:5], in0=small[:, 4:5], in1=den_ap, op=ALU.max), reads=[smk], writes=[smk])
                    P.op(dv, lambda e: e.tensor_tensor(out=small[:, 4:5], in0=small[:, 4:5], in1=colq[:, TPRE + i, 8 + h:9 + h], op=ALU.max), reads=[smk], writes=[smk])
                    P.op(dv, lambda e: e.reciprocal(out=small[:, 5:6], in_=small[:, 4:5]), reads=[smk], writes=[smk])
                    P.op(ac, lambda e: e.activation(out=junkC[:], in_=num_ap, func=AF.Square, accum_out=small[:, 6:7]), reads=[numk, smk], writes=["junkC", smk])
                    P.op(dv, lambda e: e.tensor_scalar(out=small[:, 7:8], in0=small[:, 6:7], scalar1=small[:, 5:6], scalar2=small[:, 5:6], op0=ALU.mult, op1=ALU.mult), reads=[smk], writes=[smk])
                    P.op(ac, lambda e: e.activation(out=small[:, 8:9], in_=small[:, 7:8], func=AF.Sqrt, scale=1.0 / HD, bias=EPS), reads=[smk], writes=[smk])
                    P.op(dv, lambda e: e.reciprocal(out=small[:, 9:10], in_=small[:, 8:9]), reads=[smk], writes=[smk])
                    P.op(dv, lambda e: e.tensor_tensor(out=small[:, 10:11], in0=small[:, 9:10], in1=small[:, 5:6], op=ALU.mult), reads=[smk], writes=[smk])
                    hbt, hbk = hbt_r.next()
                    P.op(dv, lambda e: e.scalar_tensor_tensor(out=hbt[:], in0=num_ap, scalar=small[:, 10:11], in1=ghb[:], op0=ALU.mult, op1=ALU.mult), reads=[numk, smk, "ghb"], writes=[hbk])
                    pt, pk = PST.next()
                    for c in range(4):
                        P.op(pe, lambda e, c=c, pt=pt: e.transpose(out=pt[:, c * 128:(c + 1) * 128], in_=hbt[:, c * 128:(c + 1) * 128], identity=identb[:]), reads=[hbk], writes=[pk])
                    P.op(ac, lambda e, pt=pt: e.activation(out=hbT[:, 4 * h:4 * h + 4, i * 128:(i + 1) * 128], in_=pt[:, 0:512].rearrange("p (c t) -> p c t", c=4), func=AF.Copy), reads=[pk], writes=[("hbT", h, i)])

                for h in range(H):
                    wload(Wq, "Wq", kchunks(w_in[:, Q0 + h * HD:Q0 + (h + 1) * HD]))
                    wload(Wk, "Wk", kchunks(w_in[:, K0 + h * HD:K0 + (h + 1) * HD]))
                    wload(Wv, "Wv", kchunks(w_in[:, V0 + h * HD:V0 + (h + 1) * HD]))
                    P.dma("sp", lambda e, h=h: e.dma_start(out=ghb[:], in_=g_head[h * HD:(h + 1) * HD].partition_broadcast(128)), writes=["ghb"])
                    P.dma("sp", lambda e, h=h: e.dma_start(out=Cst[:].rearrange("p c v -> p (c v)"), in_=cpre_d[h]), writes=["Cst"])
                    P.dma("sp", lambda e, h=h: e.dma_start(out=nst[:], in_=npre_d[h]), writes=["nst"])
                    P.op(ac, lambda e: e.activation(out=Cb[:], in_=Cst[:], func=AF.Copy), reads=["Cst"], writes=["Cb"])
                    P.op(ac, lambda e: e.activation(out=nb[:], in_=nst[:], func=AF.Copy), reads=["nst"], writes=["nb"])
                    if h == 0:
                        dbg("cst0", Cst[:], [128, 4, HD])
                    for i in range(TOWN):
                        samp = (i == TOWN - 1)
                        ci = TPRE + i
                        if samp:
                            qT, qk = s_qT[:, h], ("s_qT", h)
                            ktl, ktk = s_kt[:, h], ("s_kt", h)
                            vt, vk = s_v[:, h], ("s_v", h)
                        else:
                            t_, qk = qT_r.next(); qT = t_[:]
                            t_, ktk = kt_r.next(); ktl = t_[:]
                            t_, vk = v_r.next(); vt = t_[:]
                        t_, kTk = kT_r.next(); kT = t_[:]
                        pq, pqk = PSB.next()
                        for cc in range(4):
                            for c in range(KC):
                                P.op(pe, lambda e, c=c, cc=cc, pq=pq, i=i: e.matmul(pq[:, cc * 128:(cc + 1) * 128], lhsT=Wq[:, c, cc * 128:(cc + 1) * 128], rhs=hNTo[:, c, i * 128:(i + 1) * 128], start=(c == 0), stop=(c == KC - 1)), reads=["Wq"], writes=[pqk])
                        P.op(ac, lambda e, pq=pq, qT=qT: e.activation(out=qT.rearrange("p c t -> p (c t)"), in_=pq[:], func=AF.Copy), reads=[pqk], writes=[qk])
                        pkT, pkTk = PSB.next()
                        for cc in range(4):
                            for c in range(KC):
                                P.op(pe, lambda e, c=c, cc=cc, pkT=pkT, i=i: e.matmul(pkT[:, cc * 128:(cc + 1) * 128], lhsT=Wk[:, c, cc * 128:(cc + 1) * 128], rhs=hNTo[:, c, i * 128:(i + 1) * 128], start=(c == 0), stop=(c == KC - 1)), reads=["Wk"], writes=[pkTk])
                        P.op(dv, lambda e, pkT=pkT, kT=kT: e.tensor_scalar(out=kT.rearrange("p c t -> p (c t)"), in0=pkT[:], scalar1=KSCALE, scalar2=None, op0=ALU.mult), reads=[pkTk], writes=[kTk])
                        pk_, pkk = PSB.next()
                        for c in range(KC):
                            P.op(pe, lambda e, c=c, pk_=pk_, i=i: e.matmul(pk_[:], lhsT=hNTo[:, c, i * 128:(i + 1) * 128], rhs=Wk[:, c, :], start=(c == 0), stop=(c == KC - 1)), reads=["Wk"], writes=[pkk])
                        P.op(dv, lambda e, pk_=pk_, ktl=ktl, ci=ci, h=h: e.tensor_scalar(out=ktl, in0=pk_[:], scalar1=colq[:, ci, 12 + h:13 + h], scalar2=KSCALE, op0=ALU.mult, op1=ALU.mult), reads=[pkk], writes=[ktk])
                        pv_, pvk = PSB.next()
                        for c in range(KC):
                            P.op(pe, lambda e, c=c, pv_=pv_, i=i: e.matmul(pv_[:], lhsT=hNTo[:, c, i * 128:(i + 1) * 128], rhs=Wv[:, c, :], start=(c == 0), stop=(c == KC - 1)), reads=["Wv"], writes=[pvk])
                        P.op(ac, lambda e, pv_=pv_, vt=vt: e.activation(out=vt, in_=pv_[:], func=AF.Copy), reads=[pvk], writes=[vk])
                        if h == 0 and i == 0:
                            dbg("v0", vt, [128, HD], cast=True)
                            dbg("kt0", ktl, [128, HD], cast=True)
                            dbg("wv", Wv[:, 0, :], [128, HD], cast=True)
                            dbg("qT0", qT, [128, 4, 128], cast=True)
                        pS, pSk = PSB.next()
                        for c in range(4):
                            P.op(pe, lambda e, c=c, pS=pS, kT=kT, qT=qT: e.matmul(pS[:, 0:128], lhsT=kT[:, c, :], rhs=qT[:, c, :], start=(c == 0), stop=(c == 3)), reads=[kTk, qk], writes=[pSk])
                        Mt, Mtk = Mt_r.next()
                        P.op(ac, lambda e, Mt=Mt, ci=ci, h=h: e.activation(out=Mt[:], in_=maskP[:], func=AF.Identity, bias=colq[:, ci, 16 + h:17 + h], scale=0.0), reads=[], writes=[Mtk])
                        P.op(pe, lambda e, pS=pS, Mt=Mt: e.transpose(out=pS[:, 128:256], in_=Mt[:], identity=identf[:]), reads=[Mtk], writes=[pSk])
                        Wt, Wtk = Wt_r.next(); PT, PTk = PT_r.next()
                        msk = maskS if samp else maskP
                        P.op(dv, lambda e, Wt=Wt, pS=pS, msk=msk: e.tensor_tensor(out=Wt[:], in0=pS[:, 128:256], in1=msk[:], op=ALU.add), reads=[pSk], writes=[Wtk])
                        P.op(ac, lambda e, Wt=Wt, ci=ci, h=h: e.activation(out=Wt[:], in_=Wt[:], func=AF.Exp, bias=colq[:, ci, h:h + 1], scale=1.0), reads=[Wtk], writes=[Wtk])
                        P.op(dv, lambda e, Wt=Wt, PT=PT, pS=pS: e.tensor_tensor(out=PT[:], in0=pS[:, 0:128], in1=Wt[:], op=ALU.mult), reads=[pSk, Wtk], writes=[PTk])
                        pn, pnk = PSB.next()
                        P.op(pe, lambda e, pn=pn, PT=PT, vt=vt: e.matmul(pn[:], lhsT=PT[:], rhs=vt, start=True, stop=True), reads=[PTk, vk], writes=[pnk])
                        P.op(pe, lambda e, PT=PT: e.matmul(pss[:, 16:17], lhsT=PT[:], rhs=ones_b[:], start=True, stop=True), reads=[PTk], writes=["pssd"])
                        if samp:
                            P.op(ac, lambda e, pn=pn, h=h: e.activation(out=s_num[:, h, :], in_=pn[:], func=AF.Copy), reads=[pnk], writes=[("s_num", h)])
                            P.op(ac, lambda e, h=h: e.activation(out=s_den[:, h:h + 1], in_=pss[:, 16:17], func=AF.Copy), reads=["pssd"], writes=[("s_den", h)])
                            continue
                        pi_, pik = PSB.next()
                        for c in range(4):
                            P.op(pe, lambda e, c=c, pi_=pi_, qT=qT: e.matmul(pi_[:], lhsT=qT[:, c, :], rhs=Cb[:, c, :], start=(c == 0), stop=(c == 3)), reads=[qk, "Cb"], writes=[pik])
                        for c in range(4):
                            P.op(pe, lambda e, c=c, qT=qT: e.matmul(pss[:, 17:18], lhsT=qT[:, c, :], rhs=nb[:, c:c + 1], start=(c == 0), stop=(c == 3)), reads=[qk, "nb"], writes=["pssd"])
                        small, smk = sm_r.next()
                        P.op(ac, lambda e, small=small: e.activation(out=small[:, 0:2], in_=pss[:, 16:18], func=AF.Copy), reads=["pssd"], writes=[smk])
                        tmpn, tmpk = tmpn_r.next(); num, numk = num_r.next()
                        P.op(ac, lambda e, tmpn=tmpn, pi_=pi_, ci=ci, h=h: e.activation(out=tmpn[:], in_=pi_[:], func=AF.Copy, scale=colq[:, ci, 4 + h:5 + h]), reads=[pik], writes=[tmpk])
                        P.op(dv, lambda e, num=num, tmpn=tmpn, pn=pn: e.tensor_tensor(out=num[:], in0=tmpn[:], in1=pn[:], op=ALU.add), reads=[tmpk, pnk], writes=[numk])
                        P.op(dv, lambda e, small=small, ci=ci, h=h: e.scalar_tensor_tensor(out=small[:, 3:4], in0=small[:, 1:2], scalar=colq[:, ci, 4 + h:5 + h], in1=small[:, 0:1], op0=ALU.mult, op1=ALU.add), reads=[smk], writes=[smk])
                        finish_tile(h, i, num[:], numk, small[:, 3:4], small, smk)
                        if h == 0 and i == 0:
                            dbg("num0", num[:], [128, HD])
                            dbg("small0", small[:], [128, 16])
                            dbg("Wt0", Wt[:], [128, 128])
                        for c2 in range(4):
                            pkv, pkvk = PSB.next()
                            P.op(pe, lambda e, c2=c2, pkv=pkv, ktl=ktl, vt=vt: e.matmul(pkv[:], lhsT=ktl[:, c2 * 128:(c2 + 1) * 128], rhs=vt, start=True, stop=True), reads=[ktk, vk], writes=[pkvk])
                            P.op(dv, lambda e, c2=c2, pkv=pkv, h=h, i=i: e.scalar_tensor_tensor(out=Cst[:, c2, :], in0=Cst[:, c2, :], scalar=wliS[:, 8 + i, h:h + 1], in1=pkv[:], op0=ALU.mult, op1=ALU.add), reads=["Cst", pkvk], writes=["Cst"])
                        for c2 in range(4):
                            P.op(pe, lambda e, c2=c2, ktl=ktl: e.matmul(pss[:, 24 + c2:25 + c2], lhsT=ktl[:, c2 * 128:(c2 + 1) * 128], rhs=ones_b[:], start=True, stop=True), reads=[ktk], writes=["pssn"])
                        P.op(dv, lambda e, h=h, i=i: e.scalar_tensor_tensor(out=nst[:], in0=nst[:], scalar=wliS[:, 8 + i, h:h + 1], in1=pss[:, 24:28], op0=ALU.mult, op1=ALU.add), reads=["nst", "pssn"], writes=["nst"])
                        if i < TOWN - 2:
                            P.op(ac, lambda e: e.activation(out=Cb[:], in_=Cst[:], func=AF.Copy), reads=["Cst"], writes=["Cb"])
                            P.op(ac, lambda e: e.activation(out=nb[:], in_=nst[:], func=AF.Copy), reads=["nst"], writes=["nb"])
                    P.dma("sp", lambda e, h=h: e.dma_start(out=Cp_d[h].rearrange("(c p) v -> p c v", p=128), in_=Cst[:]), reads=["Cst"], is_output=True)
                    with nc.allow_non_contiguous_dma(reason="n out"):
                        P.dma("sp", lambda e, h=h: e.dma_start(out=np_d[h].rearrange("(c p) -> p c", p=128), in_=nst[:], allow_slow_non_contiguous=True), reads=["nst"], is_output=True)
                P.barrier_all()
                if maybe_stop("C1"):
                    return nc

                Cj_v = [view(M0 + k * 8192, [128, 4, HD], F32) for k in range(3)]
                Z_v = [view(M0 + 24576 + k * 3968, [128, 4, 248], F32) for k in range(2)]
                o2 = M0 + 24576 + 2 * 3968
                ktj_v = [view(o2 + k * 1024, [128, HD], BF16) for k in range(2)]
                n0 = view(o2 + 2048, [128, HD], F32)
                nout = view(o2 + 4096, [128, HD], F32)
                n0T = view(o2 + 6144, [128, 4, 64], F32)
                nnT = view(o2 + 7168, [128, 4, 64], F32)
                P.dma("sp", lambda e: e.dma_start(out=n0[0:64, :], in_=sn), writes=["n0"])
                for c in range(4):
                    P.op(pe, lambda e, c=c: e.transpose(out=pss[:, 64 * c:64 * c + 64], in_=n0[0:64, c * 128:(c + 1) * 128], identity=identf[0:64, 0:64]), reads=["n0"], writes=["pss"])
                P.op(dv, lambda e: e.tensor_copy(out=n0T.rearrange("p c j -> p (c j)"), in_=pss[:, 0:256]), reads=["pss"], writes=["n0T"])
                for k in range(2):
                    P.op(dv, lambda e, k=k: e.memset(Z_v[k], 0.0), writes=[("Z", k)])
                ci = TPRE + TOWN - 1
                seq = [(h, j) for h in range(H) for j in range(16)]

                def c2_load(idx):
                    h, j = seq[idx]
                    P.dma("sp", lambda e: e.dma_start(out=Cj_v[idx % 3], in_=sC[j, h].rearrange("(c p) v -> p c v", p=128)), writes=[("Cj", idx % 3)])
                c2_load(0); c2_load(1)
                for idx, (h, j) in enumerate(seq):
                    if idx + 2 < len(seq):
                        c2_load(idx + 2)
                    Cj, Cjk = Cj_v[idx % 3], ("Cj", idx % 3)
                    Z, Zk = Z_v[idx % 2], ("Z", idx % 2)
                    P.op(dv, lambda e, Z=Z, j=j, h=h: e.tensor_copy(out=Z[:, :, 120:128], in_=s_qT[:, h, :, 8 * j:8 * j + 8]), reads=[Zk], writes=[Zk])
                    for c in range(4):
                        P.op(pe, lambda e, c=c, Z=Z, Cj=Cj, j=j: e.matmul(pacc[:], lhsT=Z[:, c, 120 - 8 * j:248 - 8 * j], rhs=Cj[:, c, :], start=(j == 0 and c == 0), stop=(j == 15 and c == 3)), reads=[Zk, Cjk], writes=["pacc"])
                    for c in range(4):
                        P.op(pe, lambda e, c=c, Z=Z, j=j, h=h: e.matmul(pss[:, 320 + j:321 + j], lhsT=Z[:, c, 120 - 8 * j:248 - 8 * j], rhs=n0T[:, c, 4 * j + h:4 * j + h + 1], start=(c == 0), stop=(c == 3)), reads=[Zk, "n0T"], writes=["pssd2"])
                    ktj, ktjk = ktj_v[idx % 2], ("ktj", idx % 2)
                    P.op(dv, lambda e, ktj=ktj, j=j, h=h: e.tensor_scalar(out=ktj, in0=s_kt[:, h, :], scalar1=blk[:, j:j + 1], scalar2=None, op0=ALU.mult), reads=[ktjk], writes=[ktjk])
                    for c2 in range(4):
                        pkv, pkvk = PSB.next()
                        P.op(pe, lambda e, c2=c2, pkv=pkv, ktj=ktj, h=h: e.matmul(pkv[:], lhsT=ktj[:, c2 * 128:(c2 + 1) * 128], rhs=s_v[:, h, :], start=True, stop=True), reads=[ktjk], writes=[pkvk])
                        P.op(dv, lambda e, c2=c2, pkv=pkv, Cj=Cj, j=j, h=h: e.scalar_tensor_tensor(out=Cj[:, c2, :], in0=Cj[:, c2, :], scalar=wliS[:, 16 + j, h:h + 1], in1=pkv[:], op0=ALU.mult, op1=ALU.add), reads=[Cjk, pkvk], writes=[Cjk])
                    for c2 in range(4):
                        P.op(pe, lambda e, c2=c2, ktj=ktj: e.matmul(pss[:, 304 + c2:305 + c2], lhsT=ktj[:, c2 * 128:(c2 + 1) * 128], rhs=ones_b[:], start=True, stop=True), reads=[ktjk], writes=["pssn2"])
                    P.op(dv, lambda e, j=j, h=h: e.scalar_tensor_tensor(out=nnT[:, :, 4 * j + h], in0=n0T[:, :, 4 * j + h], scalar=wliS[:, 16 + j, h:h + 1], in1=pss[:, 304:308], op0=ALU.mult, op1=ALU.add), reads=["n0T", "pssn2", "nnT"], writes=["nnT"])
                    P.dma("pool", lambda e, Cj=Cj, j=j, h=h: e.dma_start(out=Cs_d[j, h].rearrange("(c p) v -> p c v", p=128), in_=Cj), reads=[Cjk], is_output=True)
                    if j == 15:
                        small, smk = sm_r.next()
                        P.op(dv, lambda e, small=small: e.tensor_reduce(out=small[:, 1:2], in_=pss[:, 320:336], axis=AX.X, op=ALU.add), reads=["pssd2"], writes=[smk])
                        tmpn, tmpk = tmpn_r.next(); num, numk = num_r.next()
                        P.op(ac, lambda e, tmpn=tmpn, h=h: e.activation(out=tmpn[:], in_=pacc[:], func=AF.Copy, scale=colq[:, ci, 4 + h:5 + h]), reads=["pacc"], writes=[tmpk])
                        P.op(dv, lambda e, num=num, tmpn=tmpn, h=h: e.tensor_tensor(out=num[:], in0=tmpn[:], in1=s_num[:, h, :], op=ALU.add), reads=[tmpk], writes=[numk])
                        P.op(dv, lambda e, small=small, h=h: e.scalar_tensor_tensor(out=small[:, 3:4], in0=small[:, 1:2], scalar=colq[:, ci, 4 + h:5 + h], in1=s_den[:, h:h + 1], op0=ALU.mult, op1=ALU.add), reads=[smk], writes=[smk])
                        P.dma("sp", lambda e, h=h: e.dma_start(out=ghb[:], in_=g_head[h * HD:(h + 1) * HD].partition_broadcast(128)), writes=["ghb"])
                        finish_tile(h, TOWN - 1, num[:], numk, small[:, 3:4], small, smk)
                for c in range(4):
                    P.op(pe, lambda e, c=c: e.transpose(out=pss[0:64, 128 * c:128 * c + 128], in_=nnT[:, c, :], identity=identf[:]), reads=["nnT"], writes=["pss", "pssd2", "pssn2"])
                P.op(dv, lambda e: e.tensor_copy(out=nout[0:64, :], in_=pss[0:64, :]), reads=["pss"], writes=["nout"])
                P.dma("sp", lambda e: e.dma_start(out=ns_d, in_=nout[0:64, :]), reads=["nout"], is_output=True)
                P.barrier_all()
                if maybe_stop("C2"):
                    return nc
            dbg("hbT", hbT, [128, KC, NOWN])
            with Scope(bump) as stP:
                pooledT = view(M0, [128, 8, NOWN], BF16)
                hist = view(M0 + 18432, [128, 8, 240], F32)
                utok = view(M0 + 26112, [128, PW], F32); utoks = view(M0 + 30208, [128, PW], F32)
                hld = Ring(stP, nc, "hld", [120, PW], F32, 1, bump=bump)
                full = sb("full", [128, 1040], F32, stP); wsA = sb("wsA", [128, 1040], F32, stP); wsB = sb("wsB", [128, 1040], F32, stP)
                fulls = sb("fulls", [128, 16, 23], F32, stP); wsAs = sb("wsAs", [128, 16, 23], F32, stP); wsBs = sb("wsBs", [128, 16, 23], F32, stP)
                tmp16 = sb("tmp16", [128, 16], F32, stP)
                pscale = sb("pscale", [128, 8], F32, stP)
                wu_ring = Ring(stP, nc, "wuP", [128, KC, 128], BF16, 2, bump=bump)
                wut_ring = Ring(stP, nc, "wutP", [128, KC, 512], BF16, 1, bump=bump)
                wp_ring = Ring(stP, nc, "wpP", [128, 2, 256], BF16, 2, bump=bump)
                with nc.allow_non_contiguous_dma(reason="pool scale"):
                    P.dma("sp", lambda e: e.dma_start(out=pscale[:], in_=pool_scale.rearrange("(c p) -> p c", p=128), allow_slow_non_contiguous=True), writes=["pscale"])
                for half in range(2):
                    hl, hlk = hld.next()
                    P.dma("sp", lambda e, hl=hl, half=half: e.dma_start(out=hl[:], in_=spool[8 * half:8 * half + 8].rearrange("j r d -> (j r) d")), writes=[hlk])
                    for fc in range(8):
                        pt_, ptk = PSB.next()
                        P.op(pe, lambda e, fc=fc, hl=hl, pt_=pt_: e.transpose(out=pt_[:, 0:120], in_=hl[:, fc * 128:(fc + 1) * 128], identity=identf[0:120, 0:120]), reads=[hlk], writes=[ptk])
                        P.op(ac, lambda e, fc=fc, half=half, pt_=pt_: e.activation(out=hist[:, fc, 120 * half:120 * half + 120], in_=pt_[:, 0:120], func=AF.Copy), reads=[ptk], writes=[("hist", fc, half)])
                for cb in range(2):
                    wut, wutk = wut_ring.next()
                    wload(wut[:], wutk, kchunks(w_in[:, U0 + cb * 512:U0 + (cb + 1) * 512]))
                    for (ti, dst, dk) in [(7, utok, "utok"), (8, utoks, "utoks")]:
                        pu, puk = PSB.next()
                        for c in range(KC):
                            P.op(pe, lambda e, c=c, pu=pu, ti=ti, wut=wut: e.matmul(pu[:], lhsT=hNTo[:, c, ti * 128:(ti + 1) * 128], rhs=wut[:, c, :], start=(c == 0), stop=(c == KC - 1)), reads=[wutk], writes=[puk])
                        P.op(ac, lambda e, pu=pu, dst=dst, cb=cb: e.activation(out=dst[:, cb * 512:(cb + 1) * 512], in_=pu[:], func=AF.Copy), reads=[puk], writes=[(dk, cb)])
                P.dma("sp", lambda e: e.dma_start(out=poolp_d, in_=utok[113:128, :]), reads=[("utok", 0), ("utok", 1)], is_output=True)
                for j in range(16):
                    P.dma("sp", lambda e, j=j: e.dma_start(out=pools_d[j, 7:15, :], in_=utoks[8 * j:8 * j + 8, :]), reads=[("utoks", 0), ("utoks", 1)], is_output=True)
                P.dma("sp", lambda e: e.dma_start(out=pools_d[:, 0:7, :], in_=spool[:, 8:15, :]), is_output=True)
                wu_t = {}

                def pool_load(fc):
                    wu, wk = wu_ring.next()
                    wload(wu[:], wk, kchunks(w_in[:, U0 + fc * 128:U0 + (fc + 1) * 128]))
                    wu_t[fc] = (wu, wk)
                pool_load(0)
                for fc in range(8):
                    if fc + 1 < 8:
                        pool_load(fc + 1)
                    g = fc // 2
                    w = 2 << g
                    wu, wk = wu_t[fc]
                    P.op(dv, lambda e, fc=fc: e.tensor_copy(out=full[:, 0:16], in_=uhist[:, fc, :]), reads=["full"], writes=["full"])
                    P.op(dv, lambda e, fc=fc: e.tensor_copy(out=fulls[:, :, 0:15], in_=hist[:, fc, :].rearrange("p (j r) -> p j r", r=15)), reads=[("hist", fc, 0), ("hist", fc, 1), "fulls"], writes=["fulls"])
                    for (t0, n) in TGS:
                        pu, puk = PSB.next()
                        for c in range(KC):
                            P.op(pe, lambda e, c=c, pu=pu, t0=t0, n=n, wu=wu: e.matmul(pu[:, 0:n], lhsT=wu[:, c, :], rhs=hNTo[:, c, t0:t0 + n], start=(c == 0), stop=(c == KC - 1)), reads=[wk], writes=[puk])
                        if t0 < 1024:
                            P.op(ac, lambda e, pu=pu, t0=t0, n=n: e.activation(out=full[:, 16 + t0:16 + t0 + n], in_=pu[:, 0:n], func=AF.Copy), reads=[puk, "full"], writes=["full"])
                        else:
                            P.op(ac, lambda e, pu=pu: e.activation(out=fulls[:, :, 15:23], in_=pu[:, 0:128].rearrange("p (j r) -> p j r", r=8), func=AF.Copy), reads=[puk, "fulls"], writes=["fulls"])
                    src, srck, srcs, srcsk = full, "full", fulls, "fulls"
                    bufs = [(wsA, "wsA", wsAs, "wsAs"), (wsB, "wsB", wsBs, "wsBs")]
                    for k in range(g + 1):
                        sh = 1 << k
                        dst, dstk, dsts, dstsk = bufs[k % 2]
                        P.op(dv, lambda e, src=src, dst=dst, sh=sh: e.tensor_tensor(out=dst[:, sh:1040], in0=src[:, sh:1040], in1=src[:, 0:1040 - sh], op=ALU.add), reads=[srck, dstk], writes=[dstk])
                        P.op(dv, lambda e, srcs=srcs, dsts=dsts, sh=sh: e.tensor_tensor(out=dsts[:, :, sh:23], in0=srcs[:, :, sh:23], in1=srcs[:, :, 0:23 - sh], op=ALU.add), reads=[srcsk, dstsk], writes=[dstsk])
                        src, srck, srcs, srcsk = dst, dstk, dsts, dstsk
                    P.op(dv, lambda e, src=src, fc=fc, w=w: e.scalar_tensor_tensor(out=pooledT[:, fc, 0:1024], in0=src[:, 16:1040], scalar=1.0 / w, in1=full[:, 16:1040], op0=ALU.mult, op1=ALU.subtract), reads=[srck, "full"], writes=[("pooledT", fc)])
                    P.op(dv, lambda e, src=src, g=g: e.tensor_tensor(out=tmp16[:], in0=src[:, 16:32], in1=rcb[:, g, :], op=ALU.mult), reads=[srck, "tmp16"], writes=["tmp16"])
                    P.op(dv, lambda e, fc=fc: e.tensor_tensor(out=pooledT[:, fc, 0:16], in0=tmp16[:], in1=full[:, 16:32], op=ALU.subtract), reads=["tmp16", "full", ("pooledT", fc)], writes=[("pooledT", fc)])
                    P.op(dv, lambda e, srcs=srcs, fc=fc, w=w: e.scalar_tensor_tensor(out=pooledT[:, fc, 1024:1152].rearrange("p (j r) -> p j r", r=8), in0=srcs[:, :, 15:23], scalar=1.0 / w, in1=fulls[:, :, 15:23], op0=ALU.mult, op1=ALU.subtract), reads=[srcsk, "fulls", ("pooledT", fc)], writes=[("pooledT", fc)])
                for g in range(4):
                    wp, wpk = wp_ring.next()
                    wload(wp[:], wpk, w_pool[g].rearrange("(c p) d -> p c d", p=128))
                    for dc in range(2):
                        for (t0, n) in TGS:
                            pm, pmk = PSB.next()
                            for cc in range(2):
                                P.op(pe, lambda e, cc=cc, pm=pm, wp=wp, dc=dc, g=g, t0=t0, n=n: e.matmul(pm[:, 0:n], lhsT=wp[:, cc, dc * 128:(dc + 1) * 128], rhs=pooledT[:, 2 * g + cc, t0:t0 + n], start=(cc == 0), stop=(cc == 1)), reads=[wpk, ("pooledT", 2 * g), ("pooledT", 2 * g + 1)], writes=[pmk])
                            P.op(ac, lambda e, pm=pm, g=g, dc=dc, t0=t0, n=n: e.activation(out=aT[:, 2 * g + dc, t0:t0 + n], in_=pm[:, 0:n], func=AF.Copy, scale=pscale[:, 2 * g + dc:2 * g + dc + 1]), reads=[pmk, "pscale"], writes=[("aT", 2 * g + dc, t0)])
                P.barrier_all()
                if maybe_stop("Pool"):
                    return nc
        dbg("aT", aT, [128, 8, NOWN])
        with Scope(bump) as stDD:
            wo_r = Ring(stDD, nc, "woD", [128, KC, 128], BF16, 2, bump=bump)
            wga_r = Ring(stDD, nc, "wga", [128, KC, 128], BF16, 2, bump=bump); wgb_r = Ring(stDD, nc, "wgb", [128, KC, 128], BF16, 2, bump=bump)
            wpa_r = Ring(stDD, nc, "wpa", [128, 8, 128], BF16, 2, bump=bump); wpb_r = Ring(stDD, nc, "wpb", [128, KC, 128], BF16, 2, bump=bump)
            sg_r = Ring(stDD, nc, "sg", [128, 512], F32, 3, bump=bump); t1_r = Ring(stDD, nc, "t1", [128, 512], F32, 2, bump=bump); t2_r = Ring(stDD, nc, "t2", [128, 512], F32, 2, bump=bump)
            wo_t = {}

            def d0_load(oc):
                wo, wok = wo_r.next()
                wload(wo[:], wok, kchunks(w_in[:, O0 + oc * 128:O0 + (oc + 1) * 128]))
                wo_t[oc] = (wo, wok)
            d0_load(0)
            for oc in range(KC):
                if oc + 1 < KC:
                    d0_load(oc + 1)
                wo, wok = wo_t[oc]
                for (t0, n) in TGS:
                    po, pok = PSB.next()
                    for c in range(KC):
                        P.op(pe, lambda e, c=c, po=po, wo=wo, t0=t0, n=n: e.matmul(po[:, 0:n], lhsT=wo[:, c, :], rhs=hNTo[:, c, t0:t0 + n], start=(c == 0), stop=(c == KC - 1)), reads=[wok], writes=[pok])
                    sg, sgk = sg_r.next()
                    P.op(ac, lambda e, sg=sg, po=po, n=n: e.activation(out=sg[:, 0:n], in_=po[:, 0:n], func=AF.Sigmoid), reads=[pok], writes=[sgk])
                    P.op(dv, lambda e, sg=sg, oc=oc, t0=t0, n=n: e.tensor_tensor(out=hbT[:, oc, t0:t0 + n], in0=hbT[:, oc, t0:t0 + n], in1=sg[:, 0:n], op=ALU.mult), reads=[sgk], writes=[("hbTg", oc, t0)])
            P.barrier_all()
            if maybe_stop("D0"):
                return nc
            w_t = {}

            def d1_load(cb):
                wga, wgak = wga_r.next(); wgb, wgbk = wgb_r.next(); wpa, wpak = wpa_r.next(); wpb, wpbk = wpb_r.next()
                wload(wga[:], wgak, kchunks(w_in[:, GA0 + cb * 128:GA0 + (cb + 1) * 128]))
                wload(wpa[:], wpak, kchunks(w_pa[:, cb * 128:(cb + 1) * 128]))
                wload(wgb[:], wgbk, kchunks(w_in[:, GB0 + cb * 128:GB0 + (cb + 1) * 128]))
                wload(wpb[:], wpbk, kchunks(w_pb[:, cb * 128:(cb + 1) * 128]))
                w_t[cb] = (wga, wgak, wgb, wgbk, wpa, wpak, wpb, wpbk)
            d1_load(0)
            for cb in range(KC):
                if cb + 1 < KC:
                    d1_load(cb + 1)
                wga, wgak, wgb, wgbk, wpa, wpak, wpb, wpbk = w_t[cb]
                for (t0, n) in TGS:
                    pga, pgak = PSB.next()
                    for c in range(KC):
                        P.op(pe, lambda e, c=c, pga=pga, wga=wga, t0=t0, n=n: e.matmul(pga[:, 0:n], lhsT=wga[:, c, :], rhs=hNTo[:, c, t0:t0 + n], start=(c == 0), stop=(c == KC - 1)), reads=[wgak], writes=[pgak])
                    pa_, pak = PSB.next()
                    for c in range(8):
                        P.op(pe, lambda e, c=c, pa_=pa_, wpa=wpa, t0=t0, n=n: e.matmul(pa_[:, 0:n], lhsT=wpa[:, c, :], rhs=aT[:, c, t0:t0 + n], start=(c == 0), stop=(c == 7)), reads=[wpak], writes=[pak])
                    sga, sgak = sg_r.next()
                    P.op(ac, lambda e, sga=sga, pga=pga, n=n: e.activation(out=sga[:, 0:n], in_=pga[:, 0:n], func=AF.Sigmoid), reads=[pgak], writes=[sgak])
                    t1, t1k = t1_r.next()
                    P.op(dv, lambda e, t1=t1, sga=sga, pa_=pa_, n=n: e.tensor_tensor(out=t1[:, 0:n], in0=sga[:, 0:n], in1=pa_[:, 0:n], op=ALU.mult), reads=[sgak, pak], writes=[t1k])
                    pgb, pgbk = PSB.next()
                    for c in range(KC):
                        P.op(pe, lambda e, c=c, pgb=pgb, wgb=wgb, t0=t0, n=n: e.matmul(pgb[:, 0:n], lhsT=wgb[:, c, :], rhs=hNTo[:, c, t0:t0 + n], start=(c == 0), stop=(c == KC - 1)), reads=[wgbk], writes=[pgbk])
                    pb_, pbk = PSB.next()
                    for c in range(KC):
                        P.op(pe, lambda e, c=c, pb_=pb_, wpb=wpb, t0=t0, n=n: e.matmul(pb_[:, 0:n], lhsT=wpb[:, c, :], rhs=hbT[:, c, t0:t0 + n], start=(c == 0), stop=(c == KC - 1)), reads=[wpbk], writes=[pbk])
                    sgb, sgbk = sg_r.next()
                    P.op(ac, lambda e, sgb=sgb, pgb=pgb, n=n: e.activation(out=sgb[:, 0:n], in_=pgb[:, 0:n], func=AF.Sigmoid), reads=[pgbk], writes=[sgbk])
                    t2, t2k = t2_r.next()
                    P.op(dv, lambda e, t2=t2, sgb=sgb, pb_=pb_, n=n: e.tensor_tensor(out=t2[:, 0:n], in0=sgb[:, 0:n], in1=pb_[:, 0:n], op=ALU.mult), reads=[sgbk, pbk], writes=[t2k])
                    P.op(dv, lambda e, t1=t1, t2=t2, cb=cb, t0=t0, n=n: e.tensor_tensor(out=mixT[:, cb, t0:t0 + n], in0=t1[:, 0:n], in1=t2[:, 0:n], op=ALU.add), reads=[t1k, t2k], writes=[("mixT", cb, t0)])
            P.barrier_all()
            if maybe_stop("D"):
                return nc
        dbg("mixT", mixT, [128, KC, NOWN])
        with Scope(bump) as stE:
            woE = Ring(stE, nc, "woE", [128, KC, 512], BF16, 2, bump=bump)
            P.dma("sp", lambda e: e.dma_start(out=yacc, in_=xo.rearrange("(t p) d -> p t d", p=128)), writes=["yacc"])
            we_t = {}

            def e_load(cb):
                wo, wok = woE.next()
                wload(wo[:], wok, kchunks(w_out[:, cb * 512:(cb + 1) * 512]))
                we_t[cb] = (wo, wok)
            e_load(0)
            for cb in range(4):
                if cb + 1 < 4:
                    e_load(cb + 1)
                wo, wok = we_t[cb]
                for t in range(TOWN):
                    px, pxk = PSB.next()
                    for c in range(KC):
                        P.op(pe, lambda e, c=c, px=px, wo=wo, t=t: e.matmul(px[:], lhsT=mixT[:, c, t * 128:(t + 1) * 128], rhs=wo[:, c, :], start=(c == 0), stop=(c == KC - 1)), reads=[wok], writes=[pxk])
                    P.op(dv, lambda e, px=px, t=t, cb=cb: e.tensor_tensor(out=yacc[:, t, cb * 512:(cb + 1) * 512], in0=yacc[:, t, cb * 512:(cb + 1) * 512], in1=px[:], op=ALU.add), reads=[pxk, "yacc"], writes=[("yacc", t, cb)])
            P.barrier_all()
            if maybe_stop("E"):
                return nc
        dbg("x1", yacc, [128, TOWN, D])
        comb = view(A0 + 9216, [128, TOWN, NE], F32)
        hT_v = [view(A0 + k * 4608, [128, 2, NOWN], BF16) for k in range(2)]
        with Scope(bump) as stF0:
            hn_ring = Ring(stF0, nc, "hnF", [128, D], BF16, 2, bump=bump)
            ssq = sb("ssqF", [128, 4], F32, stF0); junk = sb("junkF", [128, D], BF16, stF0)
            Wr = sb("Wr", [128, KC, 36], BF16, stF0); bbc = sb("bbc", [128, 36], F32, stF0)
            L_r = Ring(stF0, nc, "Lr", [128, 36], F32, 2, bump=bump); elm_r = Ring(stF0, nc, "elm", [128, 32], F32, 2, bump=bump)
            elm2_r = Ring(stF0, nc, "elm2", [128, 32], F32, 2, bump=bump); oh1_r = Ring(stF0, nc, "oh1", [128, 32], F32, 2, bump=bump); oh2_r = Ring(stF0, nc, "oh2", [128, 32], F32, 2, bump=bump)
            s_r = Ring(stF0, nc, "rs", [128, 16], F32, 2, bump=bump); j4 = sb("j4", [128, 4], F32, stF0)
            P.dma("sp", lambda e: e.dma_start(out=gbc[:], in_=g_ffn.partition_broadcast(128)), writes=["gbc"])
            with nc.allow_non_contiguous_dma(reason="router weights"):
                wload(Wr[:, :, 0:4], "Wr0", kchunks(w_rg), True)
                wload(Wr[:, :, 4:36], "Wr1", kchunks(w_re), True)
            P.dma("sp", lambda e: e.dma_start(out=bbc[:, 0:4], in_=b_rg.partition_broadcast(128)), writes=["bbc0"])
            P.dma("sp", lambda e: e.dma_start(out=bbc[:, 4:36], in_=b_re.partition_broadcast(128)), writes=["bbc1"])
            for t in range(TOWN):
                rmsnorm_T(("sb", yacc[:, t, :], ("yacc_t", t)), hFT, "hFT", t * 128, None, hn_ring, (ssq, junk))
            for t in range(TOWN):
                plg, plk = PSB.next()
                for c in range(KC):
                    P.op(pe, lambda e, c=c, plg=plg, t=t: e.matmul(plg[:, 0:36], lhsT=hFT[:, c, t * 128:(t + 1) * 128], rhs=Wr[:, c, :], start=(c == 0), stop=(c == KC - 1)), reads=["Wr0", "Wr1", ("hFT", t, 0), ("hFT", t, 1)], writes=[plk])
                L, Lk = L_r.next(); elm, ek = elm_r.next(); elm2, e2k = elm2_r.next(); oh1, o1k = oh1_r.next(); oh2, o2k = oh2_r.next(); s, sk = s_r.next()
                P.op(dv, lambda e, L=L, plg=plg: e.tensor_tensor(out=L[:], in0=plg[:, 0:36], in1=bbc[:], op=ALU.add), reads=[plk, "bbc0", "bbc1"], writes=[Lk])
                P.op(dv, lambda e, L=L, s=s: e.tensor_reduce(out=s[:, 0:1], in_=L[:, 0:4], axis=AX.X, op=ALU.max), reads=[Lk], writes=[sk])
                P.op(dv, lambda e, s=s: e.tensor_scalar(out=s[:, 1:2], in0=s[:, 0:1], scalar1=-1.0, scalar2=None, op0=ALU.mult), reads=[sk], writes=[sk])
                P.op(ac, lambda e, L=L, s=s: e.activation(out=j4[:], in_=L[:, 0:4], func=AF.Exp, bias=s[:, 1:2], scale=1.0, accum_out=s[:, 2:3]), reads=[Lk, sk], writes=["j4", sk])
                P.op(dv, lambda e, s=s: e.reciprocal(out=s[:, 3:4], in_=s[:, 2:3]), reads=[sk], writes=[sk])
                P.op(dv, lambda e, L=L, s=s: e.tensor_scalar(out=s[:, 8:12], in0=L[:, 0:4], scalar1=s[:, 0:1], scalar2=-1.0, op0=ALU.is_ge, op1=ALU.add), reads=[Lk, sk], writes=[sk])
                P.op(dv, lambda e, s=s: e.tensor_scalar(out=s[:, 8:12], in0=s[:, 8:12], scalar1=1.0e9, scalar2=None, op0=ALU.mult), reads=[sk], writes=[sk])
                for g in range(4):
                    P.op(dv, lambda e, g=g, L=L, s=s, elm=elm: e.tensor_scalar(out=elm[:, 8 * g:8 * g + 8], in0=L[:, 4 + 8 * g:12 + 8 * g], scalar1=s[:, 8 + g:9 + g], scalar2=None, op0=ALU.add), reads=[Lk, sk, ek], writes=[ek])
                P.op(dv, lambda e, s=s, elm=elm: e.tensor_reduce(out=s[:, 4:5], in_=elm[:], axis=AX.X, op=ALU.max), reads=[ek, sk], writes=[sk])
                P.op(dv, lambda e, s=s, elm=elm, oh1=oh1: e.tensor_scalar(out=oh1[:], in0=elm[:], scalar1=s[:, 4:5], scalar2=None, op0=ALU.is_ge), reads=[ek, sk], writes=[o1k])
                P.op(dv, lambda e, elm=elm, oh1=oh1, elm2=elm2: e.scalar_tensor_tensor(out=elm2[:], in0=oh1[:], scalar=-1.0e9, in1=elm[:], op0=ALU.mult, op1=ALU.add), reads=[o1k, ek], writes=[e2k])
                P.op(dv, lambda e, s=s, elm2=elm2: e.tensor_reduce(out=s[:, 5:6], in_=elm2[:], axis=AX.X, op=ALU.max), reads=[e2k, sk], writes=[sk])
                P.op(dv, lambda e, s=s, elm2=elm2, oh2=oh2: e.tensor_scalar(out=oh2[:], in0=elm2[:], scalar1=s[:, 5:6], scalar2=None, op0=ALU.is_ge), reads=[e2k, sk], writes=[o2k])
                P.op(dv, lambda e, s=s: e.tensor_tensor(out=s[:, 6:7], in0=s[:, 4:5], in1=s[:, 5:6], op=ALU.subtract), reads=[sk], writes=[sk])
                P.op(ac, lambda e, s=s: e.activation(out=s[:, 6:7], in_=s[:, 6:7], func=AF.Sigmoid), reads=[sk], writes=[sk])
                P.op(dv, lambda e, s=s: e.tensor_tensor(out=s[:, 7:8], in0=s[:, 6:7], in1=s[:, 3:4], op=ALU.mult), reads=[sk], writes=[sk])
                P.op(dv, lambda e, s=s: e.tensor_tensor(out=s[:, 12:13], in0=s[:, 3:4], in1=s[:, 7:8], op=ALU.subtract), reads=[sk], writes=[sk])
                P.op(dv, lambda e, s=s, oh1=oh1, t=t: e.tensor_scalar(out=comb[:, t, :], in0=oh1[:], scalar1=s[:, 7:8], scalar2=None, op0=ALU.mult), reads=[o1k, sk], writes=[("comb", t)])
                P.op(dv, lambda e, s=s, oh2=oh2, t=t: e.scalar_tensor_tensor(out=comb[:, t, :], in0=oh2[:], scalar=s[:, 12:13], in1=comb[:, t, :], op0=ALU.mult, op1=ALU.add), reads=[o2k, sk, ("comb", t)], writes=[("comb", t)])
            P.barrier_all()
            if maybe_stop("F0"):
                return nc
        dbg("comb", comb, [128, TOWN, NE])
        with Scope(bump) as stF1:
            Wg_r = Ring(stF1, nc, "Wg", [128, KC, 256], BF16, 2, bump=bump); Wu_r = Ring(stF1, nc, "Wu", [128, KC, 256], BF16, 2, bump=bump)
            Wd_r = Ring(stF1, nc, "Wd", [128, 2, D], BF16, 2, bump=bump)
            sgl_r = Ring(stF1, nc, "sgl", [128, 512], F32, 2, bump=bump)
            ssq = sb("ssqF1", [128, 4], F32, stF1)
            units = [(e_, fh) for e_ in range(NE) for fh in range(2)]
            wt = {}

            def f_load(u):
                e_, fh = units[u]
                Wg, wgk = Wg_r.next(); Wu, wuk = Wu_r.next(); Wd, wdk = Wd_r.next()
                wload(Wg[:], wgk, kchunks(w_eg[e_][:, fh * 256:(fh + 1) * 256]))
                wload(Wu[:], wuk, kchunks(w_eu[e_][:, fh * 256:(fh + 1) * 256]))
                wload(Wd[:], wdk, w_ed[e_][fh * 256:(fh + 1) * 256, :].rearrange("(c p) d -> p c d", p=128))
                wt[u] = (Wg, wgk, Wu, wuk, Wd, wdk)
            f_load(0)
            for u, (e_, fh) in enumerate(units):
                if u + 1 < len(units):
                    f_load(u + 1)
                Wg, wgk, Wu, wuk, Wd, wdk = wt.pop(u)
                hT, hTk = hT_v[u % 2], ("hT", u % 2)
                for fc in range(2):
                    for (t0, n) in TGS:
                        phg, phgk = PSB.next(); phu, phuk = PSB.next()
                        for c in range(KC):
                            P.op(pe, lambda e, c=c, phg=phg, Wg=Wg, fc=fc, t0=t0, n=n: e.matmul(phg[:, 0:n], lhsT=Wg[:, c, fc * 128:(fc + 1) * 128], rhs=hFT[:, c, t0:t0 + n], start=(c == 0), stop=(c == KC - 1)), reads=[wgk], writes=[phgk])
                        for c in range(KC):
                            P.op(pe, lambda e, c=c, phu=phu, Wu=Wu, fc=fc, t0=t0, n=n: e.matmul(phu[:, 0:n], lhsT=Wu[:, c, fc * 128:(fc + 1) * 128], rhs=hFT[:, c, t0:t0 + n], start=(c == 0), stop=(c == KC - 1)), reads=[wuk], writes=[phuk])
                        sgl, sglk = sgl_r.next()
                        P.op(ac, lambda e, sgl=sgl, phg=phg, n=n: e.activation(out=sgl[:, 0:n], in_=phg[:, 0:n], func=AF.Silu), reads=[phgk], writes=[sglk])
                        P.op(dv, lambda e, sgl=sgl, phu=phu, hT=hT, fc=fc, t0=t0, n=n: e.tensor_tensor(out=hT[:, fc, t0:t0 + n], in0=sgl[:, 0:n], in1=phu[:, 0:n], op=ALU.mult), reads=[sglk, phuk, hTk], writes=[hTk])
                for t in range(TOWN):
                    for cb in range(4):
                        py, pyk = PSB.next()
                        for fc in range(2):
                            P.op(pe, lambda e, fc=fc, py=py, hT=hT, Wd=Wd, t=t, cb=cb: e.matmul(py[:], lhsT=hT[:, fc, t * 128:(t + 1) * 128], rhs=Wd[:, fc, cb * 512:(cb + 1) * 512], start=(fc == 0), stop=(fc == 1)), reads=[hTk, wdk], writes=[pyk])
                        P.op(dv, lambda e, py=py, t=t, cb=cb, e_=e_: e.scalar_tensor_tensor(out=yacc[:, t, cb * 512:(cb + 1) * 512], in0=py[:], scalar=comb[:, t, e_:e_ + 1], in1=yacc[:, t, cb * 512:(cb + 1) * 512], op0=ALU.mult, op1=ALU.add), reads=[pyk, ("yacc", t, cb)], writes=[("yacc", t, cb)])
            P.dma("sp", lambda e: e.dma_start(out=gbc[:], in_=g_final.partition_broadcast(128)), writes=["gbc"])
            for t in range(TOWN):
                yk = [("yacc", t, cb) for cb in range(4)]
                sglj, sgljk = sgl_r.next()
                for cb in range(4):
                    P.op(ac, lambda e, t=t, cb=cb, sglj=sglj: e.activation(out=sglj[:], in_=yacc[:, t, cb * 512:(cb + 1) * 512], func=AF.Square, accum_out=ssq[:, cb:cb + 1]), reads=[("yacc", t, cb), "ssqF"], writes=[sgljk, "ssqF"])
                P.op(dv, lambda e: e.tensor_reduce(out=ssq[:, 0:1], in_=ssq[:, 0:4], axis=AX.X, op=ALU.add), reads=["ssqF"], writes=["ssqF"])
                P.op(ac, lambda e: e.activation(out=ssq[:, 1:2], in_=ssq[:, 0:1], func=AF.Sqrt, scale=1.0 / D, bias=EPS), reads=["ssqF"], writes=["ssqF"])
                P.op(dv, lambda e: e.reciprocal(out=ssq[:, 2:3], in_=ssq[:, 1:2]), reads=["ssqF"], writes=["ssqF"])
                P.op(dv, lambda e, t=t: e.scalar_tensor_tensor(out=yacc[:, t, :], in0=yacc[:, t, :], scalar=ssq[:, 2:3], in1=gbc[:], op0=ALU.mult, op1=ALU.mult), reads=yk + ["ssqF", "gbc"], writes=yk)
                P.dma("sp", lambda e, t=t: e.dma_start(out=y_d[t * 128:(t + 1) * 128, :], in_=yacc[:, t, :]), reads=yk, is_output=True)
            P.finish()
            P.emit()
            _DBG['P'] = P; _DBG['peak'] = bump.peak
    return nc


_NC = {}


def _get_nc(debug=None):
    key = tuple(debug) if debug else ()
    if key not in _NC:
        _NC[key] = build(debug)
    return _NC[key]


def make_in_maps(inp):
    f = lambda a: np.ascontiguousarray(np.asarray(a, dtype=np.float32))
    xpr = f(inp["x_prompt"]); xs = f(inp["x_sample"])
    shared = {
        "g_mix": f(inp["g_mix"][0]), "w_in": f(inp["w_in"][0]), "b_if": f(inp["b_if"][0]), "w_pool": f(inp["w_pool"][0]),
        "pool_scale": f(inp["pool_scale"][0]), "w_pa": f(inp["w_proj_a"][0]), "w_pb": f(inp["w_proj_b"][0]),
        "g_head": f(inp["g_head"][0]), "w_out": f(inp["w_out"][0]), "g_ffn": f(inp["g_ffn"][0]),
        "w_rg": f(inp["w_router_group"][0]), "b_rg": f(inp["b_router_group"][0]), "w_re": f(inp["w_router_expert"][0]),
        "b_re": f(inp["b_router_expert"][0]), "w_eg": f(inp["w_exp_gate"][0]), "w_eu": f(inp["w_exp_up"][0]),
        "w_ed": f(inp["w_exp_down"][0]), "g_final": f(inp["g_final"]),
    }
    spool = f(inp["state_pool"][0]); sC = f(inp["state_C"][0]); sn = f(inp["state_n"][0]); sm = f(inp["state_m"][0])
    maps = []
    for c in range(8):
        b, half = c // 2, c % 2
        xo = np.concatenate([xpr[b, half * 1024:(half + 1) * 1024], xs[16 * c:16 * c + 16].reshape(128, D)], axis=0)
        xp = xpr[b, 0:1024] if half == 1 else np.zeros((1024, D), np.float32)
        rc = np.zeros((4, 16), np.float32)
        for g in range(4):
            for t in range(16):
                rc[g, t] = 1.0 / min(half * 1024 + t + 1, 2 << g)
        m = dict(shared)
        m.update({
            "xo": np.ascontiguousarray(xo), "xp": np.ascontiguousarray(xp),
            "flag": np.full((128, 1), float(half), np.float32), "rc": rc.reshape(64),
            "spool": np.ascontiguousarray(spool[16 * c:16 * c + 16]), "sC": np.ascontiguousarray(sC[16 * c:16 * c + 16]),
            "sn": np.ascontiguousarray(sn[16 * c:16 * c + 16].reshape(64, HD)), "sm": np.ascontiguousarray(sm[16 * c:16 * c + 16]),
        })
        maps.append(m)
    return maps


def assemble(res):
    B = 4
    y_prompt = np.zeros((B, 2048, D), np.float32); y_sample = np.zeros((128, 8, D), np.float32)
    pool_p = np.zeros((1, B, 15, PW), np.float32); C_p = np.zeros((1, B, H, HD, HD), np.float32)
    n_p = np.zeros((1, B, H, HD), np.float32); m_p = np.zeros((1, B, H), np.float32)
    pool_s = np.zeros((1, 128, 15, PW), np.float32); C_s = np.zeros((1, 128, H, HD, HD), np.float32)
    n_s = np.zeros((1, 128, H, HD), np.float32); m_s = np.zeros((1, 128, H), np.float32)
    for c in range(8):
        r = res[c]
        b, half = c // 2, c % 2
        y_prompt[b, half * 1024:(half + 1) * 1024] = r["y"][0:1024]
        y_sample[16 * c:16 * c + 16] = r["y"][1024:].reshape(16, 8, D)
        if half == 1:
            pool_p[0, b] = r["pool_p"]; C_p[0, b] = r["C_p"]; n_p[0, b] = r["n_p"]; m_p[0, b] = r["m_p"][:, 0]
        pool_s[0, 16 * c:16 * c + 16] = r["pool_s"]; C_s[0, 16 * c:16 * c + 16] = r["C_s"]
        n_s[0, 16 * c:16 * c + 16] = r["n_s"].reshape(16, H, HD); m_s[0, 16 * c:16 * c + 16] = r["m_s"]
    return (y_prompt, y_sample, pool_p, C_p, n_p, m_p, pool_s, C_s, n_s, m_s)


def kernel(**inputs):
    nc = _get_nc()
    in_maps = make_in_maps(inputs)
    res = run_bass_kernel_spmd(nc, in_maps, core_ids=list(range(8)))
    return assemble(res.results)
```

```python
import contextlib
import numpy as np
import concourse.bass as bass
import concourse.mybir as mybir
from concourse.bass_utils import run_bass_kernel_spmd

F32 = mybir.dt.float32
BF16 = mybir.dt.bfloat16
AF = mybir.ActivationFunctionType
ALU = mybir.AluOpType
AX = mybir.AxisListType

ENGS = ("pe", "act", "dve", "pool", "sp")
NDMA = 8


class Prog:
    def __init__(self, nc):
        self.nc = nc
        self.lists = {e: [] for e in ENGS}
        self.cnt = {e: 0 for e in ENGS}
        self.dcnt = {e: 0 for e in ENGS}
        self.waited = {e: {} for e in ENGS}
        self.lastw = {}
        self.readers = {}
        self.out_tokens = []

    def _need(self, eng, tok, waits):
        if tok is None:
            return
        sk, v = tok
        if self.waited[eng].get(sk, 0) >= v:
            return
        if sk == "pe" and eng == "pe":
            return
        self.waited[eng][sk] = v
        waits.append((sk, v))

    def _deps(self, eng, reads, writes):
        toks = {}

        def add(t):
            if t is not None and toks.get(t[0], 0) < t[1]:
                toks[t[0]] = t[1]
        for k in reads:
            add(self.lastw.get(k))
        for k in writes:
            add(self.lastw.get(k))
            for t in self.readers.get(k, ()):
                add(t)
        waits = []
        for sk, v in toks.items():
            self._need(eng, (sk, v), waits)
        return waits

    def _commit(self, tok, reads, writes):
        for k in reads:
            self.readers.setdefault(k, []).append(tok)
        for k in writes:
            self.lastw[k] = tok
            self.readers[k] = []

    @staticmethod
    def _norm(reads, writes):
        def nk(k):
            if isinstance(k, str) and k.startswith("pss"):
                return "pss"
            return k
        def is_ps(k):
            return k in ("pss", "pacc") or (isinstance(k, tuple) and k[0] in ("psb", "pst"))
        r2, w2 = [], []
        for k in reads:
            k = nk(k)
            (w2 if is_ps(k) else r2).append(k)
        for k in writes:
            w2.append(nk(k))
        return r2, w2

    def op(self, eng, fn, reads=(), writes=()):
        reads, writes = self._norm(reads, writes)
        waits = self._deps(eng, reads, writes)
        self.cnt[eng] += 1
        tok = (eng, self.cnt[eng])
        self.lists[eng].append(("op", fn, waits, None))
        self._commit(tok, reads, writes)
        return tok

    def dma(self, q, fn, reads=(), writes=(), is_output=False):
        waits = self._deps(q, reads, writes)
        j = self.dcnt[q]
        self.dcnt[q] += 1
        sk = ("dma", q, j % NDMA)
        val = 16 * (j // NDMA + 1)
        if val > 16:
            self._need(q, (sk, val - 16), waits)
        tok = (sk, val)
        if q == "pool":
            if getattr(self, "_last_pool_tok", None) is not None:
                self._need(q, self._last_pool_tok, waits)
            self._last_pool_tok = tok
        self.lists[q].append(("dma", fn, waits, sk))
        self._commit(tok, reads, writes)
        if is_output:
            self.out_tokens.append(tok)
        return tok

    def barrier_all(self):
        toks = [(e, self.cnt[e]) for e in ENGS if self.cnt[e] > 0]
        for q in ENGS:
            for s in range(min(NDMA, self.dcnt[q])):
                n_uses = (self.dcnt[q] - 1 - s) // NDMA + 1
                toks.append((("dma", q, s), 16 * n_uses))
        for e in ENGS:
            waits = []
            for t in toks:
                if t[0] == e:
                    continue
                self._need(e, t, waits)
            if waits:
                self.lists[e].append(("wait", None, waits, None))
        self.lastw.clear()
        self.readers.clear()

    def finish(self):
        waits = []
        for t in self.out_tokens:
            self._need("sp", t, waits)
        self.lists["sp"].append(("wait", None, waits, None))

    def emit(self):
        nc = self.nc
        with contextlib.ExitStack() as st:
            sems = {}
            for e in ENGS:
                sems[e] = st.enter_context(nc.semaphore("s_" + e))
                for s in range(NDMA):
                    sems[("dma", e, s)] = st.enter_context(nc.semaphore("d_%s_%d" % (e, s)))
            block = st.enter_context(nc.Block())

            def run(engname):
                def body(eng):
                    for kind, fn, waits, sk in self.lists[engname]:
                        for (wk, v) in waits:
                            eng.wait_ge(sems[wk], v)
                        if kind == "op":
                            fn(eng).then_inc(sems[engname], 1)
                        elif kind == "dma":
                            fn(eng).then_inc(sems[sk], 16)
                return body

            block.tensor(run("pe"))
            block.scalar(run("act"))
            block.vector(run("dve"))
            block.gpsimd(run("pool"))
            block.sync(run("sp"))


D = 2048
KC = 16
NPRE = 1024
NOWN = 1152
TPRE = 8
TOWN = 9
H = 4
HD = 512
PW = 1024
U0, Q0, K0, V0, O0, IG0, FG0, GA0, GB0, INC = 0, 1024, 3072, 5120, 7168, 9216, 9220, 9224, 11272, 13320
NE = 32
EFF = 512
EPS = 1e-6
NEG = -30000.0
KSCALE = HD ** -0.5
TGS = [(0, 512), (512, 512), (1024, 128)]


class Bump:
    TOTAL = 204800

    def __init__(self, arena, start):
        self.arena = arena
        self.top = start
        self.peak = start

    def view(self, off, shape, dt):
        esz = 2 if dt == BF16 else 4
        n = 1
        for s in shape[1:]:
            n *= s
        nbytes = n * esz
        assert off % 4 == 0 and nbytes % 4 == 0 and off + nbytes <= self.TOTAL, (off, shape, nbytes)
        v = self.arena[0:shape[0], off // 4:(off + nbytes) // 4]
        if dt == BF16:
            v = v.bitcast(BF16)
        if len(shape) == 3:
            v = v.rearrange("p (a b) -> p a b", a=shape[1])
        elif len(shape) == 4:
            v = v.rearrange("p (a b c) -> p a b c", a=shape[1], b=shape[2])
        return v

    def alloc(self, shape, dt):
        esz = 2 if dt == BF16 else 4
        n = 1
        for s in shape[1:]:
            n *= s
        nbytes = (n * esz + 3) // 4 * 4
        shape2 = list(shape)
        if nbytes != n * esz:
            assert len(shape) == 2
            shape2 = [shape[0], nbytes // esz]
        off = (self.top + 63) // 64 * 64
        self.top = off + nbytes
        self.peak = max(self.peak, self.top)
        assert self.top <= self.TOTAL, ("SBUF arena overflow", self.top, shape)
        v = self.view(off, shape2, dt)
        if shape2 != list(shape):
            v = v[:, 0:shape[1]]
        return v


class Scope:
    def __init__(self, bump):
        self.bump = bump

    def __enter__(self):
        self.mark = self.bump.top
        return self

    def __exit__(self, *a):
        self.bump.top = self.mark
        return False


class Ring:
    def __init__(self, st, nc, name, shape, dt, n, psum=False, bump=None):
        if psum:
            self.t = [st.enter_context(nc.psum_tensor("%s%d" % (name, i), shape, dt)) for i in range(n)]
        else:
            self.t = [bump.alloc(shape, dt) for i in range(n)]
        self.k = [(name, i) for i in range(n)]
        self.i = 0

    def next(self):
        j = self.i % len(self.t)
        self.i += 1
        return self.t[j], self.k[j]


_DBG = {}


NOBAR = {"F1", "D0", "D1", "E", "P", "B", "F0", "C1"}


def build(debug=None, stop=None):
    nc = bass.Bass("TRN2", target_bir_lowering=False)
    P = Prog(nc)
    debug = debug or ()

    def maybe_stop(tag):
        if stop == tag:
            P.barrier_all()
            P.finish()
            P.emit()
            _DBG['P'] = P
            return True
        return False

    def din(name, shape):
        return nc.dram_tensor(name, shape, F32, kind="ExternalInput").ap()

    def dout(name, shape):
        return nc.dram_tensor(name, shape, F32, kind="ExternalOutput").ap()

    xo = din("xo", [NOWN, D]); xp = din("xp", [NPRE, D])
    flag_d = din("flag", [128, 1]); rc_d = din("rc", [64])
    spool = din("spool", [16, 15, PW]); sC = din("sC", [16, H, HD, HD]); sn = din("sn", [64, HD]); sm = din("sm", [16, H])
    g_mix = din("g_mix", [D]); w_in = din("w_in", [D, INC]); b_if = din("b_if", [8])
    w_pool = din("w_pool", [4, 256, 256]); pool_scale = din("pool_scale", [PW])
    w_pa = din("w_pa", [PW, D]); w_pb = din("w_pb", [D, D]); g_head = din("g_head", [D]); w_out = din("w_out", [D, D])
    g_ffn = din("g_ffn", [D]); w_rg = din("w_rg", [D, 4]); b_rg = din("b_rg", [4]); w_re = din("w_re", [D, NE]); b_re = din("b_re", [NE])
    w_eg = din("w_eg", [NE, D, EFF]); w_eu = din("w_eu", [NE, D, EFF]); w_ed = din("w_ed", [NE, EFF, D]); g_final = din("g_final", [D])

    y_d = dout("y", [NOWN, D]); poolp_d = dout("pool_p", [15, PW]); Cp_d = dout("C_p", [H, HD, HD]); np_d = dout("n_p", [H, HD]); mp_d = dout("m_p", [H, 1])
    pools_d = dout("pool_s", [16, 15, PW]); Cs_d = dout("C_s", [16, H, HD, HD]); ns_d = dout("n_s", [64, HD]); ms_d = dout("m_s", [16, H])
    cpre_d = dout("scr_cpre", [H, 128, 4 * HD])
    npre_d = dout("scr_npre", [H, 128, 4])

    def kchunks(ap2d):
        return ap2d.rearrange("(c p) n -> p c n", p=128)

    dv, pl, ac, pe = "dve", "pool", "act", "pe"

    with contextlib.ExitStack() as st0:
        Y0, M0, A0, AEND = 0, 73728, 110592, 129024
        arena = st0.enter_context(nc.sbuf_tensor("arena", [128, Bump.TOTAL // 4], F32))
        bump = Bump(arena, AEND)
        view = bump.view

        def sb(name, shape, dt=F32, st=None):
            return bump.alloc(shape, dt)

        hNTo = view(Y0, [128, KC, NOWN], BF16)
        hbT = view(Y0 + 36864, [128, KC, NOWN], BF16)
        yacc = view(Y0, [128, TOWN, D], F32)
        hNTp = view(M0, [128, KC, NPRE], BF16)
        mixT = view(M0, [128, KC, NOWN], BF16)
        hFT = view(M0, [128, KC, NOWN], BF16)
        aT = view(A0, [128, 8, NOWN], BF16)

        PSB = Ring(st0, nc, "psb", [128, 512], F32, 4, psum=True)
        pss = st0.enter_context(nc.psum_tensor("pss", [128, 512], F32))
        pacc = st0.enter_context(nc.psum_tensor("pacc", [128, 512], F32))
        PST = Ring(st0, nc, "pst", [128, 1024], BF16, 2, psum=True)

        identb = sb("identb", [128, 128], BF16); identf = sb("identf", [128, 128])
        maskP = sb("maskP", [128, 128]); maskS = sb("maskS", [128, 128])
        E16 = sb("E16", [16, 128]); blk = sb("blk", [128, 16])
        sel = sb("sel", [4, 4, 128]); ones_b = sb("ones_b", [128, 1], BF16)
        flag = sb("flag", [128, 1]); rcb = sb("rcb", [128, 4, 16])
        gbc = sb("gbc", [128, D])

        QA = Bump.TOTAL // 16
        for qi, eng_ in enumerate([dv, pl, dv, pl]):
            P.op(eng_, lambda e, qi=qi: e.memset(arena[:, qi * QA:(qi + 1) * QA], 0.0), writes=[("arena0", qi)])
        P.barrier_all()
        P.op(pl, lambda e: e.memset(identf[:], 1.0), writes=["identf"])
        P.op(pl, lambda e: e.affine_select(out=identf[:], in_=identf[:], pattern=[[-1, 128]], compare_op=ALU.is_equal, fill=0.0, base=0, channel_multiplier=1), reads=["identf"], writes=["identf"])
        P.op(dv, lambda e: e.tensor_copy(out=identb[:], in_=identf[:]), reads=["identf"], writes=["identb"])
        P.op(pl, lambda e: e.memset(maskP[:], 0.0), writes=["maskP"])
        P.op(pl, lambda e: e.affine_select(out=maskP[:], in_=maskP[:], pattern=[[1, 128]], compare_op=ALU.is_ge, fill=NEG, base=0, channel_multiplier=-1), reads=["maskP"], writes=["maskP"])
        P.op(pl, lambda e: e.memset(E16[:], 1.0), writes=["E16"])
        P.op(pl, lambda e: e.affine_select(out=E16[:], in_=E16[:], pattern=[[1, 128]], compare_op=ALU.is_ge, fill=0.0, base=0, channel_multiplier=-8), reads=["E16"], writes=["E16"])
        P.op(pl, lambda e: e.affine_select(out=E16[:], in_=E16[:], pattern=[[-1, 128]], compare_op=ALU.is_ge, fill=0.0, base=7, channel_multiplier=8), reads=["E16"], writes=["E16"])
        P.op(pe, lambda e: e.matmul(pss[:, 0:128], lhsT=E16[:], rhs=E16[:], start=True, stop=True), reads=["E16"], writes=["pss"])
        P.op(dv, lambda e: e.tensor_scalar(out=maskS[:], in0=pss[:, 0:128], scalar1=-1.0, scalar2=-NEG, op0=ALU.add, op1=ALU.mult), reads=["pss"], writes=["maskS"])
        P.op(dv, lambda e: e.tensor_tensor(out=maskS[:], in0=maskS[:], in1=maskP[:], op=ALU.add), reads=["maskS", "maskP"], writes=["maskS"])
        P.op(pe, lambda e: e.transpose(out=pss[:, 128:144], in_=E16[:], identity=identf[0:16, 0:16]), reads=["E16", "identf", "maskS"], writes=["pss"])
        P.op(dv, lambda e: e.tensor_copy(out=blk[:], in_=pss[:, 128:144]), reads=["pss"], writes=["blk"])
        P.op(pl, lambda e: e.memset(sel[:], 1.0), writes=["sel"])
        P.op(pl, lambda e: e.affine_select(out=sel[:], in_=sel[:], pattern=[[-1, 4], [0, 128]], compare_op=ALU.is_equal, fill=0.0, base=0, channel_multiplier=1), reads=["sel"], writes=["sel"])
        P.op(pl, lambda e: e.memset(ones_b[:], 1.0), writes=["ones_b"])
        P.dma("sp", lambda e: e.dma_start(out=flag[:], in_=flag_d), writes=["flag"])
        P.dma("sp", lambda e: e.dma_start(out=rcb[:].rearrange("p a b -> p (a b)"), in_=rc_d.partition_broadcast(128)), writes=["rcb"])
        P.dma("sp", lambda e: e.dma_start(out=gbc[:], in_=g_mix.partition_broadcast(128)), writes=["gbc"])

        def dbg(name, ap_sb, shape, cast=False):
            if name not in debug:
                return
            d = dout("dbg_" + name, shape)
            P.barrier_all()
            P.dma("pool" if cast else "sp", lambda e: e.dma_start(out=d, in_=ap_sb), is_output=True)
            P.barrier_all()

        def rmsnorm_T(src, dstT, dst_key, tok0, xt_ring, hn_ring, small):
            if isinstance(src, tuple):
                _, xt_ap, xk = src
            else:
                xt, xk = xt_ring.next()
                P.dma("sp", lambda e: e.dma_start(out=xt[:], in_=src), writes=[xk])
                xt_ap = xt[:]
            hn, hk = hn_ring.next()
            ssq, junk = small
            P.op(ac, lambda e: e.activation(out=junk[:], in_=xt_ap, func=AF.Square, accum_out=ssq[:, 0:1]), reads=[xk], writes=["junk", "ssq"])
            P.op(ac, lambda e: e.activation(out=ssq[:, 1:2], in_=ssq[:, 0:1], func=AF.Sqrt, scale=1.0 / D, bias=EPS), reads=["ssq"], writes=["ssq"])
            P.op(dv, lambda e: e.reciprocal(out=ssq[:, 2:3], in_=ssq[:, 1:2]), reads=["ssq"], writes=["ssq"])
            P.op(dv, lambda e: e.scalar_tensor_tensor(out=hn[:], in0=xt_ap, scalar=ssq[:, 2:3], in1=gbc[:], op0=ALU.mult, op1=ALU.mult), reads=[xk, "ssq", "gbc"], writes=[hk])
            for half in range(2):
                pt, pk = PST.next()
                for c in range(8):
                    cc = half * 8 + c
                    P.op(pe, lambda e, c=c, cc=cc, pt=pt: e.transpose(out=pt[:, c * 128:(c + 1) * 128], in_=hn[:, cc * 128:(cc + 1) * 128], identity=identb[:]), reads=[hk, "identb"], writes=[pk])
                if half == 0:
                    P.op(ac, lambda e, pt=pt, half=half: e.activation(out=dstT[:, half * 8:(half + 1) * 8, tok0:tok0 + 128], in_=pt[:].rearrange("p (c t) -> p c t", c=8), func=AF.Copy), reads=[pk], writes=[(dst_key, tok0 // 128, half)])
                else:
                    P.op(dv, lambda e, pt=pt, half=half: e.tensor_copy(out=dstT[:, half * 8:(half + 1) * 8, tok0:tok0 + 128], in_=pt[:].rearrange("p (c t) -> p c t", c=8)), reads=[pk], writes=[(dst_key, tok0 // 128, half)])

        def wload(dst, key, src_ap, slow=False, bar=True):
            if bar:
                P.barrier_all()
            P.dma("pool", lambda e: e.dma_start(out=dst, in_=src_ap, allow_slow_non_contiguous=slow), writes=[key])
            if bar:
                P.barrier_all()

        with Scope(bump) as stMid:
            colq = sb("colq", [128, TPRE + TOWN, 20], F32, stMid)
            WLI = sb("WLI", [4, 32], F32, stMid); wliS = sb("wliS", [128, 32, 4], F32, stMid)
            alpha = sb("alpha", [4, NOWN], F32, stMid)
            uhist = sb("uhist", [128, 8, 16], F32, stMid)
            with Scope(bump) as stA:
                xt_ring = Ring(stA, nc, "xt", [128, D], F32, 2, bump=bump)
                hn_ring = Ring(stA, nc, "hn", [128, D], BF16, 2, bump=bump)
                ssq = sb("ssqA", [128, 4], F32, stA); junk = sb("junkA", [128, D], BF16, stA)
                for i in range(TPRE):
                    rmsnorm_T(xp[i * 128:(i + 1) * 128, :], hNTp, "hNTp", i * 128, xt_ring, hn_ring, (ssq, junk))
                for i in range(TOWN):
                    rmsnorm_T(xo[i * 128:(i + 1) * 128, :], hNTo, "hNTo", i * 128, xt_ring, hn_ring, (ssq, junk))
                P.barrier_all()
                if maybe_stop("A"):
                    return nc
            NT = NPRE + NOWN
            with Scope(bump) as stB:
                R = [sb("row%d" % i, [4, NT], F32, stB) for i in range(6)]
                ig, lf, mrow, brow, arow, beta = R
                Wig = sb("Wig", [128, KC, 4], BF16, stB); Wfg = sb("Wfg", [128, KC, 4], BF16, stB)
                bi = sb("bi", [4, 2], F32, stB); nbf = sb("nbf", [4, 1], F32, stB)
                smT = sb("smT", [4, 16], F32, stB); zrow = sb("zrow", [4, 128], F32, stB)
                mprev = sb("mprev", [4, 16], F32, stB); tmp4 = sb("tmp4", [4, 16], F32, stB)
                with nc.allow_non_contiguous_dma(reason="tiny gate loads"):
                    wload(Wig[:], "Wig", kchunks(w_in[:, IG0:IG0 + 4]), True, bar=("B" not in NOBAR))
                    wload(Wfg[:], "Wfg", kchunks(w_in[:, FG0:FG0 + 4]), True, bar=("B" not in NOBAR))
                    P.dma("sp", lambda e: e.dma_start(out=bi[:], in_=b_if.rearrange("(two h) -> h two", two=2), allow_slow_non_contiguous=True), writes=["bi"])
                    P.dma("sp", lambda e: e.dma_start(out=smT[:], in_=sm.rearrange("j h -> h j"), allow_slow_non_contiguous=True), writes=["smT"])
                P.op(dv, lambda e: e.tensor_scalar(out=nbf[:], in0=bi[:, 1:2], scalar1=-1.0, scalar2=None, op0=ALU.mult), reads=["bi"], writes=["nbf"])
                P.op(dv, lambda e: e.memset(zrow[:], 0.0), writes=["zrow"])
                if maybe_stop("B1"):
                    return nc
                groups = [(hNTp, 0, 512, 0), (hNTp, 512, 512, 512)] + [(hNTo, t0, n, NPRE + t0) for (t0, n) in TGS]
                for (src, t0, n, col0) in groups:
                    pg, pgk = PSB.next(); pf, pfk = PSB.next()
                    for c in range(KC):
                        P.op(pe, lambda e, c=c, pg=pg, src=src, t0=t0, n=n: e.matmul(pg[0:4, 0:n], lhsT=Wig[:, c, :], rhs=src[:, c, t0:t0 + n], start=(c == 0), stop=(c == KC - 1)), reads=["Wig"], writes=[pgk])
                    for c in range(KC):
                        P.op(pe, lambda e, c=c, pf=pf, src=src, t0=t0, n=n: e.matmul(pf[0:4, 0:n], lhsT=Wfg[:, c, :], rhs=src[:, c, t0:t0 + n], start=(c == 0), stop=(c == KC - 1)), reads=["Wfg"], writes=[pfk])
                    P.op(ac, lambda e, pg=pg, col0=col0, n=n: e.activation(out=ig[:, col0:col0 + n], in_=pg[0:4, 0:n], func=AF.Identity, bias=bi[:, 0:1], scale=1.0), reads=[pgk, "bi"], writes=["ig"])
                    P.op(ac, lambda e, pf=pf, col0=col0, n=n: e.activation(out=lf[:, col0:col0 + n], in_=pf[0:4, 0:n], func=AF.Exp, bias=nbf[:, 0:1], scale=-1.0), reads=[pfk, "nbf"], writes=["lf"])
                P.op(ac, lambda e: e.activation(out=lf[:], in_=lf[:], func=AF.Ln, bias=1.0, scale=1.0), reads=["lf"], writes=["lf"])
                P.op(dv, lambda e: e.tensor_scalar(out=lf[:], in0=lf[:], scalar1=-1.0, scalar2=None, op0=ALU.mult), reads=["lf"], writes=["lf"])
                if maybe_stop("B2"):
                    return nc
                P.op(dv, lambda e: e.tensor_tensor_scan(out=mrow[:, 0:NPRE], data0=lf[:, 0:NPRE], data1=ig[:, 0:NPRE], initial=0.0, op0=ALU.add, op1=ALU.max), reads=["lf", "ig"], writes=["mrow"])
                P.op(dv, lambda e: e.memset(mprev[:], 0.0), writes=["mprev"])
                P.op(dv, lambda e: e.tensor_tensor(out=mprev[:, 8:9], in0=mrow[:, NPRE - 1:NPRE], in1=flag[0:4, :], op=ALU.mult), reads=["mrow", "flag", "mprev"], writes=["mprev"])
                P.op(dv, lambda e: e.tensor_tensor_scan(out=mrow[:, NPRE:2048], data0=lf[:, NPRE:2048], data1=ig[:, NPRE:2048], initial=mprev[:, 8:9], op0=ALU.add, op1=ALU.max), reads=["lf", "ig", "mprev", "mrow"], writes=["mrow"])
                for j in range(16):
                    a = 2048 + 8 * j
                    P.op(dv, lambda e, a=a, j=j: e.tensor_tensor_scan(out=mrow[:, a:a + 8], data0=lf[:, a:a + 8], data1=ig[:, a:a + 8], initial=smT[:, j:j + 1], op0=ALU.add, op1=ALU.max), reads=["lf", "ig", "smT", "mrow"], writes=["mrow"])
                    P.op(dv, lambda e, a=a: e.tensor_tensor_scan(out=brow[:, a:a + 8], data0=lf[:, a:a + 8], data1=zrow[:, 0:8], initial=0.0, op0=ALU.add, op1=ALU.add), reads=["lf", "zrow", "brow"], writes=["brow"])
                for i in range(16):
                    a = 128 * i
                    P.op(dv, lambda e, a=a: e.tensor_tensor_scan(out=brow[:, a:a + 128], data0=lf[:, a:a + 128], data1=zrow[:], initial=0.0, op0=ALU.add, op1=ALU.add), reads=["lf", "zrow", "brow"], writes=["brow"])
                P.op(dv, lambda e: e.tensor_copy(out=mprev[:, 1:8], in_=mrow[:, 127:127 + 7 * 128:128]), reads=["mrow", "mprev"], writes=["mprev"])
                P.op(dv, lambda e: e.tensor_copy(out=mprev[:, 9:16], in_=mrow[:, NPRE + 127:NPRE + 127 + 7 * 128:128]), reads=["mrow", "mprev"], writes=["mprev"])
                if maybe_stop("B3"):
                    return nc
                P.dma("sp", lambda e: e.dma_start(out=mp_d, in_=mrow[:, 2047:2048]), reads=["mrow"], is_output=True)
                with nc.allow_non_contiguous_dma(reason="tiny m out"):
                    P.dma("sp", lambda e: e.dma_start(out=ms_d.rearrange("j h -> h j"), in_=mrow[:, 2048 + 7:NT:8], allow_slow_non_contiguous=True), reads=["mrow"], is_output=True)
                P.op(dv, lambda e: e.tensor_tensor(out=arow[:], in0=brow[:], in1=mrow[:], op=ALU.subtract), reads=["brow", "mrow"], writes=["arow"])
                if maybe_stop("B4"):
                    return nc
                P.op(dv, lambda e: e.tensor_tensor(out=beta[:], in0=ig[:], in1=brow[:], op=ALU.subtract), reads=["ig", "brow"], writes=["beta"])
                P.op(dv, lambda e: e.tensor_copy(out=alpha[:], in_=arow[:, NPRE:NT]), reads=["arow"], writes=["alpha"])
                emneg, winter, wl = brow, lf, ig
                P.op(ac, lambda e: e.activation(out=emneg[:], in_=mrow[:], func=AF.Exp, scale=-1.0), reads=["mrow", "brow", "beta", "arow"], writes=["brow"])
                for i in range(16):
                    a = 128 * i
                    P.op(ac, lambda e, a=a, i=i: e.activation(out=winter[:, a:a + 128], in_=arow[:, a:a + 128], func=AF.Exp, bias=mprev[:, i:i + 1], scale=1.0), reads=["arow", "mprev", "lf", "mrow"], writes=["lf"])
                    P.op(ac, lambda e, a=a: e.activation(out=wl[:, a:a + 128], in_=beta[:, a:a + 128], func=AF.Exp, bias=arow[:, a + 127:a + 128], scale=1.0), reads=["beta", "arow", "ig"], writes=["ig"])
                for j in range(16):
                    a = 2048 + 8 * j
                    P.op(ac, lambda e, a=a, j=j: e.activation(out=winter[:, a:a + 8], in_=arow[:, a:a + 8], func=AF.Exp, bias=smT[:, j:j + 1], scale=1.0), reads=["arow", "smT", "lf"], writes=["lf"])
                    P.op(ac, lambda e, a=a: e.activation(out=wl[:, a:a + 8], in_=beta[:, a:a + 8], func=AF.Exp, bias=arow[:, a + 7:a + 8], scale=1.0), reads=["beta", "arow", "ig"], writes=["ig"])
                P.op(dv, lambda e: e.tensor_tensor(out=tmp4[:], in0=arow[:, 127:2048:128], in1=mprev[:], op=ALU.add), reads=["arow", "mprev"], writes=["tmp4"])
                P.op(ac, lambda e: e.activation(out=WLI[:, 0:16], in_=tmp4[:], func=AF.Exp), reads=["tmp4"], writes=["WLI"])
                P.op(dv, lambda e: e.tensor_tensor(out=tmp4[:], in0=arow[:, 2048 + 7:NT:8], in1=smT[:], op=ALU.add), reads=["arow", "smT", "tmp4", "WLI"], writes=["tmp4"])
                P.op(ac, lambda e: e.activation(out=WLI[:, 16:32], in_=tmp4[:], func=AF.Exp), reads=["tmp4", "WLI"], writes=["WLI"])
                if maybe_stop("B5"):
                    return nc
                for i in range(TPRE + TOWN):
                    a = 128 * i
                    for qi, (rowt, rk) in enumerate([(beta, "beta"), (winter, "lf"), (emneg, "brow"), (wl, "ig"), (arow, "arow")]):
                        P.op(pe, lambda e, rowt=rowt, a=a, qi=qi: e.transpose(out=pss[:, 256 + 4 * qi:256 + 4 * qi + 4], in_=rowt[:, a:a + 128], identity=identf[0:4, 0:4]), reads=[rk, "identf"], writes=["pssq"])
                    P.op(dv, lambda e, i=i: e.tensor_copy(out=colq[:, i, :], in_=pss[:, 256:276]), reads=["pssq"], writes=[("colq", i)])
                    if i == 0 and maybe_stop("B5a"):
                        return nc
                    if i == 7 and maybe_stop("B5b"):
                        return nc
                    if i == 16 and maybe_stop("B5c"):
                        return nc
                rowc_r = Ring(stB, nc, "rowc", [4, 128], F32, 2, bump=bump)
                for i in range(32):
                    rowc, rck = rowc_r.next()
                    P.op(ac, lambda e, rowc=rowc, i=i: e.activation(out=rowc[:], in_=zrow[:], func=AF.Identity, bias=WLI[:, i:i + 1], scale=0.0), reads=["WLI", "zrow"], writes=[rck])
                    P.op(pe, lambda e, rowc=rowc, i=i: e.transpose(out=pss[:, 288 + 4 * i:288 + 4 * i + 4], in_=rowc[:], identity=identf[0:4, 0:4]), reads=[rck], writes=["pssw"])
                P.op(dv, lambda e: e.tensor_copy(out=wliS[:].rearrange("p i h -> p (i h)"), in_=pss[:, 288:416]), reads=["pssw"], writes=["wliS"])
                if maybe_stop("B6"):
                    return nc
                wuB = [view(Y0 + 36864 + k * 4096, [128, KC, 128], BF16) for k in range(2)]
                for fc in range(8):
                    wu, wk = wuB[fc % 2], ("wuB", fc % 2)
                    wload(wu, wk, kchunks(w_in[:, U0 + fc * 128:U0 + (fc + 1) * 128]), bar=("B" not in NOBAR))
                    pu, puk = PSB.next()
                    for c in range(KC):
                        P.op(pe, lambda e, c=c, wu=wu, pu=pu: e.matmul(pu[:, 0:16], lhsT=wu[:, c, :], rhs=hNTp[:, c, NPRE - 16:NPRE], start=(c == 0), stop=(c == KC - 1)), reads=[wk], writes=[puk])
                    P.op(ac, lambda e, fc=fc, pu=pu: e.activation(out=uhist[:, fc, :], in_=pu[:, 0:16], func=AF.Copy), reads=[puk], writes=[("uhist", fc)])
                if "rows" in debug:
                    for nm, t in [("ig_wl", ig), ("lf_winter", lf), ("mrow", mrow), ("b_emneg", brow), ("arow", arow), ("beta", beta)]:
                        dbg_ap = t[:]
                        d = dout("dbg_" + nm, [4, NT])
                        P.barrier_all()
                        P.dma("sp", lambda e, d=d, dbg_ap=dbg_ap: e.dma_start(out=d, in_=dbg_ap), is_output=True)
                P.barrier_all()
                if maybe_stop("B"):
                    return nc
            dbg("hNTo", hNTo, [128, KC, NOWN])
            dbg("colq", colq[:], [128, TPRE + TOWN, 20])
            with Scope(bump) as stC0:
                WkC0 = [view(Y0 + 36864 + k * 16384, [128, KC, HD], BF16) for k in range(2)]
                Wv_r = Ring(stC0, nc, "WvC0_", [128, KC, HD], BF16, 2, bump=bump)
                pad0 = sb("pad0", [128, 1024], F32, stC0)
                Cst = sb("Cst0", [128, 4, HD], F32, stC0); nst = sb("nst0", [128, 4], F32, stC0)
                kt_r = Ring(stC0, nc, "ktC0_", [128, HD], BF16, 2, bump=bump); v_r = Ring(stC0, nc, "vC0_", [128, HD], BF16, 2, bump=bump)
                wlib = sb("wlib0", [128, 2], F32, stC0)

                WvA = view(A0, [128, KC, HD], BF16)
                for h in range(H):
                    Wk, wkk = WkC0[1], ("WkC0", 1)
                    Wv, wvk = WvA, "WvA"
                    wload(Wk, wkk, kchunks(w_in[:, K0 + h * HD:K0 + (h + 1) * HD]), bar=("C0" not in NOBAR))
                    wload(Wv, wvk, kchunks(w_in[:, V0 + h * HD:V0 + (h + 1) * HD]), bar=("C0" not in NOBAR))
                    P.op(dv, lambda e: e.memset(Cst[:], 0.0), reads=["Cst"], writes=["Cst"])
                    P.op(dv, lambda e: e.memset(nst[:], 0.0), reads=["nst"], writes=["nst"])
                    for i in range(TPRE):
                        pk_, pkk = PSB.next(); pv_, pvk = PSB.next()
                        for c in range(KC):
                            P.op(pe, lambda e, c=c, pk_=pk_, i=i, Wk=Wk: e.matmul(pk_[:], lhsT=hNTp[:, c, i * 128:(i + 1) * 128], rhs=Wk[:, c, :], start=(c == 0), stop=(c == KC - 1)), reads=[wkk], writes=[pkk])
                        for c in range(KC):
                            P.op(pe, lambda e, c=c, pv_=pv_, i=i, Wv=Wv: e.matmul(pv_[:], lhsT=hNTp[:, c, i * 128:(i + 1) * 128], rhs=Wv[:, c, :], start=(c == 0), stop=(c == KC - 1)), reads=[wvk], writes=[pvk])
                        kt, ktk = kt_r.next(); vt, vk = v_r.next()
                        P.op(dv, lambda e, kt=kt, pk_=pk_, i=i, h=h: e.tensor_scalar(out=kt[:], in0=pk_[:], scalar1=colq[:, i, 12 + h:13 + h], scalar2=KSCALE, op0=ALU.mult, op1=ALU.mult), reads=[pkk], writes=[ktk])
                        P.op(ac, lambda e, vt=vt, pv_=pv_: e.activation(out=vt[:], in_=pv_[:], func=AF.Copy), reads=[pvk], writes=[vk])
                        if h == 0 and i == 0:
                            dbg("c0v", vt[:], [128, HD], cast=True)
                            dbg("c0kt", kt[:], [128, HD], cast=True)
                            dbg("c0wv", Wv[:, 0, :], [128, HD], cast=True)
                            dbg("c0wv15", Wv[:, 15, :], [128, HD], cast=True)
                            dbg("c0hn", hNTp[:, 0, 0:128], [128, 128], cast=True)
                        for c2 in range(4):
                            pkv, pkvk = PSB.next()
                            P.op(pe, lambda e, c2=c2, pkv=pkv, kt=kt, vt=vt: e.matmul(pkv[:], lhsT=kt[:, c2 * 128:(c2 + 1) * 128], rhs=vt[:], start=True, stop=True), reads=[ktk, vk], writes=[pkvk])
                            P.op(dv, lambda e, c2=c2, pkv=pkv, h=h, i=i: e.scalar_tensor_tensor(out=Cst[:, c2, :], in0=Cst[:, c2, :], scalar=wliS[:, i, h:h + 1], in1=pkv[:], op0=ALU.mult, op1=ALU.add), reads=["Cst", pkvk], writes=["Cst"])
                        for c2 in range(4):
                            P.op(pe, lambda e, c2=c2, kt=kt: e.matmul(pss[:, 8 + c2:9 + c2], lhsT=kt[:, c2 * 128:(c2 + 1) * 128], rhs=ones_b[:], start=True, stop=True), reads=[ktk], writes=["pss1"])
                        P.op(dv, lambda e, h=h, i=i: e.scalar_tensor_tensor(out=nst[:], in0=nst[:], scalar=wliS[:, i, h:h + 1], in1=pss[:, 8:12], op0=ALU.mult, op1=ALU.add), reads=["nst", "pss1"], writes=["nst"])
                        P.barrier_all()
                        if h == 0 and i == 0:
                            dbg("c0cst", Cst[:], [128, 4, HD])
                        if h == 1 and i == 0:
                            dbg("wvA", Wv, [128, KC, HD], cast=True)
                        if h == 1 and i == 7:
                            dbg("wvB", Wv, [128, KC, HD], cast=True)
                            dbg("c0cst7b", Cst[:], [128, 4, HD])
                        if h == 0 and i == 7:
                            dbg("c0cst7", Cst[:], [128, 4, HD])
                            dbg("c0v7", vt[:], [128, HD], cast=True)
                            dbg("c0kt7", kt[:], [128, HD], cast=True)
                        if h == 0 and i == 3:
                            dbg("c0cst3", Cst[:], [128, 4, HD])
                    P.op(dv, lambda e: e.tensor_scalar(out=Cst[:], in0=Cst[:], scalar1=flag[:, 0:1], scalar2=None, op0=ALU.mult), reads=["Cst"], writes=["Cst"])
                    P.op(dv, lambda e: e.tensor_scalar(out=nst[:], in0=nst[:], scalar1=flag[:, 0:1], scalar2=None, op0=ALU.mult), reads=["nst"], writes=["nst"])
                    if h == 0:
                        dbg("c0cstF", Cst[:], [128, 4, HD])
                        dbg("wliS", wliS[:], [128, 32, 4])
                        dbg("flag", flag[:], [128, 1])
                    P.dma("sp", lambda e, h=h: e.dma_start(out=cpre_d[h], in_=Cst[:].rearrange("p c v -> p (c v)")), reads=["Cst"], writes=[("cpre", h)])
                    P.dma("sp", lambda e, h=h: e.dma_start(out=npre_d[h], in_=nst[:]), reads=["nst"], writes=[("npre", h)])
                P.barrier_all()
                if maybe_stop("C0"):
                    return nc
            with Scope(bump) as stC:
                Wq = view(M0, [128, KC, HD], BF16); Wk = view(M0 + 16384, [128, KC, HD], BF16); Wv = view(M0 + 32768, [128, KC, HD], BF16)
                Cst = sb("Cst", [128, 4, HD], F32, stC); Cb = sb("Cb", [128, 4, HD], BF16, stC)
                nst = sb("nst", [128, 4], F32, stC); nb = sb("nb", [128, 4], BF16, stC)
                ghb = sb("ghb", [128, HD], F32, stC)
                s_qT = sb("s_qT", [128, H, 4, 128], BF16, stC); s_kt = sb("s_kt", [128, H, HD], BF16, stC)
                s_v = sb("s_v", [128, H, HD], BF16, stC); s_num = sb("s_num", [128, H, HD], F32, stC); s_den = sb("s_den", [128, H], F32, stC)
                qT_r = Ring(stC, nc, "qT", [128, 4, 128], BF16, 2, bump=bump); kT_r = Ring(stC, nc, "kT", [128, 4, 128], BF16, 2, bump=bump)
                kt_r = Ring(stC, nc, "kt", [128, HD], BF16, 2, bump=bump); v_r = Ring(stC, nc, "vt", [128, HD], BF16, 2, bump=bump)
                Mt_r = Ring(stC, nc, "Mt", [128, 128], F32, 2, bump=bump)
                Wt_r = Ring(stC, nc, "Wt", [128, 128], F32, 2, bump=bump); PT_r = Ring(stC, nc, "PT", [128, 128], BF16, 2, bump=bump)
                num_r = Ring(stC, nc, "num", [128, HD], F32, 1, bump=bump); tmpn_r = Ring(stC, nc, "tmpn", [128, HD], F32, 1, bump=bump)
                hbt_r = Ring(stC, nc, "hbt", [128, HD], BF16, 2, bump=bump)
                sm_r = Ring(stC, nc, "smalls", [128, 16], F32, 2, bump=bump)
                junkC = sb("junkC", [128, HD], BF16, stC)

                def finish_tile(h, i, num_ap, numk, den_ap, small, smk):
                    P.op(dv, lambda e: e.tensor_scalar(out=small[:, 4:5], in0=den_ap, scalar1=-1.0, scalar2=None, op0=ALU.mult), reads=[smk], writes=[smk])
                    P.op(dv, lambda e: e.tensor_tensor(out=small[:, 4:5], in0=small[:, 4:5], in1=den_ap, op=ALU.max), reads=[smk], writes=[smk])
                    P.op(dv, lambda e: e.tensor_tensor(out=small[:, 4:5], in0=small[:, 4:5], in1=colq[:, TPRE + i, 8 + h:9 + h], op=ALU.max), reads=[smk], writes=[smk])
                    P.op(dv, lambda e: e.reciprocal(out=small[:, 5:6], in_=small[:, 4:5]), reads=[smk], writes=[smk])
                    P.op(ac, lambda e: e.activation(out=junkC[:], in_=num_ap, func=AF.Square, accum_out=small[:, 6:7]), reads=[numk, smk], writes=["junkC", smk])
                    P.op(dv, lambda e: e.tensor_scalar(out=small[:, 7:8], in0=small[:, 6:7], scalar1=small[:, 5:6], scalar2=small[:, 5:6], op0=ALU.mult, op1=ALU.mult), reads=[smk], writes=[smk])
                    P.op(ac, lambda e: e.activation(out=small[:, 8:9], in_=small[:, 7:8], func=AF.Sqrt, scale=1.0 / HD, bias=EPS), reads=[smk], writes=[smk])
                    P.op(dv, lambda e: e.reciprocal(out=small[:, 9:10], in_=small[:, 8:9]), reads=[smk], writes=[smk])
                    P.op(dv, lambda e: e.tensor_tensor(out=small[:, 10:11], in0=small[:, 9:10], in1=small[:, 5:6], op=ALU.mult), reads=[smk], writes=[smk])
                    hbt, hbk = hbt_r.next()
                    P.op(dv, lambda e: e.scalar_tensor_tensor(out=hbt[:], in0=num_ap, scalar=small[:, 10:11], in1=ghb[:], op0=ALU.mult, op1=ALU.mult), reads=[numk, smk, "ghb"], writes=[hbk])
                    pt, pk = PST.next()
                    for c in range(4):
                        P.op(pe, lambda e, c=c, pt=pt: e.transpose(out=pt[:, c * 128:(c + 1) * 128], in_=hbt[:, c * 128:(c + 1) * 128], identity=identb[:]), reads=[hbk], writes=[pk])
                    P.op(ac, lambda e, pt=pt: e.activation(out=hbT[:, 4 * h:4 * h + 4, i * 128:(i + 1) * 128], in_=pt[:, 0:512].rearrange("p (c t) -> p c t", c=4), func=AF.Copy), reads=[pk], writes=[("hbT", h, i)])

                for h in range(H):
                    wload(Wq, "Wq", kchunks(w_in[:, Q0 + h * HD:Q0 + (h + 1) * HD]), bar=("C1" not in NOBAR))
                    wload(Wk, "Wk", kchunks(w_in[:, K0 + h * HD:K0 + (h + 1) * HD]), bar=("C1" not in NOBAR))
                    wload(Wv, "Wv", kchunks(w_in[:, V0 + h * HD:V0 + (h + 1) * HD]), bar=("C1" not in NOBAR))
                    P.dma("sp", lambda e, h=h: e.dma_start(out=ghb[:], in_=g_head[h * HD:(h + 1) * HD].partition_broadcast(128)), writes=["ghb"])
                    P.dma("sp", lambda e, h=h: e.dma_start(out=Cst[:].rearrange("p c v -> p (c v)"), in_=cpre_d[h]), writes=["Cst"])
                    P.dma("sp", lambda e, h=h: e.dma_start(out=nst[:], in_=npre_d[h]), writes=["nst"])
                    P.op(ac, lambda e: e.activation(out=Cb[:], in_=Cst[:], func=AF.Copy), reads=["Cst"], writes=["Cb"])
                    P.op(ac, lambda e: e.activation(out=nb[:], in_=nst[:], func=AF.Copy), reads=["nst"], writes=["nb"])
                    if h == 0:
                        dbg("cst0", Cst[:], [128, 4, HD])
                    for i in range(TOWN):
                        samp = (i == TOWN - 1)
                        ci = TPRE + i
                        if samp:
                            qT, qk = s_qT[:, h], ("s_qT", h)
                            ktl, ktk = s_kt[:, h], ("s_kt", h)
                            vt, vk = s_v[:, h], ("s_v", h)
                        else:
                            t_, qk = qT_r.next(); qT = t_[:]
                            t_, ktk = kt_r.next(); ktl = t_[:]
                            t_, vk = v_r.next(); vt = t_[:]
                        t_, kTk = kT_r.next(); kT = t_[:]
                        pq, pqk = PSB.next()
                        for cc in range(4):
                            for c in range(KC):
                                P.op(pe, lambda e, c=c, cc=cc, pq=pq, i=i: e.matmul(pq[:, cc * 128:(cc + 1) * 128], lhsT=Wq[:, c, cc * 128:(cc + 1) * 128], rhs=hNTo[:, c, i * 128:(i + 1) * 128], start=(c == 0), stop=(c == KC - 1)), reads=["Wq"], writes=[pqk])
                        P.op(ac, lambda e, pq=pq, qT=qT: e.activation(out=qT.rearrange("p c t -> p (c t)"), in_=pq[:], func=AF.Copy), reads=[pqk], writes=[qk])
                        pkT, pkTk = PSB.next()
                        for cc in range(4):
                            for c in range(KC):
                                P.op(pe, lambda e, c=c, cc=cc, pkT=pkT, i=i: e.matmul(pkT[:, cc * 128:(cc + 1) * 128], lhsT=Wk[:, c, cc * 128:(cc + 1) * 128], rhs=hNTo[:, c, i * 128:(i + 1) * 128], start=(c == 0), stop=(c == KC - 1)), reads=["Wk"], writes=[pkTk])
                        P.op(dv, lambda e, pkT=pkT, kT=kT: e.tensor_scalar(out=kT.rearrange("p c t -> p (c t)"), in0=pkT[:], scalar1=KSCALE, scalar2=None, op0=ALU.mult), reads=[pkTk], writes=[kTk])
                        pk_, pkk = PSB.next()
                        for c in range(KC):
                            P.op(pe, lambda e, c=c, pk_=pk_, i=i: e.matmul(pk_[:], lhsT=hNTo[:, c, i * 128:(i + 1) * 128], rhs=Wk[:, c, :], start=(c == 0), stop=(c == KC - 1)), reads=["Wk"], writes=[pkk])
                        P.op(dv, lambda e, pk_=pk_, ktl=ktl, ci=ci, h=h: e.tensor_scalar(out=ktl, in0=pk_[:], scalar1=colq[:, ci, 12 + h:13 + h], scalar2=KSCALE, op0=ALU.mult, op1=ALU.mult), reads=[pkk], writes=[ktk])
                        pv_, pvk = PSB.next()
                        for c in range(KC):
                            P.op(pe, lambda e, c=c, pv_=pv_, i=i: e.matmul(pv_[:], lhsT=hNTo[:, c, i * 128:(i + 1) * 128], rhs=Wv[:, c, :], start=(c == 0), stop=(c == KC - 1)), reads=["Wv"], writes=[pvk])
                        P.op(ac, lambda e, pv_=pv_, vt=vt: e.activation(out=vt, in_=pv_[:], func=AF.Copy), reads=[pvk], writes=[vk])
                        if h == 0 and i == 0:
                            dbg("v0", vt, [128, HD], cast=True)
                            dbg("kt0", ktl, [128, HD], cast=True)
                            dbg("wv", Wv[:, 0, :], [128, HD], cast=True)
                            dbg("qT0", qT, [128, 4, 128], cast=True)
                        pS, pSk = PSB.next()
                        for c in range(4):
                            P.op(pe, lambda e, c=c, pS=pS, kT=kT, qT=qT: e.matmul(pS[:, 0:128], lhsT=kT[:, c, :], rhs=qT[:, c, :], start=(c == 0), stop=(c == 3)), reads=[kTk, qk], writes=[pSk])
                        Mt, Mtk = Mt_r.next()
                        P.op(ac, lambda e, Mt=Mt, ci=ci, h=h: e.activation(out=Mt[:], in_=maskP[:], func=AF.Identity, bias=colq[:, ci, 16 + h:17 + h], scale=0.0), reads=[], writes=[Mtk])
                        P.op(pe, lambda e, pS=pS, Mt=Mt: e.transpose(out=pS[:, 128:256], in_=Mt[:], identity=identf[:]), reads=[Mtk], writes=[pSk])
                        Wt, Wtk = Wt_r.next(); PT, PTk = PT_r.next()
                        msk = maskS if samp else maskP
                        P.op(dv, lambda e, Wt=Wt, pS=pS, msk=msk: e.tensor_tensor(out=Wt[:], in0=pS[:, 128:256], in1=msk[:], op=ALU.add), reads=[pSk], writes=[Wtk])
                        P.op(ac, lambda e, Wt=Wt, ci=ci, h=h: e.activation(out=Wt[:], in_=Wt[:], func=AF.Exp, bias=colq[:, ci, h:h + 1], scale=1.0), reads=[Wtk], writes=[Wtk])
                        P.op(dv, lambda e, Wt=Wt, PT=PT, pS=pS: e.tensor_tensor(out=PT[:], in0=pS[:, 0:128], in1=Wt[:], op=ALU.mult), reads=[pSk, Wtk], writes=[PTk])
                        pn, pnk = PSB.next()
                        P.op(pe, lambda e, pn=pn, PT=PT, vt=vt: e.matmul(pn[:], lhsT=PT[:], rhs=vt, start=True, stop=True), reads=[PTk, vk], writes=[pnk])
                        P.op(pe, lambda e, PT=PT: e.matmul(pss[:, 16:17], lhsT=PT[:], rhs=ones_b[:], start=True, stop=True), reads=[PTk], writes=["pssd"])
                        if samp:
                            P.op(ac, lambda e, pn=pn, h=h: e.activation(out=s_num[:, h, :], in_=pn[:], func=AF.Copy), reads=[pnk], writes=[("s_num", h)])
                            P.op(ac, lambda e, h=h: e.activation(out=s_den[:, h:h + 1], in_=pss[:, 16:17], func=AF.Copy), reads=["pssd"], writes=[("s_den", h)])
                            continue
                        pi_, pik = PSB.next()
                        for c in range(4):
                            P.op(pe, lambda e, c=c, pi_=pi_, qT=qT: e.matmul(pi_[:], lhsT=qT[:, c, :], rhs=Cb[:, c, :], start=(c == 0), stop=(c == 3)), reads=[qk, "Cb"], writes=[pik])
                        for c in range(4):
                            P.op(pe, lambda e, c=c, qT=qT: e.matmul(pss[:, 17:18], lhsT=qT[:, c, :], rhs=nb[:, c:c + 1], start=(c == 0), stop=(c == 3)), reads=[qk, "nb"], writes=["pssd"])
                        small, smk = sm_r.next()
                        P.op(ac, lambda e, small=small: e.activation(out=small[:, 0:2], in_=pss[:, 16:18], func=AF.Copy), reads=["pssd"], writes=[smk])
                        tmpn, tmpk = tmpn_r.next(); num, numk = num_r.next()
                        P.op(ac, lambda e, tmpn=tmpn, pi_=pi_, ci=ci, h=h: e.activation(out=tmpn[:], in_=pi_[:], func=AF.Copy, scale=colq[:, ci, 4 + h:5 + h]), reads=[pik], writes=[tmpk])
                        P.op(dv, lambda e, num=num, tmpn=tmpn, pn=pn: e.tensor_tensor(out=num[:], in0=tmpn[:], in1=pn[:], op=ALU.add), reads=[tmpk, pnk], writes=[numk])
                        P.op(dv, lambda e, small=small, ci=ci, h=h: e.scalar_tensor_tensor(out=small[:, 3:4], in0=small[:, 1:2], scalar=colq[:, ci, 4 + h:5 + h], in1=small[:, 0:1], op0=ALU.mult, op1=ALU.add), reads=[smk], writes=[smk])
                        finish_tile(h, i, num[:], numk, small[:, 3:4], small, smk)
                        if h == 0 and i == 0:
                            dbg("num0", num[:], [128, HD])
                            dbg("small0", small[:], [128, 16])
                            dbg("Wt0", Wt[:], [128, 128])
                        for c2 in range(4):
                            pkv, pkvk = PSB.next()
                            P.op(pe, lambda e, c2=c2, pkv=pkv, ktl=ktl, vt=vt: e.matmul(pkv[:], lhsT=ktl[:, c2 * 128:(c2 + 1) * 128], rhs=vt, start=True, stop=True), reads=[ktk, vk], writes=[pkvk])
                            P.op(dv, lambda e, c2=c2, pkv=pkv, h=h, i=i: e.scalar_tensor_tensor(out=Cst[:, c2, :], in0=Cst[:, c2, :], scalar=wliS[:, 8 + i, h:h + 1], in1=pkv[:], op0=ALU.mult, op1=ALU.add), reads=["Cst", pkvk], writes=["Cst"])
                        for c2 in range(4):
                            P.op(pe, lambda e, c2=c2, ktl=ktl: e.matmul(pss[:, 24 + c2:25 + c2], lhsT=ktl[:, c2 * 128:(c2 + 1) * 128], rhs=ones_b[:], start=True, stop=True), reads=[ktk], writes=["pssn"])
                        P.op(dv, lambda e, h=h, i=i: e.scalar_tensor_tensor(out=nst[:], in0=nst[:], scalar=wliS[:, 8 + i, h:h + 1], in1=pss[:, 24:28], op0=ALU.mult, op1=ALU.add), reads=["nst", "pssn"], writes=["nst"])
                        if i < TOWN - 2:
                            P.op(ac, lambda e: e.activation(out=Cb[:], in_=Cst[:], func=AF.Copy), reads=["Cst"], writes=["Cb"])
                            P.op(ac, lambda e: e.activation(out=nb[:], in_=nst[:], func=AF.Copy), reads=["nst"], writes=["nb"])
                    P.dma("sp", lambda e, h=h: e.dma_start(out=Cp_d[h].rearrange("(c p) v -> p c v", p=128), in_=Cst[:]), reads=["Cst"], is_output=True)
                    with nc.allow_non_contiguous_dma(reason="n out"):
                        P.dma("sp", lambda e, h=h: e.dma_start(out=np_d[h].rearrange("(c p) -> p c", p=128), in_=nst[:], allow_slow_non_contiguous=True), reads=["nst"], is_output=True)
                P.barrier_all()
                if maybe_stop("C1"):
                    return nc

                Cj_v = [view(M0 + k * 8192, [128, 4, HD], F32) for k in range(3)]
                Z_v = [view(M0 + 24576 + k * 3968, [128, 4, 248], F32) for k in range(2)]
                o2 = M0 + 24576 + 2 * 3968
                ktj_v = [view(o2 + k * 1024, [128, HD], BF16) for k in range(2)]
                n0 = view(o2 + 2048, [128, HD], F32)
                nout = view(o2 + 4096, [128, HD], F32)
                n0T = view(o2 + 6144, [128, 4, 64], F32)
                nnT = view(o2 + 7168, [128, 4, 64], F32)
                P.dma("sp", lambda e: e.dma_start(out=n0[0:64, :], in_=sn), writes=["n0"])
                for c in range(4):
                    P.op(pe, lambda e, c=c: e.transpose(out=pss[:, 64 * c:64 * c + 64], in_=n0[0:64, c * 128:(c + 1) * 128], identity=identf[0:64, 0:64]), reads=["n0"], writes=["pss"])
                P.op(dv, lambda e: e.tensor_copy(out=n0T.rearrange("p c j -> p (c j)"), in_=pss[:, 0:256]), reads=["pss"], writes=["n0T"])
                for k in range(2):
                    P.op(dv, lambda e, k=k: e.memset(Z_v[k], 0.0), writes=[("Z", k)])
                ci = TPRE + TOWN - 1
                seq = [(h, j) for h in range(H) for j in range(16)]

                def c2_load(idx):
                    h, j = seq[idx]
                    P.dma("sp", lambda e: e.dma_start(out=Cj_v[idx % 3], in_=sC[j, h].rearrange("(c p) v -> p c v", p=128)), writes=[("Cj", idx % 3)])
                c2_load(0); c2_load(1)
                for idx, (h, j) in enumerate(seq):
                    if idx + 2 < len(seq):
                        c2_load(idx + 2)
                    Cj, Cjk = Cj_v[idx % 3], ("Cj", idx % 3)
                    Z, Zk = Z_v[idx % 2], ("Z", idx % 2)
                    P.op(dv, lambda e, Z=Z, j=j, h=h: e.tensor_copy(out=Z[:, :, 120:128], in_=s_qT[:, h, :, 8 * j:8 * j + 8]), reads=[Zk], writes=[Zk])
                    for c in range(4):
                        P.op(pe, lambda e, c=c, Z=Z, Cj=Cj, j=j: e.matmul(pacc[:], lhsT=Z[:, c, 120 - 8 * j:248 - 8 * j], rhs=Cj[:, c, :], start=(j == 0 and c == 0), stop=(j == 15 and c == 3)), reads=[Zk, Cjk], writes=["pacc"])
                    for c in range(4):
                        P.op(pe, lambda e, c=c, Z=Z, j=j, h=h: e.matmul(pss[:, 320 + j:321 + j], lhsT=Z[:, c, 120 - 8 * j:248 - 8 * j], rhs=n0T[:, c, 4 * j + h:4 * j + h + 1], start=(c == 0), stop=(c == 3)), reads=[Zk, "n0T"], writes=["pssd2"])
                    ktj, ktjk = ktj_v[idx % 2], ("ktj", idx % 2)
                    P.op(dv, lambda e, ktj=ktj, j=j, h=h: e.tensor_scalar(out=ktj, in0=s_kt[:, h, :], scalar1=blk[:, j:j + 1], scalar2=None, op0=ALU.mult), reads=[ktjk], writes=[ktjk])
                    for c2 in range(4):
                        pkv, pkvk = PSB.next()
                        P.op(pe, lambda e, c2=c2, pkv=pkv, ktj=ktj, h=h: e.matmul(pkv[:], lhsT=ktj[:, c2 * 128:(c2 + 1) * 128], rhs=s_v[:, h, :], start=True, stop=True), reads=[ktjk], writes=[pkvk])
                        P.op(dv, lambda e, c2=c2, pkv=pkv, Cj=Cj, j=j, h=h: e.scalar_tensor_tensor(out=Cj[:, c2, :], in0=Cj[:, c2, :], scalar=wliS[:, 16 + j, h:h + 1], in1=pkv[:], op0=ALU.mult, op1=ALU.add), reads=[Cjk, pkvk], writes=[Cjk])
                    for c2 in range(4):
                        P.op(pe, lambda e, c2=c2, ktj=ktj: e.matmul(pss[:, 304 + c2:305 + c2], lhsT=ktj[:, c2 * 128:(c2 + 1) * 128], rhs=ones_b[:], start=True, stop=True), reads=[ktjk], writes=["pssn2"])
                    P.op(dv, lambda e, j=j, h=h: e.scalar_tensor_tensor(out=nnT[:, :, 4 * j + h], in0=n0T[:, :, 4 * j + h], scalar=wliS[:, 16 + j, h:h + 1], in1=pss[:, 304:308], op0=ALU.mult, op1=ALU.add), reads=["n0T", "pssn2", "nnT"], writes=["nnT"])
                    P.dma("pool", lambda e, Cj=Cj, j=j, h=h: e.dma_start(out=Cs_d[j, h].rearrange("(c p) v -> p c v", p=128), in_=Cj), reads=[Cjk], is_output=True)
                    if j == 15:
                        small, smk = sm_r.next()
                        P.op(dv, lambda e, small=small: e.tensor_reduce(out=small[:, 1:2], in_=pss[:, 320:336], axis=AX.X, op=ALU.add), reads=["pssd2"], writes=[smk])
                        tmpn, tmpk = tmpn_r.next(); num, numk = num_r.next()
                        P.op(ac, lambda e, tmpn=tmpn, h=h: e.activation(out=tmpn[:], in_=pacc[:], func=AF.Copy, scale=colq[:, ci, 4 + h:5 + h]), reads=["pacc"], writes=[tmpk])
                        P.op(dv, lambda e, num=num, tmpn=tmpn, h=h: e.tensor_tensor(out=num[:], in0=tmpn[:], in1=s_num[:, h, :], op=ALU.add), reads=[tmpk], writes=[numk])
                        P.op(dv, lambda e, small=small, h=h: e.scalar_tensor_tensor(out=small[:, 3:4], in0=small[:, 1:2], scalar=colq[:, ci, 4 + h:5 + h], in1=s_den[:, h:h + 1], op0=ALU.mult, op1=ALU.add), reads=[smk], writes=[smk])
                        P.dma("sp", lambda e, h=h: e.dma_start(out=ghb[:], in_=g_head[h * HD:(h + 1) * HD].partition_broadcast(128)), writes=["ghb"])
                        finish_tile(h, TOWN - 1, num[:], numk, small[:, 3:4], small, smk)
                for c in range(4):
                    P.op(pe, lambda e, c=c: e.transpose(out=pss[0:64, 128 * c:128 * c + 128], in_=nnT[:, c, :], identity=identf[:]), reads=["nnT"], writes=["pss", "pssd2", "pssn2"])
                P.op(dv, lambda e: e.tensor_copy(out=nout[0:64, :], in_=pss[0:64, :]), reads=["pss"], writes=["nout"])
                P.dma("sp", lambda e: e.dma_start(out=ns_d, in_=nout[0:64, :]), reads=["nout"], is_output=True)
                P.barrier_all()
                if maybe_stop("C2"):
                    return nc
            dbg("hbT", hbT, [128, KC, NOWN])
            with Scope(bump) as stP:
                pooledT = view(M0, [128, 8, NOWN], BF16)
                hist = view(M0 + 18432, [128, 8, 240], F32)
                utok = view(M0 + 26112, [128, PW], F32); utoks = view(M0 + 30208, [128, PW], F32)
                hld = Ring(stP, nc, "hld", [120, PW], F32, 1, bump=bump)
                full = sb("full", [128, 1040], F32, stP); wsA = sb("wsA", [128, 1040], F32, stP); wsB = sb("wsB", [128, 1040], F32, stP)
                fulls = sb("fulls", [128, 16, 23], F32, stP); wsAs = sb("wsAs", [128, 16, 23], F32, stP); wsBs = sb("wsBs", [128, 16, 23], F32, stP)
                tmp16 = sb("tmp16", [128, 16], F32, stP)
                pscale = sb("pscale", [128, 8], F32, stP)
                wu_ring = Ring(stP, nc, "wuP", [128, KC, 128], BF16, 2, bump=bump)
                wut_ring = Ring(stP, nc, "wutP", [128, KC, 512], BF16, 1, bump=bump)
                wp_ring = Ring(stP, nc, "wpP", [128, 2, 256], BF16, 2, bump=bump)
                with nc.allow_non_contiguous_dma(reason="pool scale"):
                    P.dma("sp", lambda e: e.dma_start(out=pscale[:], in_=pool_scale.rearrange("(c p) -> p c", p=128), allow_slow_non_contiguous=True), writes=["pscale"])
                for half in range(2):
                    hl, hlk = hld.next()
                    P.dma("sp", lambda e, hl=hl, half=half: e.dma_start(out=hl[:], in_=spool[8 * half:8 * half + 8].rearrange("j r d -> (j r) d")), writes=[hlk])
                    for fc in range(8):
                        pt_, ptk = PSB.next()
                        P.op(pe, lambda e, fc=fc, hl=hl, pt_=pt_: e.transpose(out=pt_[:, 0:120], in_=hl[:, fc * 128:(fc + 1) * 128], identity=identf[0:120, 0:120]), reads=[hlk], writes=[ptk])
                        P.op(ac, lambda e, fc=fc, half=half, pt_=pt_: e.activation(out=hist[:, fc, 120 * half:120 * half + 120], in_=pt_[:, 0:120], func=AF.Copy), reads=[ptk], writes=[("hist", fc, half)])
                for cb in range(2):
                    wut, wutk = wut_ring.next()
                    wload(wut[:], wutk, kchunks(w_in[:, U0 + cb * 512:U0 + (cb + 1) * 512]), bar=("P" not in NOBAR))
                    for (ti, dst, dk) in [(7, utok, "utok"), (8, utoks, "utoks")]:
                        pu, puk = PSB.next()
                        for c in range(KC):
                            P.op(pe, lambda e, c=c, pu=pu, ti=ti, wut=wut: e.matmul(pu[:], lhsT=hNTo[:, c, ti * 128:(ti + 1) * 128], rhs=wut[:, c, :], start=(c == 0), stop=(c == KC - 1)), reads=[wutk], writes=[puk])
                        P.op(ac, lambda e, pu=pu, dst=dst, cb=cb: e.activation(out=dst[:, cb * 512:(cb + 1) * 512], in_=pu[:], func=AF.Copy), reads=[puk], writes=[(dk, cb)])
                P.dma("sp", lambda e: e.dma_start(out=poolp_d, in_=utok[113:128, :]), reads=[("utok", 0), ("utok", 1)], is_output=True)
                for j in range(16):
                    P.dma("sp", lambda e, j=j: e.dma_start(out=pools_d[j, 7:15, :], in_=utoks[8 * j:8 * j + 8, :]), reads=[("utoks", 0), ("utoks", 1)], is_output=True)
                P.dma("sp", lambda e: e.dma_start(out=pools_d[:, 0:7, :], in_=spool[:, 8:15, :]), is_output=True)
                wu_t = {}

                def pool_load(fc):
                    wu, wk = wu_ring.next()
                    wload(wu[:], wk, kchunks(w_in[:, U0 + fc * 128:U0 + (fc + 1) * 128]), bar=("P" not in NOBAR))
                    wu_t[fc] = (wu, wk)
                pool_load(0)
                for fc in range(8):
                    if fc + 1 < 8:
                        pool_load(fc + 1)
                    g = fc // 2
                    w = 2 << g
                    wu, wk = wu_t[fc]
                    P.op(dv, lambda e, fc=fc: e.tensor_copy(out=full[:, 0:16], in_=uhist[:, fc, :]), reads=["full"], writes=["full"])
                    P.op(dv, lambda e, fc=fc: e.tensor_copy(out=fulls[:, :, 0:15], in_=hist[:, fc, :].rearrange("p (j r) -> p j r", r=15)), reads=[("hist", fc, 0), ("hist", fc, 1), "fulls"], writes=["fulls"])
                    for (t0, n) in TGS:
                        pu, puk = PSB.next()
                        for c in range(KC):
                            P.op(pe, lambda e, c=c, pu=pu, t0=t0, n=n, wu=wu: e.matmul(pu[:, 0:n], lhsT=wu[:, c, :], rhs=hNTo[:, c, t0:t0 + n], start=(c == 0), stop=(c == KC - 1)), reads=[wk], writes=[puk])
                        if t0 < 1024:
                            P.op(ac, lambda e, pu=pu, t0=t0, n=n: e.activation(out=full[:, 16 + t0:16 + t0 + n], in_=pu[:, 0:n], func=AF.Copy), reads=[puk, "full"], writes=["full"])
                        else:
                            P.op(ac, lambda e, pu=pu: e.activation(out=fulls[:, :, 15:23], in_=pu[:, 0:128].rearrange("p (j r) -> p j r", r=8), func=AF.Copy), reads=[puk, "fulls"], writes=["fulls"])
                    src, srck, srcs, srcsk = full, "full", fulls, "fulls"
                    bufs = [(wsA, "wsA", wsAs, "wsAs"), (wsB, "wsB", wsBs, "wsBs")]
                    for k in range(g + 1):
                        sh = 1 << k
                        dst, dstk, dsts, dstsk = bufs[k % 2]
                        P.op(dv, lambda e, src=src, dst=dst, sh=sh: e.tensor_tensor(out=dst[:, sh:1040], in0=src[:, sh:1040], in1=src[:, 0:1040 - sh], op=ALU.add), reads=[srck, dstk], writes=[dstk])
                        P.op(dv, lambda e, srcs=srcs, dsts=dsts, sh=sh: e.tensor_tensor(out=dsts[:, :, sh:23], in0=srcs[:, :, sh:23], in1=srcs[:, :, 0:23 - sh], op=ALU.add), reads=[srcsk, dstsk], writes=[dstsk])
                        src, srck, srcs, srcsk = dst, dstk, dsts, dstsk
                    P.op(dv, lambda e, src=src, fc=fc, w=w: e.scalar_tensor_tensor(out=pooledT[:, fc, 0:1024], in0=src[:, 16:1040], scalar=1.0 / w, in1=full[:, 16:1040], op0=ALU.mult, op1=ALU.subtract), reads=[srck, "full"], writes=[("pooledT", fc)])
                    P.op(dv, lambda e, src=src, g=g: e.tensor_tensor(out=tmp16[:], in0=src[:, 16:32], in1=rcb[:, g, :], op=ALU.mult), reads=[srck, "tmp16"], writes=["tmp16"])
                    P.op(dv, lambda e, fc=fc: e.tensor_tensor(out=pooledT[:, fc, 0:16], in0=tmp16[:], in1=full[:, 16:32], op=ALU.subtract), reads=["tmp16", "full", ("pooledT", fc)], writes=[("pooledT", fc)])
                    P.op(dv, lambda e, srcs=srcs, fc=fc, w=w: e.scalar_tensor_tensor(out=pooledT[:, fc, 1024:1152].rearrange("p (j r) -> p j r", r=8), in0=srcs[:, :, 15:23], scalar=1.0 / w, in1=fulls[:, :, 15:23], op0=ALU.mult, op1=ALU.subtract), reads=[srcsk, "fulls", ("pooledT", fc)], writes=[("pooledT", fc)])
                for g in range(4):
                    wp, wpk = wp_ring.next()
                    wload(wp[:], wpk, w_pool[g].rearrange("(c p) d -> p c d", p=128), bar=("P" not in NOBAR))
                    for dc in range(2):
                        for (t0, n) in TGS:
                            pm, pmk = PSB.next()
                            for cc in range(2):
                                P.op(pe, lambda e, cc=cc, pm=pm, wp=wp, dc=dc, g=g, t0=t0, n=n: e.matmul(pm[:, 0:n], lhsT=wp[:, cc, dc * 128:(dc + 1) * 128], rhs=pooledT[:, 2 * g + cc, t0:t0 + n], start=(cc == 0), stop=(cc == 1)), reads=[wpk, ("pooledT", 2 * g), ("pooledT", 2 * g + 1)], writes=[pmk])
                            P.op(ac, lambda e, pm=pm, g=g, dc=dc, t0=t0, n=n: e.activation(out=aT[:, 2 * g + dc, t0:t0 + n], in_=pm[:, 0:n], func=AF.Copy, scale=pscale[:, 2 * g + dc:2 * g + dc + 1]), reads=[pmk, "pscale"], writes=[("aT", 2 * g + dc, t0)])
                P.barrier_all()
                if maybe_stop("Pool"):
                    return nc
        dbg("aT", aT, [128, 8, NOWN])
        with Scope(bump) as stDD:
            wo_r = Ring(stDD, nc, "woD", [128, KC, 128], BF16, 2, bump=bump)
            wga_r = Ring(stDD, nc, "wga", [128, KC, 128], BF16, 2, bump=bump); wgb_r = Ring(stDD, nc, "wgb", [128, KC, 128], BF16, 2, bump=bump)
            wpa_r = Ring(stDD, nc, "wpa", [128, 8, 128], BF16, 2, bump=bump); wpb_r = Ring(stDD, nc, "wpb", [128, KC, 128], BF16, 2, bump=bump)
            sg_r = Ring(stDD, nc, "sg", [128, 512], F32, 3, bump=bump); t1_r = Ring(stDD, nc, "t1", [128, 512], F32, 2, bump=bump); t2_r = Ring(stDD, nc, "t2", [128, 512], F32, 2, bump=bump)
            wo_t = {}

            def d0_load(oc):
                wo, wok = wo_r.next()
                wload(wo[:], wok, kchunks(w_in[:, O0 + oc * 128:O0 + (oc + 1) * 128]), bar=("D0" not in NOBAR))
                wo_t[oc] = (wo, wok)
            d0_load(0)
            for oc in range(KC):
                if oc + 1 < KC:
                    d0_load(oc + 1)
                wo, wok = wo_t[oc]
                for (t0, n) in TGS:
                    po, pok = PSB.next()
                    for c in range(KC):
                        P.op(pe, lambda e, c=c, po=po, wo=wo, t0=t0, n=n: e.matmul(po[:, 0:n], lhsT=wo[:, c, :], rhs=hNTo[:, c, t0:t0 + n], start=(c == 0), stop=(c == KC - 1)), reads=[wok], writes=[pok])
                    sg, sgk = sg_r.next()
                    P.op(ac, lambda e, sg=sg, po=po, n=n: e.activation(out=sg[:, 0:n], in_=po[:, 0:n], func=AF.Sigmoid), reads=[pok], writes=[sgk])
                    P.op(dv, lambda e, sg=sg, oc=oc, t0=t0, n=n: e.tensor_tensor(out=hbT[:, oc, t0:t0 + n], in0=hbT[:, oc, t0:t0 + n], in1=sg[:, 0:n], op=ALU.mult), reads=[sgk], writes=[("hbTg", oc, t0)])
            P.barrier_all()
            if maybe_stop("D0"):
                return nc
            w_t = {}

            def d1_load(cb):
                wga, wgak = wga_r.next(); wgb, wgbk = wgb_r.next(); wpa, wpak = wpa_r.next(); wpb, wpbk = wpb_r.next()
                wload(wga[:], wgak, kchunks(w_in[:, GA0 + cb * 128:GA0 + (cb + 1) * 128]), bar=("D1" not in NOBAR))
                wload(wpa[:], wpak, kchunks(w_pa[:, cb * 128:(cb + 1) * 128]), bar=("D1" not in NOBAR))
                wload(wgb[:], wgbk, kchunks(w_in[:, GB0 + cb * 128:GB0 + (cb + 1) * 128]), bar=("D1" not in NOBAR))
                wload(wpb[:], wpbk, kchunks(w_pb[:, cb * 128:(cb + 1) * 128]), bar=("D1" not in NOBAR))
                w_t[cb] = (wga, wgak, wgb, wgbk, wpa, wpak, wpb, wpbk)
            d1_load(0)
            for cb in range(KC):
                if cb + 1 < KC:
                    d1_load(cb + 1)
                wga, wgak, wgb, wgbk, wpa, wpak, wpb, wpbk = w_t[cb]
                for (t0, n) in TGS:
                    pga, pgak = PSB.next()
                    for c in range(KC):
                        P.op(pe, lambda e, c=c, pga=pga, wga=wga, t0=t0, n=n: e.matmul(pga[:, 0:n], lhsT=wga[:, c, :], rhs=hNTo[:, c, t0:t0 + n], start=(c == 0), stop=(c == KC - 1)), reads=[wgak], writes=[pgak])
                    pa_, pak = PSB.next()
                    for c in range(8):
                        P.op(pe, lambda e, c=c, pa_=pa_, wpa=wpa, t0=t0, n=n: e.matmul(pa_[:, 0:n], lhsT=wpa[:, c, :], rhs=aT[:, c, t0:t0 + n], start=(c == 0), stop=(c == 7)), reads=[wpak], writes=[pak])
                    sga, sgak = sg_r.next()
                    P.op(ac, lambda e, sga=sga, pga=pga, n=n: e.activation(out=sga[:, 0:n], in_=pga[:, 0:n], func=AF.Sigmoid), reads=[pgak], writes=[sgak])
                    t1, t1k = t1_r.next()
                    P.op(dv, lambda e, t1=t1, sga=sga, pa_=pa_, n=n: e.tensor_tensor(out=t1[:, 0:n], in0=sga[:, 0:n], in1=pa_[:, 0:n], op=ALU.mult), reads=[sgak, pak], writes=[t1k])
                    pgb, pgbk = PSB.next()
                    for c in range(KC):
                        P.op(pe, lambda e, c=c, pgb=pgb, wgb=wgb, t0=t0, n=n: e.matmul(pgb[:, 0:n], lhsT=wgb[:, c, :], rhs=hNTo[:, c, t0:t0 + n], start=(c == 0), stop=(c == KC - 1)), reads=[wgbk], writes=[pgbk])
                    pb_, pbk = PSB.next()
                    for c in range(KC):
                        P.op(pe, lambda e, c=c, pb_=pb_, wpb=wpb, t0=t0, n=n: e.matmul(pb_[:, 0:n], lhsT=wpb[:, c, :], rhs=hbT[:, c, t0:t0 + n], start=(c == 0), stop=(c == KC - 1)), reads=[wpbk], writes=[pbk])
                    sgb, sgbk = sg_r.next()
                    P.op(ac, lambda e, sgb=sgb, pgb=pgb, n=n: e.activation(out=sgb[:, 0:n], in_=pgb[:, 0:n], func=AF.Sigmoid), reads=[pgbk], writes=[sgbk])
                    t2, t2k = t2_r.next()
                    P.op(dv, lambda e, t2=t2, sgb=sgb, pb_=pb_, n=n: e.tensor_tensor(out=t2[:, 0:n], in0=sgb[:, 0:n], in1=pb_[:, 0:n], op=ALU.mult), reads=[sgbk, pbk], writes=[t2k])
                    P.op(dv, lambda e, t1=t1, t2=t2, cb=cb, t0=t0, n=n: e.tensor_tensor(out=mixT[:, cb, t0:t0 + n], in0=t1[:, 0:n], in1=t2[:, 0:n], op=ALU.add), reads=[t1k, t2k], writes=[("mixT", cb, t0)])
            P.barrier_all()
            if maybe_stop("D"):
                return nc
        dbg("mixT", mixT, [128, KC, NOWN])
        with Scope(bump) as stE:
            woE = Ring(stE, nc, "woE", [128, KC, 512], BF16, 2, bump=bump)
            P.dma("sp", lambda e: e.dma_start(out=yacc, in_=xo.rearrange("(t p) d -> p t d", p=128)), writes=["yacc"])
            we_t = {}

            def e_load(cb):
                wo, wok = woE.next()
                wload(wo[:], wok, kchunks(w_out[:, cb * 512:(cb + 1) * 512]), bar=("E" not in NOBAR))
                we_t[cb] = (wo, wok)
            e_load(0)
            for cb in range(4):
                if cb + 1 < 4:
                    e_load(cb + 1)
                wo, wok = we_t[cb]
                for t in range(TOWN):
                    px, pxk = PSB.next()
                    for c in range(KC):
                        P.op(pe, lambda e, c=c, px=px, wo=wo, t=t: e.matmul(px[:], lhsT=mixT[:, c, t * 128:(t + 1) * 128], rhs=wo[:, c, :], start=(c == 0), stop=(c == KC - 1)), reads=[wok], writes=[pxk])
                    P.op(dv, lambda e, px=px, t=t, cb=cb: e.tensor_tensor(out=yacc[:, t, cb * 512:(cb + 1) * 512], in0=yacc[:, t, cb * 512:(cb + 1) * 512], in1=px[:], op=ALU.add), reads=[pxk, "yacc"], writes=[("yacc", t, cb)])
            P.barrier_all()
            if maybe_stop("E"):
                return nc
        dbg("x1", yacc, [128, TOWN, D])
        comb = view(A0 + 9216, [128, TOWN, NE], F32)
        hT_v = [view(A0 + k * 4608, [128, 2, NOWN], BF16) for k in range(2)]
        with Scope(bump) as stF0:
            hn_ring = Ring(stF0, nc, "hnF", [128, D], BF16, 2, bump=bump)
            ssq = sb("ssqF", [128, 4], F32, stF0); junk = sb("junkF", [128, D], BF16, stF0)
            Wr = sb("Wr", [128, KC, 36], BF16, stF0); bbc = sb("bbc", [128, 36], F32, stF0)
            L_r = Ring(stF0, nc, "Lr", [128, 36], F32, 2, bump=bump); elm_r = Ring(stF0, nc, "elm", [128, 32], F32, 2, bump=bump)
            elm2_r = Ring(stF0, nc, "elm2", [128, 32], F32, 2, bump=bump); oh1_r = Ring(stF0, nc, "oh1", [128, 32], F32, 2, bump=bump); oh2_r = Ring(stF0, nc, "oh2", [128, 32], F32, 2, bump=bump)
            s_r = Ring(stF0, nc, "rs", [128, 16], F32, 2, bump=bump); j4 = sb("j4", [128, 4], F32, stF0)
            P.dma("sp", lambda e: e.dma_start(out=gbc[:], in_=g_ffn.partition_broadcast(128)), writes=["gbc"])
            with nc.allow_non_contiguous_dma(reason="router weights"):
                wload(Wr[:, :, 0:4], "Wr0", kchunks(w_rg), True, bar=("F0" not in NOBAR))
                wload(Wr[:, :, 4:36], "Wr1", kchunks(w_re), True, bar=("F0" not in NOBAR))
            P.dma("sp", lambda e: e.dma_start(out=bbc[:, 0:4], in_=b_rg.partition_broadcast(128)), writes=["bbc0"])
            P.dma("sp", lambda e: e.dma_start(out=bbc[:, 4:36], in_=b_re.partition_broadcast(128)), writes=["bbc1"])
            for t in range(TOWN):
                rmsnorm_T(("sb", yacc[:, t, :], ("yacc_t", t)), hFT, "hFT", t * 128, None, hn_ring, (ssq, junk))
            for t in range(TOWN):
                plg, plk = PSB.next()
                for c in range(KC):
                    P.op(pe, lambda e, c=c, plg=plg, t=t: e.matmul(plg[:, 0:36], lhsT=hFT[:, c, t * 128:(t + 1) * 128], rhs=Wr[:, c, :], start=(c == 0), stop=(c == KC - 1)), reads=["Wr0", "Wr1", ("hFT", t, 0), ("hFT", t, 1)], writes=[plk])
                L, Lk = L_r.next(); elm, ek = elm_r.next(); elm2, e2k = elm2_r.next(); oh1, o1k = oh1_r.next(); oh2, o2k = oh2_r.next(); s, sk = s_r.next()
                P.op(dv, lambda e, L=L, plg=plg: e.tensor_tensor(out=L[:], in0=plg[:, 0:36], in1=bbc[:], op=ALU.add), reads=[plk, "bbc0", "bbc1"], writes=[Lk])
                P.op(dv, lambda e, L=L, s=s: e.tensor_reduce(out=s[:, 0:1], in_=L[:, 0:4], axis=AX.X, op=ALU.max), reads=[Lk], writes=[sk])
                P.op(dv, lambda e, s=s: e.tensor_scalar(out=s[:, 1:2], in0=s[:, 0:1], scalar1=-1.0, scalar2=None, op0=ALU.mult), reads=[sk], writes=[sk])
                P.op(ac, lambda e, L=L, s=s: e.activation(out=j4[:], in_=L[:, 0:4], func=AF.Exp, bias=s[:, 1:2], scale=1.0, accum_out=s[:, 2:3]), reads=[Lk, sk], writes=["j4", sk])
                P.op(dv, lambda e, s=s: e.reciprocal(out=s[:, 3:4], in_=s[:, 2:3]), reads=[sk], writes=[sk])
                P.op(dv, lambda e, L=L, s=s: e.tensor_scalar(out=s[:, 8:12], in0=L[:, 0:4], scalar1=s[:, 0:1], scalar2=-1.0, op0=ALU.is_ge, op1=ALU.add), reads=[Lk, sk], writes=[sk])
                P.op(dv, lambda e, s=s: e.tensor_scalar(out=s[:, 8:12], in0=s[:, 8:12], scalar1=1.0e9, scalar2=None, op0=ALU.mult), reads=[sk], writes=[sk])
                for g in range(4):
                    P.op(dv, lambda e, g=g, L=L, s=s, elm=elm: e.tensor_scalar(out=elm[:, 8 * g:8 * g + 8], in0=L[:, 4 + 8 * g:12 + 8 * g], scalar1=s[:, 8 + g:9 + g], scalar2=None, op0=ALU.add), reads=[Lk, sk, ek], writes=[ek])
                P.op(dv, lambda e, s=s, elm=elm: e.tensor_reduce(out=s[:, 4:5], in_=elm[:], axis=AX.X, op=ALU.max), reads=[ek, sk], writes=[sk])
                P.op(dv, lambda e, s=s, elm=elm, oh1=oh1: e.tensor_scalar(out=oh1[:], in0=elm[:], scalar1=s[:, 4:5], scalar2=None, op0=ALU.is_ge), reads=[ek, sk], writes=[o1k])
                P.op(dv, lambda e, elm=elm, oh1=oh1, elm2=elm2: e.scalar_tensor_tensor(out=elm2[:], in0=oh1[:], scalar=-1.0e9, in1=elm[:], op0=ALU.mult, op1=ALU.add), reads=[o1k, ek], writes=[e2k])
                P.op(dv, lambda e, s=s, elm2=elm2: e.tensor_reduce(out=s[:, 5:6], in_=elm2[:], axis=AX.X, op=ALU.max), reads=[e2k, sk], writes=[sk])
                P.op(dv, lambda e, s=s, elm2=elm2, oh2=oh2: e.tensor_scalar(out=oh2[:], in0=elm2[:], scalar1=s[:, 5:6], scalar2=None, op0=ALU.is_ge), reads=[e2k, sk], writes=[o2k])
                P.op(dv, lambda e, s=s: e.tensor_tensor(out=s[:, 6:7], in0=s[:, 4:5], in1=s[:, 5:6], op=ALU.subtract), reads=[sk], writes=[sk])
                P.op(ac, lambda e, s=s: e.activation(out=s[:, 6:7], in_=s[:, 6:7], func=AF.Sigmoid), reads=[sk], writes=[sk])
                P.op(dv, lambda e, s=s: e.tensor_tensor(out=s[:, 7:8], in0=s[:, 6:7], in1=s[:, 3:4], op=ALU.mult), reads=[sk], writes=[sk])
                P.op(dv, lambda e, s=s: e.tensor_tensor(out=s[:, 12:13], in0=s[:, 3:4], in1=s[:, 7:8], op=ALU.subtract), reads=[sk], writes=[sk])
                P.op(dv, lambda e, s=s, oh1=oh1, t=t: e.tensor_scalar(out=comb[:, t, :], in0=oh1[:], scalar1=s[:, 7:8], scalar2=None, op0=ALU.mult), reads=[o1k, sk], writes=[("comb", t)])
                P.op(dv, lambda e, s=s, oh2=oh2, t=t: e.scalar_tensor_tensor(out=comb[:, t, :], in0=oh2[:], scalar=s[:, 12:13], in1=comb[:, t, :], op0=ALU.mult, op1=ALU.add), reads=[o2k, sk, ("comb", t)], writes=[("comb", t)])
            P.barrier_all()
            if maybe_stop("F0"):
                return nc
        dbg("comb", comb, [128, TOWN, NE])
        with Scope(bump) as stF1:
            Wg_r = Ring(stF1, nc, "Wg", [128, KC, 256], BF16, 2, bump=bump); Wu_r = Ring(stF1, nc, "Wu", [128, KC, 256], BF16, 2, bump=bump)
            Wd_r = Ring(stF1, nc, "Wd", [128, 2, D], BF16, 2, bump=bump)
            sgl_r = Ring(stF1, nc, "sgl", [128, 512], F32, 2, bump=bump)
            ssq = sb("ssqF1", [128, 4], F32, stF1)
            units = [(e_, fh) for e_ in range(NE) for fh in range(2)]
            wt = {}

            def f_load(u):
                e_, fh = units[u]
                Wg, wgk = Wg_r.next(); Wu, wuk = Wu_r.next(); Wd, wdk = Wd_r.next()
                wload(Wg[:], wgk, kchunks(w_eg[e_][:, fh * 256:(fh + 1) * 256]), bar=("F1" not in NOBAR))
                wload(Wu[:], wuk, kchunks(w_eu[e_][:, fh * 256:(fh + 1) * 256]), bar=("F1" not in NOBAR))
                wload(Wd[:], wdk, w_ed[e_][fh * 256:(fh + 1) * 256, :].rearrange("(c p) d -> p c d", p=128), bar=("F1" not in NOBAR))
                wt[u] = (Wg, wgk, Wu, wuk, Wd, wdk)
            f_load(0)
            for u, (e_, fh) in enumerate(units):
                if u + 1 < len(units):
                    f_load(u + 1)
                Wg, wgk, Wu, wuk, Wd, wdk = wt.pop(u)
                hT, hTk = hT_v[u % 2], ("hT", u % 2)
                for fc in range(2):
                    for (t0, n) in TGS:
                        phg, phgk = PSB.next(); phu, phuk = PSB.next()
                        for c in range(KC):
                            P.op(pe, lambda e, c=c, phg=phg, Wg=Wg, fc=fc, t0=t0, n=n: e.matmul(phg[:, 0:n], lhsT=Wg[:, c, fc * 128:(fc + 1) * 128], rhs=hFT[:, c, t0:t0 + n], start=(c == 0), stop=(c == KC - 1)), reads=[wgk], writes=[phgk])
                        for c in range(KC):
                            P.op(pe, lambda e, c=c, phu=phu, Wu=Wu, fc=fc, t0=t0, n=n: e.matmul(phu[:, 0:n], lhsT=Wu[:, c, fc * 128:(fc + 1) * 128], rhs=hFT[:, c, t0:t0 + n], start=(c == 0), stop=(c == KC - 1)), reads=[wuk], writes=[phuk])
                        sgl, sglk = sgl_r.next()
                        P.op(ac, lambda e, sgl=sgl, phg=phg, n=n: e.activation(out=sgl[:, 0:n], in_=phg[:, 0:n], func=AF.Silu), reads=[phgk], writes=[sglk])
                        P.op(dv, lambda e, sgl=sgl, phu=phu, hT=hT, fc=fc, t0=t0, n=n: e.tensor_tensor(out=hT[:, fc, t0:t0 + n], in0=sgl[:, 0:n], in1=phu[:, 0:n], op=ALU.mult), reads=[sglk, phuk, hTk], writes=[hTk])
                for t in range(TOWN):
                    for cb in range(4):
                        py, pyk = PSB.next()
                        for fc in range(2):
                            P.op(pe, lambda e, fc=fc, py=py, hT=hT, Wd=Wd, t=t, cb=cb: e.matmul(py[:], lhsT=hT[:, fc, t * 128:(t + 1) * 128], rhs=Wd[:, fc, cb * 512:(cb + 1) * 512], start=(fc == 0), stop=(fc == 1)), reads=[hTk, wdk], writes=[pyk])
                        P.op(dv, lambda e, py=py, t=t, cb=cb, e_=e_: e.scalar_tensor_tensor(out=yacc[:, t, cb * 512:(cb + 1) * 512], in0=py[:], scalar=comb[:, t, e_:e_ + 1], in1=yacc[:, t, cb * 512:(cb + 1) * 512], op0=ALU.mult, op1=ALU.add), reads=[pyk, ("yacc", t, cb)], writes=[("yacc", t, cb)])
            P.dma("sp", lambda e: e.dma_start(out=gbc[:], in_=g_final.partition_broadcast(128)), writes=["gbc"])
            for t in range(TOWN):
                yk = [("yacc", t, cb) for cb in range(4)]
                sglj, sgljk = sgl_r.next()
                for cb in range(4):
                    P.op(ac, lambda e, t=t, cb=cb, sglj=sglj: e.activation(out=sglj[:], in_=yacc[:, t, cb * 512:(cb + 1) * 512], func=AF.Square, accum_out=ssq[:, cb:cb + 1]), reads=[("yacc", t, cb), "ssqF"], writes=[sgljk, "ssqF"])
                P.op(dv, lambda e: e.tensor_reduce(out=ssq[:, 0:1], in_=ssq[:, 0:4], axis=AX.X, op=ALU.add), reads=["ssqF"], writes=["ssqF"])
                P.op(ac, lambda e: e.activation(out=ssq[:, 1:2], in_=ssq[:, 0:1], func=AF.Sqrt, scale=1.0 / D, bias=EPS), reads=["ssqF"], writes=["ssqF"])
                P.op(dv, lambda e: e.reciprocal(out=ssq[:, 2:3], in_=ssq[:, 1:2]), reads=["ssqF"], writes=["ssqF"])
                P.op(dv, lambda e, t=t: e.scalar_tensor_tensor(out=yacc[:, t, :], in0=yacc[:, t, :], scalar=ssq[:, 2:3], in1=gbc[:], op0=ALU.mult, op1=ALU.mult), reads=yk + ["ssqF", "gbc"], writes=yk)
                P.dma("sp", lambda e, t=t: e.dma_start(out=y_d[t * 128:(t + 1) * 128, :], in_=yacc[:, t, :]), reads=yk, is_output=True)
            P.finish()
            P.emit()
            _DBG['P'] = P; _DBG['peak'] = bump.peak
    return nc


_NC = {}


def _get_nc(debug=None):
    key = tuple(debug) if debug else ()
    if key not in _NC:
        _NC[key] = build(debug)
    return _NC[key]


def make_in_maps(inp):
    f = lambda a: np.ascontiguousarray(np.asarray(a, dtype=np.float32))
    xpr = f(inp["x_prompt"]); xs = f(inp["x_sample"])
    shared = {
        "g_mix": f(inp["g_mix"][0]), "w_in": f(inp["w_in"][0]), "b_if": f(inp["b_if"][0]), "w_pool": f(inp["w_pool"][0]),
        "pool_scale": f(inp["pool_scale"][0]), "w_pa": f(inp["w_proj_a"][0]), "w_pb": f(inp["w_proj_b"][0]),
        "g_head": f(inp["g_head"][0]), "w_out": f(inp["w_out"][0]), "g_ffn": f(inp["g_ffn"][0]),
        "w_rg": f(inp["w_router_group"][0]), "b_rg": f(inp["b_router_group"][0]), "w_re": f(inp["w_router_expert"][0]),
        "b_re": f(inp["b_router_expert"][0]), "w_eg": f(inp["w_exp_gate"][0]), "w_eu": f(inp["w_exp_up"][0]),
        "w_ed": f(inp["w_exp_down"][0]), "g_final": f(inp["g_final"]),
    }
    spool = f(inp["state_pool"][0]); sC = f(inp["state_C"][0]); sn = f(inp["state_n"][0]); sm = f(inp["state_m"][0])
    maps = []
    for c in range(8):
        b, half = c // 2, c % 2
        xo = np.concatenate([xpr[b, half * 1024:(half + 1) * 1024], xs[16 * c:16 * c + 16].reshape(128, D)], axis=0)
        xp = xpr[b, 0:1024] if half == 1 else np.zeros((1024, D), np.float32)
        rc = np.zeros((4, 16), np.float32)
        for g in range(4):
            for t in range(16):
                rc[g, t] = 1.0 / min(half * 1024 + t + 1, 2 << g)
        m = dict(shared)
        m.update({
            "xo": np.ascontiguousarray(xo), "xp": np.ascontiguousarray(xp),
            "flag": np.full((128, 1), float(half), np.float32), "rc": rc.reshape(64),
            "spool": np.ascontiguousarray(spool[16 * c:16 * c + 16]), "sC": np.ascontiguousarray(sC[16 * c:16 * c + 16]),
            "sn": np.ascontiguousarray(sn[16 * c:16 * c + 16].reshape(64, HD)), "sm": np.ascontiguousarray(sm[16 * c:16 * c + 16]),
        })
        maps.append(m)
    return maps


def assemble(res):
    B = 4
    y_prompt = np.zeros((B, 2048, D), np.float32); y_sample = np.zeros((128, 8, D), np.float32)
    pool_p = np.zeros((1, B, 15, PW), np.float32); C_p = np.zeros((1, B, H, HD, HD), np.float32)
    n_p = np.zeros((1, B, H, HD), np.float32); m_p = np.zeros((1, B, H), np.float32)
    pool_s = np.zeros((1, 128, 15, PW), np.float32); C_s = np.zeros((1, 128, H, HD, HD), np.float32)
    n_s = np.zeros((1, 128, H, HD), np.float32); m_s = np.zeros((1, 128, H), np.float32)
    for c in range(8):
        r = res[c]
        b, half = c // 2, c % 2
        y_prompt[b, half * 1024:(half + 1) * 1024] = r["y"][0:1024]
        y_sample[16 * c:16 * c + 16] = r["y"][1024:].reshape(16, 8, D)
        if half == 1:
            pool_p[0, b] = r["pool_p"]; C_p[0, b] = r["C_p"]; n_p[0, b] = r["n_p"]; m_p[0, b] = r["m_p"][:, 0]
        pool_s[0, 16 * c:16 * c + 16] = r["pool_s"]; C_s[0, 16 * c:16 * c + 16] = r["C_s"]
        n_s[0, 16 * c:16 * c + 16] = r["n_s"].reshape(16, H, HD); m_s[0, 16 * c:16 * c + 16] = r["m_s"]
    return (y_prompt, y_sample, pool_p, C_p, n_p, m_p, pool_s, C_s, n_s, m_s)


def kernel(**inputs):
    nc = _get_nc()
    in_maps = make_in_maps(inputs)
    res = run_bass_kernel_spmd(nc, in_maps, core_ids=list(range(8)))
    return assemble(res.results)
```

```python
import contextlib
import numpy as np
import concourse.bass as bass
import concourse.mybir as mybir
from concourse.bass_utils import run_bass_kernel_spmd

F32 = mybir.dt.float32
BF16 = mybir.dt.bfloat16
AF = mybir.ActivationFunctionType
ALU = mybir.AluOpType
AX = mybir.AxisListType

ENGS = ("pe", "act", "dve", "pool", "sp")
NDMA = 8


class Prog:
    def __init__(self, nc):
        self.nc = nc
        self.lists = {e: [] for e in ENGS}
        self.cnt = {e: 0 for e in ENGS}
        self.dcnt = {e: 0 for e in ENGS}
        self.waited = {e: {} for e in ENGS}
        self.lastw = {}
        self.readers = {}
        self.out_tokens = []

    def _need(self, eng, tok, waits):
        if tok is None:
            return
        sk, v = tok
        if self.waited[eng].get(sk, 0) >= v:
            return
        if sk == "pe" and eng == "pe":
            return
        self.waited[eng][sk] = v
        waits.append((sk, v))

    def _deps(self, eng, reads, writes):
        toks = {}

        def add(t):
            if t is not None and toks.get(t[0], 0) < t[1]:
                toks[t[0]] = t[1]
        for k in reads:
            add(self.lastw.get(k))
        for k in writes:
            add(self.lastw.get(k))
            for t in self.readers.get(k, ()):
                add(t)
        waits = []
        for sk, v in toks.items():
            self._need(eng, (sk, v), waits)
        return waits

    def _commit(self, tok, reads, writes):
        for k in reads:
            self.readers.setdefault(k, []).append(tok)
        for k in writes:
            self.lastw[k] = tok
            self.readers[k] = []

    @staticmethod
    def _norm(reads, writes):
        def nk(k):
            if isinstance(k, str) and k.startswith("pss"):
                return "pss"
            return k
        def is_ps(k):
            return k in ("pss", "pacc") or (isinstance(k, tuple) and k[0] in ("psb", "pst"))
        r2, w2 = [], []
        for k in reads:
            k = nk(k)
            (w2 if is_ps(k) else r2).append(k)
        for k in writes:
            w2.append(nk(k))
        return r2, w2

    def op(self, eng, fn, reads=(), writes=()):
        reads, writes = self._norm(reads, writes)
        waits = self._deps(eng, reads, writes)
        self.cnt[eng] += 1
        tok = (eng, self.cnt[eng])
        self.lists[eng].append(("op", fn, waits, None))
        self._commit(tok, reads, writes)
        return tok

    def dma(self, q, fn, reads=(), writes=(), is_output=False):
        waits = self._deps(q, reads, writes)
        j = self.dcnt[q]
        self.dcnt[q] += 1
        sk = ("dma", q, j % NDMA)
        val = 16 * (j // NDMA + 1)
        if val > 16:
            self._need(q, (sk, val - 16), waits)
        tok = (sk, val)
        if q == "pool":
            if getattr(self, "_last_pool_tok", None) is not None:
                self._need(q, self._last_pool_tok, waits)
            self._last_pool_tok = tok
        self.lists[q].append(("dma", fn, waits, sk))
        self._commit(tok, reads, writes)
        if is_output:
            self.out_tokens.append(tok)
        return tok

    def barrier_all(self):
        toks = [(e, self.cnt[e]) for e in ENGS if self.cnt[e] > 0]
        for q in ENGS:
            for s in range(min(NDMA, self.dcnt[q])):
                n_uses = (self.dcnt[q] - 1 - s) // NDMA + 1
                toks.append((("dma", q, s), 16 * n_uses))
        for e in ENGS:
            waits = []
            for t in toks:
                if t[0] == e:
                    continue
                self._need(e, t, waits)
            if waits:
                self.lists[e].append(("wait", None, waits, None))
        self.lastw.clear()
        self.readers.clear()

    def finish(self):
        waits = []
        for t in self.out_tokens:
            self._need("sp", t, waits)
        self.lists["sp"].append(("wait", None, waits, None))

    def emit(self):
        nc = self.nc
        with contextlib.ExitStack() as st:
            sems = {}
            for e in ENGS:
                sems[e] = st.enter_context(nc.semaphore("s_" + e))
                for s in range(NDMA):
                    sems[("dma", e, s)] = st.enter_context(nc.semaphore("d_%s_%d" % (e, s)))
            block = st.enter_context(nc.Block())

            def run(engname):
                def body(eng):
                    for kind, fn, waits, sk in self.lists[engname]:
                        for (wk, v) in waits:
                            eng.wait_ge(sems[wk], v)
                        if kind == "op":
                            fn(eng).then_inc(sems[engname], 1)
                        elif kind == "dma":
                            fn(eng).then_inc(sems[sk], 16)
                return body

            block.tensor(run("pe"))
            block.scalar(run("act"))
            block.vector(run("dve"))
            block.gpsimd(run("pool"))
            block.sync(run("sp"))


D = 2048
KC = 16
NPRE = 1024
NOWN = 1152
TPRE = 8
TOWN = 9
H = 4
HD = 512
PW = 1024
U0, Q0, K0, V0, O0, IG0, FG0, GA0, GB0, INC = 0, 1024, 3072, 5120, 7168, 9216, 9220, 9224, 11272, 13320
NE = 32
EFF = 512
EPS = 1e-6
NEG = -30000.0
KSCALE = HD ** -0.5
TGS = [(0, 512), (512, 512), (1024, 128)]


class Bump:
    TOTAL = 204800

    def __init__(self, arena, start):
        self.arena = arena
        self.top = start
        self.peak = start

    def view(self, off, shape, dt):
        esz = 2 if dt == BF16 else 4
        n = 1
        for s in shape[1:]:
            n *= s
        nbytes = n * esz
        assert off % 4 == 0 and nbytes % 4 == 0 and off + nbytes <= self.TOTAL, (off, shape, nbytes)
        v = self.arena[0:shape[0], off // 4:(off + nbytes) // 4]
        if dt == BF16:
            v = v.bitcast(BF16)
        if len(shape) == 3:
            v = v.rearrange("p (a b) -> p a b", a=shape[1])
        elif len(shape) == 4:
            v = v.rearrange("p (a b c) -> p a b c", a=shape[1], b=shape[2])
        return v

    def alloc(self, shape, dt):
        esz = 2 if dt == BF16 else 4
        n = 1
        for s in shape[1:]:
            n *= s
        nbytes = (n * esz + 3) // 4 * 4
        shape2 = list(shape)
        if nbytes != n * esz:
            assert len(shape) == 2
            shape2 = [shape[0], nbytes // esz]
        off = (self.top + 63) // 64 * 64
        self.top = off + nbytes
        self.peak = max(self.peak, self.top)
        assert self.top <= self.TOTAL, ("SBUF arena overflow", self.top, shape)
        v = self.view(off, shape2, dt)
        if shape2 != list(shape):
            v = v[:, 0:shape[1]]
        return v


class Scope:
    def __init__(self, bump):
        self.bump = bump

    def __enter__(self):
        self.mark = self.bump.top
        return self

    def __exit__(self, *a):
        self.bump.top = self.mark
        return False


class Ring:
    def __init__(self, st, nc, name, shape, dt, n, psum=False, bump=None):
        if psum:
            self.t = [st.enter_context(nc.psum_tensor("%s%d" % (name, i), shape, dt)) for i in range(n)]
        else:
            self.t = [bump.alloc(shape, dt) for i in range(n)]
        self.k = [(name, i) for i in range(n)]
        self.i = 0

    def next(self):
        j = self.i % len(self.t)
        self.i += 1
        return self.t[j], self.k[j]


_DBG = {}


C2Q = "act"
NOBAR = {"F1", "D0", "D1", "E", "P", "B", "F0", "C1"}


def build(debug=None, stop=None):
    nc = bass.Bass("TRN2", target_bir_lowering=False)
    P = Prog(nc)
    debug = debug or ()

    def maybe_stop(tag):
        if stop == tag:
            P.barrier_all()
            P.finish()
            P.emit()
            _DBG['P'] = P
            return True
        return False

    def din(name, shape):
        return nc.dram_tensor(name, shape, F32, kind="ExternalInput").ap()

    def dout(name, shape):
        return nc.dram_tensor(name, shape, F32, kind="ExternalOutput").ap()

    xo = din("xo", [NOWN, D]); xp = din("xp", [NPRE, D])
    flag_d = din("flag", [128, 1]); rc_d = din("rc", [64])
    spool = din("spool", [16, 15, PW]); sC = din("sC", [16, H, HD, HD]); sn = din("sn", [64, HD]); sm = din("sm", [16, H])
    g_mix = din("g_mix", [D]); w_in = din("w_in", [D, INC]); b_if = din("b_if", [8])
    w_pool = din("w_pool", [4, 256, 256]); pool_scale = din("pool_scale", [PW])
    w_pa = din("w_pa", [PW, D]); w_pb = din("w_pb", [D, D]); g_head = din("g_head", [D]); w_out = din("w_out", [D, D])
    g_ffn = din("g_ffn", [D]); w_rg = din("w_rg", [D, 4]); b_rg = din("b_rg", [4]); w_re = din("w_re", [D, NE]); b_re = din("b_re", [NE])
    w_eg = din("w_eg", [NE, D, EFF]); w_eu = din("w_eu", [NE, D, EFF]); w_ed = din("w_ed", [NE, EFF, D]); g_final = din("g_final", [D])

    y_d = dout("y", [NOWN, D]); poolp_d = dout("pool_p", [15, PW]); Cp_d = dout("C_p", [H, HD, HD]); np_d = dout("n_p", [H, HD]); mp_d = dout("m_p", [H, 1])
    pools_d = dout("pool_s", [16, 15, PW]); Cs_d = dout("C_s", [16, H, HD, HD]); ns_d = dout("n_s", [64, HD]); ms_d = dout("m_s", [16, H])
    cpre_d = dout("scr_cpre", [H, 128, 4 * HD])
    npre_d = dout("scr_npre", [H, 128, 4])

    def kchunks(ap2d):
        return ap2d.rearrange("(c p) n -> p c n", p=128)

    dv, pl, ac, pe = "dve", "pool", "act", "pe"

    with contextlib.ExitStack() as st0:
        Y0, M0, A0, AEND = 0, 73728, 110592, 129024
        arena = st0.enter_context(nc.sbuf_tensor("arena", [128, Bump.TOTAL // 4], F32))
        bump = Bump(arena, AEND)
        view = bump.view

        def sb(name, shape, dt=F32, st=None):
            return bump.alloc(shape, dt)

        hNTo = view(Y0, [128, KC, NOWN], BF16)
        hbT = view(Y0 + 36864, [128, KC, NOWN], BF16)
        yacc = view(Y0, [128, TOWN, D], F32)
        hNTp = view(M0, [128, KC, NPRE], BF16)
        mixT = view(M0, [128, KC, NOWN], BF16)
        hFT = view(M0, [128, KC, NOWN], BF16)
        aT = view(A0, [128, 8, NOWN], BF16)

        PSB = Ring(st0, nc, "psb", [128, 512], F32, 4, psum=True)
        pss = st0.enter_context(nc.psum_tensor("pss", [128, 512], F32))
        pacc = st0.enter_context(nc.psum_tensor("pacc", [128, 512], F32))
        PST = Ring(st0, nc, "pst", [128, 1024], BF16, 2, psum=True)

        identb = sb("identb", [128, 128], BF16); identf = sb("identf", [128, 128])
        maskP = sb("maskP", [128, 128]); maskS = sb("maskS", [128, 128])
        E16 = sb("E16", [16, 128]); blk = sb("blk", [128, 16])
        sel = sb("sel", [4, 4, 128]); ones_b = sb("ones_b", [128, 1], BF16)
        flag = sb("flag", [128, 1]); rcb = sb("rcb", [128, 4, 16])
        gbc = sb("gbc", [128, D])

        QA = Bump.TOTAL // 16
        for qi, eng_ in enumerate([dv, pl, dv, pl]):
            P.op(eng_, lambda e, qi=qi: e.memset(arena[:, qi * QA:(qi + 1) * QA], 0.0), writes=[("arena0", qi)])
        P.barrier_all()
        P.op(pl, lambda e: e.memset(identf[:], 1.0), writes=["identf"])
        P.op(pl, lambda e: e.affine_select(out=identf[:], in_=identf[:], pattern=[[-1, 128]], compare_op=ALU.is_equal, fill=0.0, base=0, channel_multiplier=1), reads=["identf"], writes=["identf"])
        P.op(dv, lambda e: e.tensor_copy(out=identb[:], in_=identf[:]), reads=["identf"], writes=["identb"])
        P.op(pl, lambda e: e.memset(maskP[:], 0.0), writes=["maskP"])
        P.op(pl, lambda e: e.affine_select(out=maskP[:], in_=maskP[:], pattern=[[1, 128]], compare_op=ALU.is_ge, fill=NEG, base=0, channel_multiplier=-1), reads=["maskP"], writes=["maskP"])
        P.op(pl, lambda e: e.memset(E16[:], 1.0), writes=["E16"])
        P.op(pl, lambda e: e.affine_select(out=E16[:], in_=E16[:], pattern=[[1, 128]], compare_op=ALU.is_ge, fill=0.0, base=0, channel_multiplier=-8), reads=["E16"], writes=["E16"])
        P.op(pl, lambda e: e.affine_select(out=E16[:], in_=E16[:], pattern=[[-1, 128]], compare_op=ALU.is_ge, fill=0.0, base=7, channel_multiplier=8), reads=["E16"], writes=["E16"])
        P.op(pe, lambda e: e.matmul(pss[:, 0:128], lhsT=E16[:], rhs=E16[:], start=True, stop=True), reads=["E16"], writes=["pss"])
        P.op(dv, lambda e: e.tensor_scalar(out=maskS[:], in0=pss[:, 0:128], scalar1=-1.0, scalar2=-NEG, op0=ALU.add, op1=ALU.mult), reads=["pss"], writes=["maskS"])
        P.op(dv, lambda e: e.tensor_tensor(out=maskS[:], in0=maskS[:], in1=maskP[:], op=ALU.add), reads=["maskS", "maskP"], writes=["maskS"])
        P.op(pe, lambda e: e.transpose(out=pss[:, 128:144], in_=E16[:], identity=identf[0:16, 0:16]), reads=["E16", "identf", "maskS"], writes=["pss"])
        P.op(dv, lambda e: e.tensor_copy(out=blk[:], in_=pss[:, 128:144]), reads=["pss"], writes=["blk"])
        P.op(pl, lambda e: e.memset(sel[:], 1.0), writes=["sel"])
        P.op(pl, lambda e: e.affine_select(out=sel[:], in_=sel[:], pattern=[[-1, 4], [0, 128]], compare_op=ALU.is_equal, fill=0.0, base=0, channel_multiplier=1), reads=["sel"], writes=["sel"])
        P.op(pl, lambda e: e.memset(ones_b[:], 1.0), writes=["ones_b"])
        P.dma("sp", lambda e: e.dma_start(out=flag[:], in_=flag_d), writes=["flag"])
        P.dma("sp", lambda e: e.dma_start(out=rcb[:].rearrange("p a b -> p (a b)"), in_=rc_d.partition_broadcast(128)), writes=["rcb"])
        P.dma("sp", lambda e: e.dma_start(out=gbc[:], in_=g_mix.partition_broadcast(128)), writes=["gbc"])

        def dbg(name, ap_sb, shape, cast=False):
            if name not in debug:
                return
            d = dout("dbg_" + name, shape)
            P.barrier_all()
            P.dma("pool" if cast else "sp", lambda e: e.dma_start(out=d, in_=ap_sb), is_output=True)
            P.barrier_all()

        def rmsnorm_T(src, dstT, dst_key, tok0, xt_ring, hn_ring, small):
            if isinstance(src, tuple):
                _, xt_ap, xk = src
            else:
                xt, xk = xt_ring.next()
                P.dma("sp", lambda e: e.dma_start(out=xt[:], in_=src), writes=[xk])
                xt_ap = xt[:]
            hn, hk = hn_ring.next()
            ssq, junk = small
            P.op(ac, lambda e: e.activation(out=junk[:], in_=xt_ap, func=AF.Square, accum_out=ssq[:, 0:1]), reads=[xk], writes=["junk", "ssq"])
            P.op(ac, lambda e: e.activation(out=ssq[:, 1:2], in_=ssq[:, 0:1], func=AF.Sqrt, scale=1.0 / D, bias=EPS), reads=["ssq"], writes=["ssq"])
            P.op(dv, lambda e: e.reciprocal(out=ssq[:, 2:3], in_=ssq[:, 1:2]), reads=["ssq"], writes=["ssq"])
            P.op(dv, lambda e: e.scalar_tensor_tensor(out=hn[:], in0=xt_ap, scalar=ssq[:, 2:3], in1=gbc[:], op0=ALU.mult, op1=ALU.mult), reads=[xk, "ssq", "gbc"], writes=[hk])
            for half in range(2):
                pt, pk = PST.next()
                for c in range(8):
                    cc = half * 8 + c
                    P.op(pe, lambda e, c=c, cc=cc, pt=pt: e.transpose(out=pt[:, c * 128:(c + 1) * 128], in_=hn[:, cc * 128:(cc + 1) * 128], identity=identb[:]), reads=[hk, "identb"], writes=[pk])
                if half == 0:
                    P.op(ac, lambda e, pt=pt, half=half: e.activation(out=dstT[:, half * 8:(half + 1) * 8, tok0:tok0 + 128], in_=pt[:].rearrange("p (c t) -> p c t", c=8), func=AF.Copy), reads=[pk], writes=[(dst_key, tok0 // 128, half)])
                else:
                    P.op(dv, lambda e, pt=pt, half=half: e.tensor_copy(out=dstT[:, half * 8:(half + 1) * 8, tok0:tok0 + 128], in_=pt[:].rearrange("p (c t) -> p c t", c=8)), reads=[pk], writes=[(dst_key, tok0 // 128, half)])

        def wload(dst, key, src_ap, slow=False, bar=True):
            if bar:
                P.barrier_all()
            P.dma("pool", lambda e: e.dma_start(out=dst, in_=src_ap, allow_slow_non_contiguous=slow), writes=[key])
            if bar:
                P.barrier_all()

        with Scope(bump) as stMid:
            colq = sb("colq", [128, TPRE + TOWN, 20], F32, stMid)
            WLI = sb("WLI", [4, 32], F32, stMid); wliS = sb("wliS", [128, 32, 4], F32, stMid)
            alpha = sb("alpha", [4, NOWN], F32, stMid)
            uhist = sb("uhist", [128, 8, 16], F32, stMid)
            with Scope(bump) as stA:
                xt_ring = Ring(stA, nc, "xt", [128, D], F32, 2, bump=bump)
                hn_ring = Ring(stA, nc, "hn", [128, D], BF16, 2, bump=bump)
                ssq = sb("ssqA", [128, 4], F32, stA); junk = sb("junkA", [128, D], BF16, stA)
                for i in range(TPRE):
                    rmsnorm_T(xp[i * 128:(i + 1) * 128, :], hNTp, "hNTp", i * 128, xt_ring, hn_ring, (ssq, junk))
                for i in range(TOWN):
                    rmsnorm_T(xo[i * 128:(i + 1) * 128, :], hNTo, "hNTo", i * 128, xt_ring, hn_ring, (ssq, junk))
                P.barrier_all()
                if maybe_stop("A"):
                    return nc
            NT = NPRE + NOWN
            with Scope(bump) as stB:
                R = [sb("row%d" % i, [4, NT], F32, stB) for i in range(6)]
                ig, lf, mrow, brow, arow, beta = R
                Wig = sb("Wig", [128, KC, 4], BF16, stB); Wfg = sb("Wfg", [128, KC, 4], BF16, stB)
                bi = sb("bi", [4, 2], F32, stB); nbf = sb("nbf", [4, 1], F32, stB)
                smT = sb("smT", [4, 16], F32, stB); zrow = sb("zrow", [4, 128], F32, stB)
                mprev = sb("mprev", [4, 16], F32, stB); tmp4 = sb("tmp4", [4, 16], F32, stB)
                with nc.allow_non_contiguous_dma(reason="tiny gate loads"):
                    wload(Wig[:], "Wig", kchunks(w_in[:, IG0:IG0 + 4]), True, bar=("B" not in NOBAR))
                    wload(Wfg[:], "Wfg", kchunks(w_in[:, FG0:FG0 + 4]), True, bar=("B" not in NOBAR))
                    P.dma("sp", lambda e: e.dma_start(out=bi[:], in_=b_if.rearrange("(two h) -> h two", two=2), allow_slow_non_contiguous=True), writes=["bi"])
                    P.dma("sp", lambda e: e.dma_start(out=smT[:], in_=sm.rearrange("j h -> h j"), allow_slow_non_contiguous=True), writes=["smT"])
                P.op(dv, lambda e: e.tensor_scalar(out=nbf[:], in0=bi[:, 1:2], scalar1=-1.0, scalar2=None, op0=ALU.mult), reads=["bi"], writes=["nbf"])
                P.op(dv, lambda e: e.memset(zrow[:], 0.0), writes=["zrow"])
                if maybe_stop("B1"):
                    return nc
                groups = [(hNTp, 0, 512, 0), (hNTp, 512, 512, 512)] + [(hNTo, t0, n, NPRE + t0) for (t0, n) in TGS]
                for (src, t0, n, col0) in groups:
                    pg, pgk = PSB.next(); pf, pfk = PSB.next()
                    for c in range(KC):
                        P.op(pe, lambda e, c=c, pg=pg, src=src, t0=t0, n=n: e.matmul(pg[0:4, 0:n], lhsT=Wig[:, c, :], rhs=src[:, c, t0:t0 + n], start=(c == 0), stop=(c == KC - 1)), reads=["Wig"], writes=[pgk])
                    for c in range(KC):
                        P.op(pe, lambda e, c=c, pf=pf, src=src, t0=t0, n=n: e.matmul(pf[0:4, 0:n], lhsT=Wfg[:, c, :], rhs=src[:, c, t0:t0 + n], start=(c == 0), stop=(c == KC - 1)), reads=["Wfg"], writes=[pfk])
                    P.op(ac, lambda e, pg=pg, col0=col0, n=n: e.activation(out=ig[:, col0:col0 + n], in_=pg[0:4, 0:n], func=AF.Identity, bias=bi[:, 0:1], scale=1.0), reads=[pgk, "bi"], writes=["ig"])
                    P.op(ac, lambda e, pf=pf, col0=col0, n=n: e.activation(out=lf[:, col0:col0 + n], in_=pf[0:4, 0:n], func=AF.Exp, bias=nbf[:, 0:1], scale=-1.0), reads=[pfk, "nbf"], writes=["lf"])
                P.op(ac, lambda e: e.activation(out=lf[:], in_=lf[:], func=AF.Ln, bias=1.0, scale=1.0), reads=["lf"], writes=["lf"])
                P.op(dv, lambda e: e.tensor_scalar(out=lf[:], in0=lf[:], scalar1=-1.0, scalar2=None, op0=ALU.mult), reads=["lf"], writes=["lf"])
                if maybe_stop("B2"):
                    return nc
                P.op(dv, lambda e: e.tensor_tensor_scan(out=mrow[:, 0:NPRE], data0=lf[:, 0:NPRE], data1=ig[:, 0:NPRE], initial=0.0, op0=ALU.add, op1=ALU.max), reads=["lf", "ig"], writes=["mrow"])
                P.op(dv, lambda e: e.memset(mprev[:], 0.0), writes=["mprev"])
                P.op(dv, lambda e: e.tensor_tensor(out=mprev[:, 8:9], in0=mrow[:, NPRE - 1:NPRE], in1=flag[0:4, :], op=ALU.mult), reads=["mrow", "flag", "mprev"], writes=["mprev"])
                P.op(dv, lambda e: e.tensor_tensor_scan(out=mrow[:, NPRE:2048], data0=lf[:, NPRE:2048], data1=ig[:, NPRE:2048], initial=mprev[:, 8:9], op0=ALU.add, op1=ALU.max), reads=["lf", "ig", "mprev", "mrow"], writes=["mrow"])
                for j in range(16):
                    a = 2048 + 8 * j
                    P.op(dv, lambda e, a=a, j=j: e.tensor_tensor_scan(out=mrow[:, a:a + 8], data0=lf[:, a:a + 8], data1=ig[:, a:a + 8], initial=smT[:, j:j + 1], op0=ALU.add, op1=ALU.max), reads=["lf", "ig", "smT", "mrow"], writes=["mrow"])
                    P.op(dv, lambda e, a=a: e.tensor_tensor_scan(out=brow[:, a:a + 8], data0=lf[:, a:a + 8], data1=zrow[:, 0:8], initial=0.0, op0=ALU.add, op1=ALU.add), reads=["lf", "zrow", "brow"], writes=["brow"])
                for i in range(16):
                    a = 128 * i
                    P.op(dv, lambda e, a=a: e.tensor_tensor_scan(out=brow[:, a:a + 128], data0=lf[:, a:a + 128], data1=zrow[:], initial=0.0, op0=ALU.add, op1=ALU.add), reads=["lf", "zrow", "brow"], writes=["brow"])
                P.op(dv, lambda e: e.tensor_copy(out=mprev[:, 1:8], in_=mrow[:, 127:127 + 7 * 128:128]), reads=["mrow", "mprev"], writes=["mprev"])
                P.op(dv, lambda e: e.tensor_copy(out=mprev[:, 9:16], in_=mrow[:, NPRE + 127:NPRE + 127 + 7 * 128:128]), reads=["mrow", "mprev"], writes=["mprev"])
                if maybe_stop("B3"):
                    return nc
                P.dma("sp", lambda e: e.dma_start(out=mp_d, in_=mrow[:, 2047:2048]), reads=["mrow"], is_output=True)
                with nc.allow_non_contiguous_dma(reason="tiny m out"):
                    P.dma("sp", lambda e: e.dma_start(out=ms_d.rearrange("j h -> h j"), in_=mrow[:, 2048 + 7:NT:8], allow_slow_non_contiguous=True), reads=["mrow"], is_output=True)
                P.op(dv, lambda e: e.tensor_tensor(out=arow[:], in0=brow[:], in1=mrow[:], op=ALU.subtract), reads=["brow", "mrow"], writes=["arow"])
                if maybe_stop("B4"):
                    return nc
                P.op(dv, lambda e: e.tensor_tensor(out=beta[:], in0=ig[:], in1=brow[:], op=ALU.subtract), reads=["ig", "brow"], writes=["beta"])
                P.op(dv, lambda e: e.tensor_copy(out=alpha[:], in_=arow[:, NPRE:NT]), reads=["arow"], writes=["alpha"])
                emneg, winter, wl = brow, lf, ig
                P.op(ac, lambda e: e.activation(out=emneg[:], in_=mrow[:], func=AF.Exp, scale=-1.0), reads=["mrow", "brow", "beta", "arow"], writes=["brow"])
                for i in range(16):
                    a = 128 * i
                    P.op(ac, lambda e, a=a, i=i: e.activation(out=winter[:, a:a + 128], in_=arow[:, a:a + 128], func=AF.Exp, bias=mprev[:, i:i + 1], scale=1.0), reads=["arow", "mprev", "lf", "mrow"], writes=["lf"])
                    P.op(ac, lambda e, a=a: e.activation(out=wl[:, a:a + 128], in_=beta[:, a:a + 128], func=AF.Exp, bias=arow[:, a + 127:a + 128], scale=1.0), reads=["beta", "arow", "ig"], writes=["ig"])
                for j in range(16):
                    a = 2048 + 8 * j
                    P.op(ac, lambda e, a=a, j=j: e.activation(out=winter[:, a:a + 8], in_=arow[:, a:a + 8], func=AF.Exp, bias=smT[:, j:j + 1], scale=1.0), reads=["arow", "smT", "lf"], writes=["lf"])
                    P.op(ac, lambda e, a=a: e.activation(out=wl[:, a:a + 8], in_=beta[:, a:a + 8], func=AF.Exp, bias=arow[:, a + 7:a + 8], scale=1.0), reads=["beta", "arow", "ig"], writes=["ig"])
                P.op(dv, lambda e: e.tensor_tensor(out=tmp4[:], in0=arow[:, 127:2048:128], in1=mprev[:], op=ALU.add), reads=["arow", "mprev"], writes=["tmp4"])
                P.op(ac, lambda e: e.activation(out=WLI[:, 0:16], in_=tmp4[:], func=AF.Exp), reads=["tmp4"], writes=["WLI"])
                P.op(dv, lambda e: e.tensor_tensor(out=tmp4[:], in0=arow[:, 2048 + 7:NT:8], in1=smT[:], op=ALU.add), reads=["arow", "smT", "tmp4", "WLI"], writes=["tmp4"])
                P.op(ac, lambda e: e.activation(out=WLI[:, 16:32], in_=tmp4[:], func=AF.Exp), reads=["tmp4", "WLI"], writes=["WLI"])
                if maybe_stop("B5"):
                    return nc
                for i in range(TPRE + TOWN):
                    a = 128 * i
                    for qi, (rowt, rk) in enumerate([(beta, "beta"), (winter, "lf"), (emneg, "brow"), (wl, "ig"), (arow, "arow")]):
                        P.op(pe, lambda e, rowt=rowt, a=a, qi=qi: e.transpose(out=pss[:, 256 + 4 * qi:256 + 4 * qi + 4], in_=rowt[:, a:a + 128], identity=identf[0:4, 0:4]), reads=[rk, "identf"], writes=["pssq"])
                    P.op(dv, lambda e, i=i: e.tensor_copy(out=colq[:, i, :], in_=pss[:, 256:276]), reads=["pssq"], writes=[("colq", i)])
                    if i == 0 and maybe_stop("B5a"):
                        return nc
                    if i == 7 and maybe_stop("B5b"):
                        return nc
                    if i == 16 and maybe_stop("B5c"):
                        return nc
                rowc_r = Ring(stB, nc, "rowc", [4, 128], F32, 2, bump=bump)
                for i in range(32):
                    rowc, rck = rowc_r.next()
                    P.op(ac, lambda e, rowc=rowc, i=i: e.activation(out=rowc[:], in_=zrow[:], func=AF.Identity, bias=WLI[:, i:i + 1], scale=0.0), reads=["WLI", "zrow"], writes=[rck])
                    P.op(pe, lambda e, rowc=rowc, i=i: e.transpose(out=pss[:, 288 + 4 * i:288 + 4 * i + 4], in_=rowc[:], identity=identf[0:4, 0:4]), reads=[rck], writes=["pssw"])
                P.op(dv, lambda e: e.tensor_copy(out=wliS[:].rearrange("p i h -> p (i h)"), in_=pss[:, 288:416]), reads=["pssw"], writes=["wliS"])
                if maybe_stop("B6"):
                    return nc
                wuB = [view(Y0 + 36864 + k * 4096, [128, KC, 128], BF16) for k in range(2)]
                for fc in range(8):
                    wu, wk = wuB[fc % 2], ("wuB", fc % 2)
                    wload(wu, wk, kchunks(w_in[:, U0 + fc * 128:U0 + (fc + 1) * 128]), bar=("B" not in NOBAR))
                    pu, puk = PSB.next()
                    for c in range(KC):
                        P.op(pe, lambda e, c=c, wu=wu, pu=pu: e.matmul(pu[:, 0:16], lhsT=wu[:, c, :], rhs=hNTp[:, c, NPRE - 16:NPRE], start=(c == 0), stop=(c == KC - 1)), reads=[wk], writes=[puk])
                    P.op(ac, lambda e, fc=fc, pu=pu: e.activation(out=uhist[:, fc, :], in_=pu[:, 0:16], func=AF.Copy), reads=[puk], writes=[("uhist", fc)])
                if "rows" in debug:
                    for nm, t in [("ig_wl", ig), ("lf_winter", lf), ("mrow", mrow), ("b_emneg", brow), ("arow", arow), ("beta", beta)]:
                        dbg_ap = t[:]
                        d = dout("dbg_" + nm, [4, NT])
                        P.barrier_all()
                        P.dma("sp", lambda e, d=d, dbg_ap=dbg_ap: e.dma_start(out=d, in_=dbg_ap), is_output=True)
                P.barrier_all()
                if maybe_stop("B"):
                    return nc
            dbg("hNTo", hNTo, [128, KC, NOWN])
            dbg("colq", colq[:], [128, TPRE + TOWN, 20])
            with Scope(bump) as stC0:
                WkC0 = [view(Y0 + 36864 + k * 16384, [128, KC, HD], BF16) for k in range(2)]
                Wv_r = Ring(stC0, nc, "WvC0_", [128, KC, HD], BF16, 2, bump=bump)
                pad0 = sb("pad0", [128, 1024], F32, stC0)
                Cst = sb("Cst0", [128, 4, HD], F32, stC0); nst = sb("nst0", [128, 4], F32, stC0)
                kt_r = Ring(stC0, nc, "ktC0_", [128, HD], BF16, 2, bump=bump); v_r = Ring(stC0, nc, "vC0_", [128, HD], BF16, 2, bump=bump)
                wlib = sb("wlib0", [128, 2], F32, stC0)

                WvA = view(A0, [128, KC, HD], BF16)
                for h in range(H):
                    Wk, wkk = WkC0[1], ("WkC0", 1)
                    Wv, wvk = WvA, "WvA"
                    wload(Wk, wkk, kchunks(w_in[:, K0 + h * HD:K0 + (h + 1) * HD]), bar=("C0" not in NOBAR))
                    wload(Wv, wvk, kchunks(w_in[:, V0 + h * HD:V0 + (h + 1) * HD]), bar=("C0" not in NOBAR))
                    P.op(dv, lambda e: e.memset(Cst[:], 0.0), reads=["Cst"], writes=["Cst"])
                    P.op(dv, lambda e: e.memset(nst[:], 0.0), reads=["nst"], writes=["nst"])
                    for i in range(TPRE):
                        pk_, pkk = PSB.next(); pv_, pvk = PSB.next()
                        for c in range(KC):
                            P.op(pe, lambda e, c=c, pk_=pk_, i=i, Wk=Wk: e.matmul(pk_[:], lhsT=hNTp[:, c, i * 128:(i + 1) * 128], rhs=Wk[:, c, :], start=(c == 0), stop=(c == KC - 1)), reads=[wkk], writes=[pkk])
                        for c in range(KC):
                            P.op(pe, lambda e, c=c, pv_=pv_, i=i, Wv=Wv: e.matmul(pv_[:], lhsT=hNTp[:, c, i * 128:(i + 1) * 128], rhs=Wv[:, c, :], start=(c == 0), stop=(c == KC - 1)), reads=[wvk], writes=[pvk])
                        kt, ktk = kt_r.next(); vt, vk = v_r.next()
                        P.op(dv, lambda e, kt=kt, pk_=pk_, i=i, h=h: e.tensor_scalar(out=kt[:], in0=pk_[:], scalar1=colq[:, i, 12 + h:13 + h], scalar2=KSCALE, op0=ALU.mult, op1=ALU.mult), reads=[pkk], writes=[ktk])
                        P.op(ac, lambda e, vt=vt, pv_=pv_: e.activation(out=vt[:], in_=pv_[:], func=AF.Copy), reads=[pvk], writes=[vk])
                        if h == 0 and i == 0:
                            dbg("c0v", vt[:], [128, HD], cast=True)
                            dbg("c0kt", kt[:], [128, HD], cast=True)
                            dbg("c0wv", Wv[:, 0, :], [128, HD], cast=True)
                            dbg("c0wv15", Wv[:, 15, :], [128, HD], cast=True)
                            dbg("c0hn", hNTp[:, 0, 0:128], [128, 128], cast=True)
                        for c2 in range(4):
                            pkv, pkvk = PSB.next()
                            P.op(pe, lambda e, c2=c2, pkv=pkv, kt=kt, vt=vt: e.matmul(pkv[:], lhsT=kt[:, c2 * 128:(c2 + 1) * 128], rhs=vt[:], start=True, stop=True), reads=[ktk, vk], writes=[pkvk])
                            P.op(dv, lambda e, c2=c2, pkv=pkv, h=h, i=i: e.scalar_tensor_tensor(out=Cst[:, c2, :], in0=Cst[:, c2, :], scalar=wliS[:, i, h:h + 1], in1=pkv[:], op0=ALU.mult, op1=ALU.add), reads=["Cst", pkvk], writes=["Cst"])
                        for c2 in range(4):
                            P.op(pe, lambda e, c2=c2, kt=kt: e.matmul(pss[:, 8 + c2:9 + c2], lhsT=kt[:, c2 * 128:(c2 + 1) * 128], rhs=ones_b[:], start=True, stop=True), reads=[ktk], writes=["pss1"])
                        P.op(dv, lambda e, h=h, i=i: e.scalar_tensor_tensor(out=nst[:], in0=nst[:], scalar=wliS[:, i, h:h + 1], in1=pss[:, 8:12], op0=ALU.mult, op1=ALU.add), reads=["nst", "pss1"], writes=["nst"])
                        P.barrier_all()
                        if h == 0 and i == 0:
                            dbg("c0cst", Cst[:], [128, 4, HD])
                        if h == 1 and i == 0:
                            dbg("wvA", Wv, [128, KC, HD], cast=True)
                        if h == 1 and i == 7:
                            dbg("wvB", Wv, [128, KC, HD], cast=True)
                            dbg("c0cst7b", Cst[:], [128, 4, HD])
                        if h == 0 and i == 7:
                            dbg("c0cst7", Cst[:], [128, 4, HD])
                            dbg("c0v7", vt[:], [128, HD], cast=True)
                            dbg("c0kt7", kt[:], [128, HD], cast=True)
                        if h == 0 and i == 3:
                            dbg("c0cst3", Cst[:], [128, 4, HD])
                    P.op(dv, lambda e: e.tensor_scalar(out=Cst[:], in0=Cst[:], scalar1=flag[:, 0:1], scalar2=None, op0=ALU.mult), reads=["Cst"], writes=["Cst"])
                    P.op(dv, lambda e: e.tensor_scalar(out=nst[:], in0=nst[:], scalar1=flag[:, 0:1], scalar2=None, op0=ALU.mult), reads=["nst"], writes=["nst"])
                    if h == 0:
                        dbg("c0cstF", Cst[:], [128, 4, HD])
                        dbg("wliS", wliS[:], [128, 32, 4])
                        dbg("flag", flag[:], [128, 1])
                    P.dma("sp", lambda e, h=h: e.dma_start(out=cpre_d[h], in_=Cst[:].rearrange("p c v -> p (c v)")), reads=["Cst"], writes=[("cpre", h)])
                    P.dma("sp", lambda e, h=h: e.dma_start(out=npre_d[h], in_=nst[:]), reads=["nst"], writes=[("npre", h)])
                P.barrier_all()
                if maybe_stop("C0"):
                    return nc
            with Scope(bump) as stC:
                Wq = view(M0, [128, KC, HD], BF16); Wk = view(M0 + 16384, [128, KC, HD], BF16); Wv = view(M0 + 32768, [128, KC, HD], BF16)
                Cst = sb("Cst", [128, 4, HD], F32, stC); Cb = sb("Cb", [128, 4, HD], BF16, stC)
                nst = sb("nst", [128, 4], F32, stC); nb = sb("nb", [128, 4], BF16, stC)
                ghb = sb("ghb", [128, HD], F32, stC)
                s_qT = sb("s_qT", [128, H, 4, 128], BF16, stC); s_kt = sb("s_kt", [128, H, HD], BF16, stC)
                s_v = sb("s_v", [128, H, HD], BF16, stC); s_num = sb("s_num", [128, H, HD], F32, stC); s_den = sb("s_den", [128, H], F32, stC)
                qT_r = Ring(stC, nc, "qT", [128, 4, 128], BF16, 2, bump=bump); kT_r = Ring(stC, nc, "kT", [128, 4, 128], BF16, 2, bump=bump)
                kt_r = Ring(stC, nc, "kt", [128, HD], BF16, 2, bump=bump); v_r = Ring(stC, nc, "vt", [128, HD], BF16, 2, bump=bump)
                Mt_r = Ring(stC, nc, "Mt", [128, 128], F32, 2, bump=bump)
                Wt_r = Ring(stC, nc, "Wt", [128, 128], F32, 2, bump=bump); PT_r = Ring(stC, nc, "PT", [128, 128], BF16, 2, bump=bump)
                num_r = Ring(stC, nc, "num", [128, HD], F32, 1, bump=bump); tmpn_r = Ring(stC, nc, "tmpn", [128, HD], F32, 1, bump=bump)
                hbt_r = Ring(stC, nc, "hbt", [128, HD], BF16, 2, bump=bump)
                sm_r = Ring(stC, nc, "smalls", [128, 16], F32, 2, bump=bump)
                junkC = sb("junkC", [128, HD], BF16, stC)

                def finish_tile(h, i, num_ap, numk, den_ap, small, smk):
                    P.op(dv, lambda e: e.tensor_scalar(out=small[:, 4:5], in0=den_ap, scalar1=-1.0, scalar2=None, op0=ALU.mult), reads=[smk], writes=[smk])
                    P.op(dv, lambda e: e.tensor_tensor(out=small[:, 4:5], in0=small[:, 4:5], in1=den_ap, op=ALU.max), reads=[smk], writes=[smk])
                    P.op(dv, lambda e: e.tensor_tensor(out=small[:, 4:5], in0=small[:, 4:5], in1=colq[:, TPRE + i, 8 + h:9 + h], op=ALU.max), reads=[smk], writes=[smk])
                    P.op(dv, lambda e: e.reciprocal(out=small[:, 5:6], in_=small[:, 4:5]), reads=[smk], writes=[smk])
                    P.op(ac, lambda e: e.activation(out=junkC[:], in_=num_ap, func=AF.Square, accum_out=small[:, 6:7]), reads=[numk, smk], writes=["junkC", smk])
                    P.op(dv, lambda e: e.tensor_scalar(out=small[:, 7:8], in0=small[:, 6:7], scalar1=small[:, 5:6], scalar2=small[:, 5:6], op0=ALU.mult, op1=ALU.mult), reads=[smk], writes=[smk])
                    P.op(ac, lambda e: e.activation(out=small[:, 8:9], in_=small[:, 7:8], func=AF.Sqrt, scale=1.0 / HD, bias=EPS), reads=[smk], writes=[smk])
                    P.op(dv, lambda e: e.reciprocal(out=small[:, 9:10], in_=small[:, 8:9]), reads=[smk], writes=[smk])
                    P.op(dv, lambda e: e.tensor_tensor(out=small[:, 10:11], in0=small[:, 9:10], in1=small[:, 5:6], op=ALU.mult), reads=[smk], writes=[smk])
                    hbt, hbk = hbt_r.next()
                    P.op(dv, lambda e: e.scalar_tensor_tensor(out=hbt[:], in0=num_ap, scalar=small[:, 10:11], in1=ghb[:], op0=ALU.mult, op1=ALU.mult), reads=[numk, smk, "ghb"], writes=[hbk])
                    pt, pk = PST.next()
                    for c in range(4):
                        P.op(pe, lambda e, c=c, pt=pt: e.transpose(out=pt[:, c * 128:(c + 1) * 128], in_=hbt[:, c * 128:(c + 1) * 128], identity=identb[:]), reads=[hbk], writes=[pk])
                    P.op(ac, lambda e, pt=pt: e.activation(out=hbT[:, 4 * h:4 * h + 4, i * 128:(i + 1) * 128], in_=pt[:, 0:512].rearrange("p (c t) -> p c t", c=4), func=AF.Copy), reads=[pk], writes=[("hbT", h, i)])

                for h in range(H):
                    wload(Wq, "Wq", kchunks(w_in[:, Q0 + h * HD:Q0 + (h + 1) * HD]), bar=("C1" not in NOBAR))
                    wload(Wk, "Wk", kchunks(w_in[:, K0 + h * HD:K0 + (h + 1) * HD]), bar=("C1" not in NOBAR))
                    wload(Wv, "Wv", kchunks(w_in[:, V0 + h * HD:V0 + (h + 1) * HD]), bar=("C1" not in NOBAR))
                    P.dma("sp", lambda e, h=h: e.dma_start(out=ghb[:], in_=g_head[h * HD:(h + 1) * HD].partition_broadcast(128)), writes=["ghb"])
                    P.dma("sp", lambda e, h=h: e.dma_start(out=Cst[:].rearrange("p c v -> p (c v)"), in_=cpre_d[h]), writes=["Cst"])
                    P.dma("sp", lambda e, h=h: e.dma_start(out=nst[:], in_=npre_d[h]), writes=["nst"])
                    P.op(ac, lambda e: e.activation(out=Cb[:], in_=Cst[:], func=AF.Copy), reads=["Cst"], writes=["Cb"])
                    P.op(ac, lambda e: e.activation(out=nb[:], in_=nst[:], func=AF.Copy), reads=["nst"], writes=["nb"])
                    if h == 0:
                        dbg("cst0", Cst[:], [128, 4, HD])
                    def part1(i, h=h):
                            samp = (i == TOWN - 1)
                            ci = TPRE + i
                            if samp:
                                qT, qk = s_qT[:, h], ("s_qT", h)
                                ktl, ktk = s_kt[:, h], ("s_kt", h)
                                vt, vk = s_v[:, h], ("s_v", h)
                            else:
                                t_, qk = qT_r.next(); qT = t_[:]
                                t_, ktk = kt_r.next(); ktl = t_[:]
                                t_, vk = v_r.next(); vt = t_[:]
                            t_, kTk = kT_r.next(); kT = t_[:]
                            pq, pqk = PSB.next()
                            for cc in range(4):
                                for c in range(KC):
                                    P.op(pe, lambda e, c=c, cc=cc, pq=pq, i=i: e.matmul(pq[:, cc * 128:(cc + 1) * 128], lhsT=Wq[:, c, cc * 128:(cc + 1) * 128], rhs=hNTo[:, c, i * 128:(i + 1) * 128], start=(c == 0), stop=(c == KC - 1)), reads=["Wq"], writes=[pqk])
                            P.op(ac, lambda e, pq=pq, qT=qT: e.activation(out=qT.rearrange("p c t -> p (c t)"), in_=pq[:], func=AF.Copy), reads=[pqk], writes=[qk])
                            pkT, pkTk = PSB.next()
                            for cc in range(4):
                                for c in range(KC):
                                    P.op(pe, lambda e, c=c, cc=cc, pkT=pkT, i=i: e.matmul(pkT[:, cc * 128:(cc + 1) * 128], lhsT=Wk[:, c, cc * 128:(cc + 1) * 128], rhs=hNTo[:, c, i * 128:(i + 1) * 128], start=(c == 0), stop=(c == KC - 1)), reads=["Wk"], writes=[pkTk])
                            P.op(dv, lambda e, pkT=pkT, kT=kT: e.tensor_scalar(out=kT.rearrange("p c t -> p (c t)"), in0=pkT[:], scalar1=KSCALE, scalar2=None, op0=ALU.mult), reads=[pkTk], writes=[kTk])
                            pk_, pkk = PSB.next()
                            for c in range(KC):
                                P.op(pe, lambda e, c=c, pk_=pk_, i=i: e.matmul(pk_[:], lhsT=hNTo[:, c, i * 128:(i + 1) * 128], rhs=Wk[:, c, :], start=(c == 0), stop=(c == KC - 1)), reads=["Wk"], writes=[pkk])
                            P.op(dv, lambda e, pk_=pk_, ktl=ktl, ci=ci, h=h: e.tensor_scalar(out=ktl, in0=pk_[:], scalar1=colq[:, ci, 12 + h:13 + h], scalar2=KSCALE, op0=ALU.mult, op1=ALU.mult), reads=[pkk], writes=[ktk])
                            pv_, pvk = PSB.next()
                            for c in range(KC):
                                P.op(pe, lambda e, c=c, pv_=pv_, i=i: e.matmul(pv_[:], lhsT=hNTo[:, c, i * 128:(i + 1) * 128], rhs=Wv[:, c, :], start=(c == 0), stop=(c == KC - 1)), reads=["Wv"], writes=[pvk])
                            P.op(ac, lambda e, pv_=pv_, vt=vt: e.activation(out=vt, in_=pv_[:], func=AF.Copy), reads=[pvk], writes=[vk])
                            pS, pSk = PSB.next()
                            for c in range(4):
                                P.op(pe, lambda e, c=c, pS=pS, kT=kT, qT=qT: e.matmul(pS[:, 0:128], lhsT=kT[:, c, :], rhs=qT[:, c, :], start=(c == 0), stop=(c == 3)), reads=[kTk, qk], writes=[pSk])
                            Mt, Mtk = Mt_r.next()
                            P.op(ac, lambda e, Mt=Mt, ci=ci, h=h: e.activation(out=Mt[:], in_=maskP[:], func=AF.Identity, bias=colq[:, ci, 16 + h:17 + h], scale=0.0), reads=[], writes=[Mtk])
                            P.op(pe, lambda e, pS=pS, Mt=Mt: e.transpose(out=pS[:, 128:256], in_=Mt[:], identity=identf[:]), reads=[Mtk], writes=[pSk])
                            Wt, Wtk = Wt_r.next(); PT, PTk = PT_r.next()
                            msk = maskS if samp else maskP
                            P.op(dv, lambda e, Wt=Wt, pS=pS, msk=msk: e.tensor_tensor(out=Wt[:], in0=pS[:, 128:256], in1=msk[:], op=ALU.add), reads=[pSk], writes=[Wtk])
                            P.op(ac, lambda e, Wt=Wt, ci=ci, h=h: e.activation(out=Wt[:], in_=Wt[:], func=AF.Exp, bias=colq[:, ci, h:h + 1], scale=1.0), reads=[Wtk], writes=[Wtk])
                            P.op(dv, lambda e, Wt=Wt, PT=PT, pS=pS: e.tensor_tensor(out=PT[:], in0=pS[:, 0:128], in1=Wt[:], op=ALU.mult), reads=[pSk, Wtk], writes=[PTk])
                            return dict(i=i, samp=samp, ci=ci, qT=qT, qk=qk, ktl=ktl, ktk=ktk, vt=vt, vk=vk, PT=PT, PTk=PTk)

                    def part2(cx, h=h):
                            i, samp, ci, qT, qk, ktl, ktk, vt, vk, PT, PTk = (cx[k_] for k_ in ('i', 'samp', 'ci', 'qT', 'qk', 'ktl', 'ktk', 'vt', 'vk', 'PT', 'PTk'))
                            pn, pnk = PSB.next()
                            P.op(pe, lambda e, pn=pn, PT=PT, vt=vt: e.matmul(pn[:], lhsT=PT[:], rhs=vt, start=True, stop=True), reads=[PTk, vk], writes=[pnk])
                            P.op(pe, lambda e, PT=PT: e.matmul(pss[:, 16:17], lhsT=PT[:], rhs=ones_b[:], start=True, stop=True), reads=[PTk], writes=["pssd"])
                            if samp:
                                P.op(ac, lambda e, pn=pn, h=h: e.activation(out=s_num[:, h, :], in_=pn[:], func=AF.Copy), reads=[pnk], writes=[("s_num", h)])
                                P.op(ac, lambda e, h=h: e.activation(out=s_den[:, h:h + 1], in_=pss[:, 16:17], func=AF.Copy), reads=["pssd"], writes=[("s_den", h)])
                                return
                            pi_, pik = PSB.next()
                            for c in range(4):
                                P.op(pe, lambda e, c=c, pi_=pi_, qT=qT: e.matmul(pi_[:], lhsT=qT[:, c, :], rhs=Cb[:, c, :], start=(c == 0), stop=(c == 3)), reads=[qk, "Cb"], writes=[pik])
                            for c in range(4):
                                P.op(pe, lambda e, c=c, qT=qT: e.matmul(pss[:, 17:18], lhsT=qT[:, c, :], rhs=nb[:, c:c + 1], start=(c == 0), stop=(c == 3)), reads=[qk, "nb"], writes=["pssd"])
                            small, smk = sm_r.next()
                            P.op(ac, lambda e, small=small: e.activation(out=small[:, 0:2], in_=pss[:, 16:18], func=AF.Copy), reads=["pssd"], writes=[smk])
                            tmpn, tmpk = tmpn_r.next(); num, numk = num_r.next()
                            P.op(ac, lambda e, tmpn=tmpn, pi_=pi_, ci=ci, h=h: e.activation(out=tmpn[:], in_=pi_[:], func=AF.Copy, scale=colq[:, ci, 4 + h:5 + h]), reads=[pik], writes=[tmpk])
                            P.op(dv, lambda e, num=num, tmpn=tmpn, pn=pn: e.tensor_tensor(out=num[:], in0=tmpn[:], in1=pn[:], op=ALU.add), reads=[tmpk, pnk], writes=[numk])
                            P.op(dv, lambda e, small=small, ci=ci, h=h: e.scalar_tensor_tensor(out=small[:, 3:4], in0=small[:, 1:2], scalar=colq[:, ci, 4 + h:5 + h], in1=small[:, 0:1], op0=ALU.mult, op1=ALU.add), reads=[smk], writes=[smk])
                            finish_tile(h, i, num[:], numk, small[:, 3:4], small, smk)
                            for c2 in range(4):
                                pkv, pkvk = PSB.next()
                                P.op(pe, lambda e, c2=c2, pkv=pkv, ktl=ktl, vt=vt: e.matmul(pkv[:], lhsT=ktl[:, c2 * 128:(c2 + 1) * 128], rhs=vt, start=True, stop=True), reads=[ktk, vk], writes=[pkvk])
                                P.op(dv, lambda e, c2=c2, pkv=pkv, h=h, i=i: e.scalar_tensor_tensor(out=Cst[:, c2, :], in0=Cst[:, c2, :], scalar=wliS[:, 8 + i, h:h + 1], in1=pkv[:], op0=ALU.mult, op1=ALU.add), reads=["Cst", pkvk], writes=["Cst"])
                            for c2 in range(4):
                                P.op(pe, lambda e, c2=c2, ktl=ktl: e.matmul(pss[:, 24 + c2:25 + c2], lhsT=ktl[:, c2 * 128:(c2 + 1) * 128], rhs=ones_b[:], start=True, stop=True), reads=[ktk], writes=["pssn"])
                            P.op(dv, lambda e, h=h, i=i: e.scalar_tensor_tensor(out=nst[:], in0=nst[:], scalar=wliS[:, 8 + i, h:h + 1], in1=pss[:, 24:28], op0=ALU.mult, op1=ALU.add), reads=["nst", "pssn"], writes=["nst"])
                            if i < TOWN - 2:
                                P.op(ac, lambda e: e.activation(out=Cb[:], in_=Cst[:], func=AF.Copy), reads=["Cst"], writes=["Cb"])
                                P.op(ac, lambda e: e.activation(out=nb[:], in_=nst[:], func=AF.Copy), reads=["nst"], writes=["nb"])

                    pend = None
                    for i in range(TOWN):
                        cx = part1(i)
                        if pend is not None:
                            part2(pend)
                        pend = cx
                    part2(pend)
                    P.dma("sp", lambda e, h=h: e.dma_start(out=Cp_d[h].rearrange("(c p) v -> p c v", p=128), in_=Cst[:]), reads=["Cst"], is_output=True)
                    with nc.allow_non_contiguous_dma(reason="n out"):
                        P.dma("sp", lambda e, h=h: e.dma_start(out=np_d[h].rearrange("(c p) -> p c", p=128), in_=nst[:], allow_slow_non_contiguous=True), reads=["nst"], is_output=True)
                P.barrier_all()
                if maybe_stop("C1"):
                    return nc

                Cj_v = [view(M0 + k * 8192, [128, 4, HD], F32) for k in range(3)]
                Z_v = [view(M0 + 24576 + k * 3968, [128, 4, 248], F32) for k in range(2)]
                o2 = M0 + 24576 + 2 * 3968
                ktj_v = [view(o2 + k * 1024, [128, HD], BF16) for k in range(2)]
                n0 = view(o2 + 2048, [128, HD], F32)
                nout = view(o2 + 4096, [128, HD], F32)
                n0T = view(o2 + 6144, [128, 4, 64], F32)
                nnT = view(o2 + 7168, [128, 4, 64], F32)
                P.dma("sp", lambda e: e.dma_start(out=n0[0:64, :], in_=sn), writes=["n0"])
                for c in range(4):
                    P.op(pe, lambda e, c=c: e.transpose(out=pss[:, 64 * c:64 * c + 64], in_=n0[0:64, c * 128:(c + 1) * 128], identity=identf[0:64, 0:64]), reads=["n0"], writes=["pss"])
                P.op(dv, lambda e: e.tensor_copy(out=n0T.rearrange("p c j -> p (c j)"), in_=pss[:, 0:256]), reads=["pss"], writes=["n0T"])
                for k in range(2):
                    P.op(dv, lambda e, k=k: e.memset(Z_v[k], 0.0), writes=[("Z", k)])
                ci = TPRE + TOWN - 1
                seq = [(h, j) for h in range(H) for j in range(16)]

                def c2_load(idx):
                    h, j = seq[idx]
                    P.dma("sp", lambda e: e.dma_start(out=Cj_v[idx % 3], in_=sC[j, h].rearrange("(c p) v -> p c v", p=128)), writes=[("Cj", idx % 3)])
                c2_load(0); c2_load(1)
                for idx, (h, j) in enumerate(seq):
                    if idx + 2 < len(seq):
                        c2_load(idx + 2)
                    Cj, Cjk = Cj_v[idx % 3], ("Cj", idx % 3)
                    Z, Zk = Z_v[idx % 2], ("Z", idx % 2)
                    P.op(dv, lambda e, Z=Z, j=j, h=h: e.tensor_copy(out=Z[:, :, 120:128], in_=s_qT[:, h, :, 8 * j:8 * j + 8]), reads=[Zk], writes=[Zk])
                    for c in range(4):
                        P.op(pe, lambda e, c=c, Z=Z, Cj=Cj, j=j: e.matmul(pacc[:], lhsT=Z[:, c, 120 - 8 * j:248 - 8 * j], rhs=Cj[:, c, :], start=(j == 0 and c == 0), stop=(j == 15 and c == 3)), reads=[Zk, Cjk], writes=["pacc"])
                    for c in range(4):
                        P.op(pe, lambda e, c=c, Z=Z, j=j, h=h: e.matmul(pss[:, 320 + j:321 + j], lhsT=Z[:, c, 120 - 8 * j:248 - 8 * j], rhs=n0T[:, c, 4 * j + h:4 * j + h + 1], start=(c == 0), stop=(c == 3)), reads=[Zk, "n0T"], writes=["pssd2"])
                    ktj, ktjk = ktj_v[idx % 2], ("ktj", idx % 2)
                    P.op(dv, lambda e, ktj=ktj, j=j, h=h: e.tensor_scalar(out=ktj, in0=s_kt[:, h, :], scalar1=blk[:, j:j + 1], scalar2=None, op0=ALU.mult), reads=[ktjk], writes=[ktjk])
                    for c2 in range(4):
                        pkv, pkvk = PSB.next()
                        P.op(pe, lambda e, c2=c2, pkv=pkv, ktj=ktj, h=h: e.matmul(pkv[:], lhsT=ktj[:, c2 * 128:(c2 + 1) * 128], rhs=s_v[:, h, :], start=True, stop=True), reads=[ktjk], writes=[pkvk])
                        P.op(dv, lambda e, c2=c2, pkv=pkv, Cj=Cj, j=j, h=h: e.scalar_tensor_tensor(out=Cj[:, c2, :], in0=Cj[:, c2, :], scalar=wliS[:, 16 + j, h:h + 1], in1=pkv[:], op0=ALU.mult, op1=ALU.add), reads=[Cjk, pkvk], writes=[Cjk])
                    for c2 in range(4):
                        P.op(pe, lambda e, c2=c2, ktj=ktj: e.matmul(pss[:, 304 + c2:305 + c2], lhsT=ktj[:, c2 * 128:(c2 + 1) * 128], rhs=ones_b[:], start=True, stop=True), reads=[ktjk], writes=["pssn2"])
                    P.op(dv, lambda e, j=j, h=h: e.scalar_tensor_tensor(out=nnT[:, :, 4 * j + h], in0=n0T[:, :, 4 * j + h], scalar=wliS[:, 16 + j, h:h + 1], in1=pss[:, 304:308], op0=ALU.mult, op1=ALU.add), reads=["n0T", "pssn2", "nnT"], writes=["nnT"])
                    P.dma(C2Q, lambda e, Cj=Cj, j=j, h=h: e.dma_start(out=Cs_d[j, h].rearrange("(c p) v -> p c v", p=128), in_=Cj), reads=[Cjk], is_output=True)
                    if j == 15:
                        small, smk = sm_r.next()
                        P.op(dv, lambda e, small=small: e.tensor_reduce(out=small[:, 1:2], in_=pss[:, 320:336], axis=AX.X, op=ALU.add), reads=["pssd2"], writes=[smk])
                        tmpn, tmpk = tmpn_r.next(); num, numk = num_r.next()
                        P.op(ac, lambda e, tmpn=tmpn, h=h: e.activation(out=tmpn[:], in_=pacc[:], func=AF.Copy, scale=colq[:, ci, 4 + h:5 + h]), reads=["pacc"], writes=[tmpk])
                        P.op(dv, lambda e, num=num, tmpn=tmpn, h=h: e.tensor_tensor(out=num[:], in0=tmpn[:], in1=s_num[:, h, :], op=ALU.add), reads=[tmpk], writes=[numk])
                        P.op(dv, lambda e, small=small, h=h: e.scalar_tensor_tensor(out=small[:, 3:4], in0=small[:, 1:2], scalar=colq[:, ci, 4 + h:5 + h], in1=s_den[:, h:h + 1], op0=ALU.mult, op1=ALU.add), reads=[smk], writes=[smk])
                        P.dma("sp", lambda e, h=h: e.dma_start(out=ghb[:], in_=g_head[h * HD:(h + 1) * HD].partition_broadcast(128)), writes=["ghb"])
                        finish_tile(h, TOWN - 1, num[:], numk, small[:, 3:4], small, smk)
                for c in range(4):
                    P.op(pe, lambda e, c=c: e.transpose(out=pss[0:64, 128 * c:128 * c + 128], in_=nnT[:, c, :], identity=identf[:]), reads=["nnT"], writes=["pss", "pssd2", "pssn2"])
                P.op(dv, lambda e: e.tensor_copy(out=nout[0:64, :], in_=pss[0:64, :]), reads=["pss"], writes=["nout"])
                P.dma("sp", lambda e: e.dma_start(out=ns_d, in_=nout[0:64, :]), reads=["nout"], is_output=True)
                P.barrier_all()
                if maybe_stop("C2"):
                    return nc
            dbg("hbT", hbT, [128, KC, NOWN])
            with Scope(bump) as stP:
                pooledT = view(M0, [128, 8, NOWN], BF16)
                hist = view(M0 + 18432, [128, 8, 240], F32)
                utok = view(M0 + 26112, [128, PW], F32); utoks = view(M0 + 30208, [128, PW], F32)
                hld = Ring(stP, nc, "hld", [120, PW], F32, 1, bump=bump)
                full = sb("full", [128, 1040], F32, stP); wsA = sb("wsA", [128, 1040], F32, stP); wsB = sb("wsB", [128, 1040], F32, stP)
                fulls = sb("fulls", [128, 16, 23], F32, stP); wsAs = sb("wsAs", [128, 16, 23], F32, stP); wsBs = sb("wsBs", [128, 16, 23], F32, stP)
                tmp16 = sb("tmp16", [128, 16], F32, stP)
                pscale = sb("pscale", [128, 8], F32, stP)
                wu_ring = Ring(stP, nc, "wuP", [128, KC, 128], BF16, 2, bump=bump)
                wut_ring = Ring(stP, nc, "wutP", [128, KC, 512], BF16, 1, bump=bump)
                wp_ring = Ring(stP, nc, "wpP", [128, 2, 256], BF16, 2, bump=bump)
                with nc.allow_non_contiguous_dma(reason="pool scale"):
                    P.dma("sp", lambda e: e.dma_start(out=pscale[:], in_=pool_scale.rearrange("(c p) -> p c", p=128), allow_slow_non_contiguous=True), writes=["pscale"])
                for half in range(2):
                    hl, hlk = hld.next()
                    P.dma("sp", lambda e, hl=hl, half=half: e.dma_start(out=hl[:], in_=spool[8 * half:8 * half + 8].rearrange("j r d -> (j r) d")), writes=[hlk])
                    for fc in range(8):
                        pt_, ptk = PSB.next()
                        P.op(pe, lambda e, fc=fc, hl=hl, pt_=pt_: e.transpose(out=pt_[:, 0:120], in_=hl[:, fc * 128:(fc + 1) * 128], identity=identf[0:120, 0:120]), reads=[hlk], writes=[ptk])
                        P.op(ac, lambda e, fc=fc, half=half, pt_=pt_: e.activation(out=hist[:, fc, 120 * half:120 * half + 120], in_=pt_[:, 0:120], func=AF.Copy), reads=[ptk], writes=[("hist", fc, half)])
                for cb in range(2):
                    wut, wutk = wut_ring.next()
                    wload(wut[:], wutk, kchunks(w_in[:, U0 + cb * 512:U0 + (cb + 1) * 512]), bar=("P" not in NOBAR))
                    for (ti, dst, dk) in [(7, utok, "utok"), (8, utoks, "utoks")]:
                        pu, puk = PSB.next()
                        for c in range(KC):
                            P.op(pe, lambda e, c=c, pu=pu, ti=ti, wut=wut: e.matmul(pu[:], lhsT=hNTo[:, c, ti * 128:(ti + 1) * 128], rhs=wut[:, c, :], start=(c == 0), stop=(c == KC - 1)), reads=[wutk], writes=[puk])
                        P.op(ac, lambda e, pu=pu, dst=dst, cb=cb: e.activation(out=dst[:, cb * 512:(cb + 1) * 512], in_=pu[:], func=AF.Copy), reads=[puk], writes=[(dk, cb)])
                P.dma("sp", lambda e: e.dma_start(out=poolp_d, in_=utok[113:128, :]), reads=[("utok", 0), ("utok", 1)], is_output=True)
                for j in range(16):
                    P.dma("sp", lambda e, j=j: e.dma_start(out=pools_d[j, 7:15, :], in_=utoks[8 * j:8 * j + 8, :]), reads=[("utoks", 0), ("utoks", 1)], is_output=True)
                P.dma("sp", lambda e: e.dma_start(out=pools_d[:, 0:7, :], in_=spool[:, 8:15, :]), is_output=True)
                wu_t = {}

                def pool_load(fc):
                    wu, wk = wu_ring.next()
                    wload(wu[:], wk, kchunks(w_in[:, U0 + fc * 128:U0 + (fc + 1) * 128]), bar=("P" not in NOBAR))
                    wu_t[fc] = (wu, wk)
                pool_load(0)
                for fc in range(8):
                    if fc + 1 < 8:
                        pool_load(fc + 1)
                    g = fc // 2
                    w = 2 << g
                    wu, wk = wu_t[fc]
                    P.op(dv, lambda e, fc=fc: e.tensor_copy(out=full[:, 0:16], in_=uhist[:, fc, :]), reads=["full"], writes=["full"])
                    P.op(dv, lambda e, fc=fc: e.tensor_copy(out=fulls[:, :, 0:15], in_=hist[:, fc, :].rearrange("p (j r) -> p j r", r=15)), reads=[("hist", fc, 0), ("hist", fc, 1), "fulls"], writes=["fulls"])
                    for (t0, n) in TGS:
                        pu, puk = PSB.next()
                        for c in range(KC):
                            P.op(pe, lambda e, c=c, pu=pu, t0=t0, n=n, wu=wu: e.matmul(pu[:, 0:n], lhsT=wu[:, c, :], rhs=hNTo[:, c, t0:t0 + n], start=(c == 0), stop=(c == KC - 1)), reads=[wk], writes=[puk])
                        if t0 < 1024:
                            P.op(ac, lambda e, pu=pu, t0=t0, n=n: e.activation(out=full[:, 16 + t0:16 + t0 + n], in_=pu[:, 0:n], func=AF.Copy), reads=[puk, "full"], writes=["full"])
                        else:
                            P.op(ac, lambda e, pu=pu: e.activation(out=fulls[:, :, 15:23], in_=pu[:, 0:128].rearrange("p (j r) -> p j r", r=8), func=AF.Copy), reads=[puk, "fulls"], writes=["fulls"])
                    src, srck, srcs, srcsk = full, "full", fulls, "fulls"
                    bufs = [(wsA, "wsA", wsAs, "wsAs"), (wsB, "wsB", wsBs, "wsBs")]
                    for k in range(g + 1):
                        sh = 1 << k
                        dst, dstk, dsts, dstsk = bufs[k % 2]
                        P.op(dv, lambda e, src=src, dst=dst, sh=sh: e.tensor_tensor(out=dst[:, sh:1040], in0=src[:, sh:1040], in1=src[:, 0:1040 - sh], op=ALU.add), reads=[srck, dstk], writes=[dstk])
                        P.op(dv, lambda e, srcs=srcs, dsts=dsts, sh=sh: e.tensor_tensor(out=dsts[:, :, sh:23], in0=srcs[:, :, sh:23], in1=srcs[:, :, 0:23 - sh], op=ALU.add), reads=[srcsk, dstsk], writes=[dstsk])
                        src, srck, srcs, srcsk = dst, dstk, dsts, dstsk
                    P.op(dv, lambda e, src=src, fc=fc, w=w: e.scalar_tensor_tensor(out=pooledT[:, fc, 0:1024], in0=src[:, 16:1040], scalar=1.0 / w, in1=full[:, 16:1040], op0=ALU.mult, op1=ALU.subtract), reads=[srck, "full"], writes=[("pooledT", fc)])
                    P.op(dv, lambda e, src=src, g=g: e.tensor_tensor(out=tmp16[:], in0=src[:, 16:32], in1=rcb[:, g, :], op=ALU.mult), reads=[srck, "tmp16"], writes=["tmp16"])
                    P.op(dv, lambda e, fc=fc: e.tensor_tensor(out=pooledT[:, fc, 0:16], in0=tmp16[:], in1=full[:, 16:32], op=ALU.subtract), reads=["tmp16", "full", ("pooledT", fc)], writes=[("pooledT", fc)])
                    P.op(dv, lambda e, srcs=srcs, fc=fc, w=w: e.scalar_tensor_tensor(out=pooledT[:, fc, 1024:1152].rearrange("p (j r) -> p j r", r=8), in0=srcs[:, :, 15:23], scalar=1.0 / w, in1=fulls[:, :, 15:23], op0=ALU.mult, op1=ALU.subtract), reads=[srcsk, "fulls", ("pooledT", fc)], writes=[("pooledT", fc)])
                for g in range(4):
                    wp, wpk = wp_ring.next()
                    wload(wp[:], wpk, w_pool[g].rearrange("(c p) d -> p c d", p=128), bar=("P" not in NOBAR))
                    for dc in range(2):
                        for (t0, n) in TGS:
                            pm, pmk = PSB.next()
                            for cc in range(2):
                                P.op(pe, lambda e, cc=cc, pm=pm, wp=wp, dc=dc, g=g, t0=t0, n=n: e.matmul(pm[:, 0:n], lhsT=wp[:, cc, dc * 128:(dc + 1) * 128], rhs=pooledT[:, 2 * g + cc, t0:t0 + n], start=(cc == 0), stop=(cc == 1)), reads=[wpk, ("pooledT", 2 * g), ("pooledT", 2 * g + 1)], writes=[pmk])
                            P.op(ac, lambda e, pm=pm, g=g, dc=dc, t0=t0, n=n: e.activation(out=aT[:, 2 * g + dc, t0:t0 + n], in_=pm[:, 0:n], func=AF.Copy, scale=pscale[:, 2 * g + dc:2 * g + dc + 1]), reads=[pmk, "pscale"], writes=[("aT", 2 * g + dc, t0)])
                P.barrier_all()
                if maybe_stop("Pool"):
                    return nc
        dbg("aT", aT, [128, 8, NOWN])
        with Scope(bump) as stDD:
            wo_r = Ring(stDD, nc, "woD", [128, KC, 128], BF16, 2, bump=bump)
            wga_r = Ring(stDD, nc, "wga", [128, KC, 128], BF16, 2, bump=bump); wgb_r = Ring(stDD, nc, "wgb", [128, KC, 128], BF16, 2, bump=bump)
            wpa_r = Ring(stDD, nc, "wpa", [128, 8, 128], BF16, 2, bump=bump); wpb_r = Ring(stDD, nc, "wpb", [128, KC, 128], BF16, 2, bump=bump)
            sg_r = Ring(stDD, nc, "sg", [128, 512], F32, 3, bump=bump); t1_r = Ring(stDD, nc, "t1", [128, 512], F32, 2, bump=bump); t2_r = Ring(stDD, nc, "t2", [128, 512], F32, 2, bump=bump)
            wo_t = {}

            def d0_load(oc):
                wo, wok = wo_r.next()
                wload(wo[:], wok, kchunks(w_in[:, O0 + oc * 128:O0 + (oc + 1) * 128]), bar=("D0" not in NOBAR))
                wo_t[oc] = (wo, wok)
            d0_load(0)
            for oc in range(KC):
                if oc + 1 < KC:
                    d0_load(oc + 1)
                wo, wok = wo_t[oc]
                for (t0, n) in TGS:
                    po, pok = PSB.next()
                    for c in range(KC):
                        P.op(pe, lambda e, c=c, po=po, wo=wo, t0=t0, n=n: e.matmul(po[:, 0:n], lhsT=wo[:, c, :], rhs=hNTo[:, c, t0:t0 + n], start=(c == 0), stop=(c == KC - 1)), reads=[wok], writes=[pok])
                    sg, sgk = sg_r.next()
                    P.op(ac, lambda e, sg=sg, po=po, n=n: e.activation(out=sg[:, 0:n], in_=po[:, 0:n], func=AF.Sigmoid), reads=[pok], writes=[sgk])
                    P.op(dv, lambda e, sg=sg, oc=oc, t0=t0, n=n: e.tensor_tensor(out=hbT[:, oc, t0:t0 + n], in0=hbT[:, oc, t0:t0 + n], in1=sg[:, 0:n], op=ALU.mult), reads=[sgk], writes=[("hbTg", oc, t0)])
            P.barrier_all()
            if maybe_stop("D0"):
                return nc
            w_t = {}

            def d1_load(cb):
                wga, wgak = wga_r.next(); wgb, wgbk = wgb_r.next(); wpa, wpak = wpa_r.next(); wpb, wpbk = wpb_r.next()
                wload(wga[:], wgak, kchunks(w_in[:, GA0 + cb * 128:GA0 + (cb + 1) * 128]), bar=("D1" not in NOBAR))
                wload(wpa[:], wpak, kchunks(w_pa[:, cb * 128:(cb + 1) * 128]), bar=("D1" not in NOBAR))
                wload(wgb[:], wgbk, kchunks(w_in[:, GB0 + cb * 128:GB0 + (cb + 1) * 128]), bar=("D1" not in NOBAR))
                wload(wpb[:], wpbk, kchunks(w_pb[:, cb * 128:(cb + 1) * 128]), bar=("D1" not in NOBAR))
                w_t[cb] = (wga, wgak, wgb, wgbk, wpa, wpak, wpb, wpbk)
            d1_load(0)
            for cb in range(KC):
                if cb + 1 < KC:
                    d1_load(cb + 1)
                wga, wgak, wgb, wgbk, wpa, wpak, wpb, wpbk = w_t[cb]
                for (t0, n) in TGS:
                    pga, pgak = PSB.next()
                    for c in range(KC):
                        P.op(pe, lambda e, c=c, pga=pga, wga=wga, t0=t0, n=n: e.matmul(pga[:, 0:n], lhsT=wga[:, c, :], rhs=hNTo[:, c, t0:t0 + n], start=(c == 0), stop=(c == KC - 1)), reads=[wgak], writes=[pgak])
                    pa_, pak = PSB.next()
                    for c in range(8):
                        P.op(pe, lambda e, c=c, pa_=pa_, wpa=wpa, t0=t0, n=n: e.matmul(pa_[:, 0:n], lhsT=wpa[:, c, :], rhs=aT[:, c, t0:t0 + n], start=(c == 0), stop=(c == 7)), reads=[wpak], writes=[pak])
                    sga, sgak = sg_r.next()
                    P.op(ac, lambda e, sga=sga, pga=pga, n=n: e.activation(out=sga[:, 0:n], in_=pga[:, 0:n], func=AF.Sigmoid), reads=[pgak], writes=[sgak])
                    t1, t1k = t1_r.next()
                    P.op(dv, lambda e, t1=t1, sga=sga, pa_=pa_, n=n: e.tensor_tensor(out=t1[:, 0:n], in0=sga[:, 0:n], in1=pa_[:, 0:n], op=ALU.mult), reads=[sgak, pak], writes=[t1k])
                    pgb, pgbk = PSB.next()
                    for c in range(KC):
                        P.op(pe, lambda e, c=c, pgb=pgb, wgb=wgb, t0=t0, n=n: e.matmul(pgb[:, 0:n], lhsT=wgb[:, c, :], rhs=hNTo[:, c, t0:t0 + n], start=(c == 0), stop=(c == KC - 1)), reads=[wgbk], writes=[pgbk])
                    pb_, pbk = PSB.next()
                    for c in range(KC):
                        P.op(pe, lambda e, c=c, pb_=pb_, wpb=wpb, t0=t0, n=n: e.matmul(pb_[:, 0:n], lhsT=wpb[:, c, :], rhs=hbT[:, c, t0:t0 + n], start=(c == 0), stop=(c == KC - 1)), reads=[wpbk], writes=[pbk])
                    sgb, sgbk = sg_r.next()
                    P.op(ac, lambda e, sgb=sgb, pgb=pgb, n=n: e.activation(out=sgb[:, 0:n], in_=pgb[:, 0:n], func=AF.Sigmoid), reads=[pgbk], writes=[sgbk])
                    t2, t2k = t2_r.next()
                    P.op(dv, lambda e, t2=t2, sgb=sgb, pb_=pb_, n=n: e.tensor_tensor(out=t2[:, 0:n], in0=sgb[:, 0:n], in1=pb_[:, 0:n], op=ALU.mult), reads=[sgbk, pbk], writes=[t2k])
                    P.op(dv, lambda e, t1=t1, t2=t2, cb=cb, t0=t0, n=n: e.tensor_tensor(out=mixT[:, cb, t0:t0 + n], in0=t1[:, 0:n], in1=t2[:, 0:n], op=ALU.add), reads=[t1k, t2k], writes=[("mixT", cb, t0)])
            P.barrier_all()
            if maybe_stop("D"):
                return nc
        dbg("mixT", mixT, [128, KC, NOWN])
        with Scope(bump) as stE:
            woE = Ring(stE, nc, "woE", [128, KC, 512], BF16, 2, bump=bump)
            P.dma("sp", lambda e: e.dma_start(out=yacc, in_=xo.rearrange("(t p) d -> p t d", p=128)), writes=["yacc"])
            we_t = {}

            def e_load(cb):
                wo, wok = woE.next()
                wload(wo[:], wok, kchunks(w_out[:, cb * 512:(cb + 1) * 512]), bar=("E" not in NOBAR))
                we_t[cb] = (wo, wok)
            e_load(0)
            for cb in range(4):
                if cb + 1 < 4:
                    e_load(cb + 1)
                wo, wok = we_t[cb]
                for t in range(TOWN):
                    px, pxk = PSB.next()
                    for c in range(KC):
                        P.op(pe, lambda e, c=c, px=px, wo=wo, t=t: e.matmul(px[:], lhsT=mixT[:, c, t * 128:(t + 1) * 128], rhs=wo[:, c, :], start=(c == 0), stop=(c == KC - 1)), reads=[wok], writes=[pxk])
                    P.op(dv, lambda e, px=px, t=t, cb=cb: e.tensor_tensor(out=yacc[:, t, cb * 512:(cb + 1) * 512], in0=yacc[:, t, cb * 512:(cb + 1) * 512], in1=px[:], op=ALU.add), reads=[pxk, "yacc"], writes=[("yacc", t, cb)])
            P.barrier_all()
            if maybe_stop("E"):
                return nc
        dbg("x1", yacc, [128, TOWN, D])
        comb = view(A0 + 9216, [128, TOWN, NE], F32)
        hT_v = [view(A0 + k * 4608, [128, 2, NOWN], BF16) for k in range(2)]
        with Scope(bump) as stF0:
            hn_ring = Ring(stF0, nc, "hnF", [128, D], BF16, 2, bump=bump)
            ssq = sb("ssqF", [128, 4], F32, stF0); junk = sb("junkF", [128, D], BF16, stF0)
            Wr = sb("Wr", [128, KC, 36], BF16, stF0); bbc = sb("bbc", [128, 36], F32, stF0)
            L_r = Ring(stF0, nc, "Lr", [128, 36], F32, 2, bump=bump); elm_r = Ring(stF0, nc, "elm", [128, 32], F32, 2, bump=bump)
            elm2_r = Ring(stF0, nc, "elm2", [128, 32], F32, 2, bump=bump); oh1_r = Ring(stF0, nc, "oh1", [128, 32], F32, 2, bump=bump); oh2_r = Ring(stF0, nc, "oh2", [128, 32], F32, 2, bump=bump)
            s_r = Ring(stF0, nc, "rs", [128, 16], F32, 2, bump=bump); j4 = sb("j4", [128, 4], F32, stF0)
            P.dma("sp", lambda e: e.dma_start(out=gbc[:], in_=g_ffn.partition_broadcast(128)), writes=["gbc"])
            with nc.allow_non_contiguous_dma(reason="router weights"):
                wload(Wr[:, :, 0:4], "Wr0", kchunks(w_rg), True, bar=("F0" not in NOBAR))
                wload(Wr[:, :, 4:36], "Wr1", kchunks(w_re), True, bar=("F0" not in NOBAR))
            P.dma("sp", lambda e: e.dma_start(out=bbc[:, 0:4], in_=b_rg.partition_broadcast(128)), writes=["bbc0"])
            P.dma("sp", lambda e: e.dma_start(out=bbc[:, 4:36], in_=b_re.partition_broadcast(128)), writes=["bbc1"])
            for t in range(TOWN):
                rmsnorm_T(("sb", yacc[:, t, :], ("yacc_t", t)), hFT, "hFT", t * 128, None, hn_ring, (ssq, junk))
            for t in range(TOWN):
                plg, plk = PSB.next()
                for c in range(KC):
                    P.op(pe, lambda e, c=c, plg=plg, t=t: e.matmul(plg[:, 0:36], lhsT=hFT[:, c, t * 128:(t + 1) * 128], rhs=Wr[:, c, :], start=(c == 0), stop=(c == KC - 1)), reads=["Wr0", "Wr1", ("hFT", t, 0), ("hFT", t, 1)], writes=[plk])
                L, Lk = L_r.next(); elm, ek = elm_r.next(); elm2, e2k = elm2_r.next(); oh1, o1k = oh1_r.next(); oh2, o2k = oh2_r.next(); s, sk = s_r.next()
                P.op(dv, lambda e, L=L, plg=plg: e.tensor_tensor(out=L[:], in0=plg[:, 0:36], in1=bbc[:], op=ALU.add), reads=[plk, "bbc0", "bbc1"], writes=[Lk])
                P.op(dv, lambda e, L=L, s=s: e.tensor_reduce(out=s[:, 0:1], in_=L[:, 0:4], axis=AX.X, op=ALU.max), reads=[Lk], writes=[sk])
                P.op(dv, lambda e, s=s: e.tensor_scalar(out=s[:, 1:2], in0=s[:, 0:1], scalar1=-1.0, scalar2=None, op0=ALU.mult), reads=[sk], writes=[sk])
                P.op(ac, lambda e, L=L, s=s: e.activation(out=j4[:], in_=L[:, 0:4], func=AF.Exp, bias=s[:, 1:2], scale=1.0, accum_out=s[:, 2:3]), reads=[Lk, sk], writes=["j4", sk])
                P.op(dv, lambda e, s=s: e.reciprocal(out=s[:, 3:4], in_=s[:, 2:3]), reads=[sk], writes=[sk])
                P.op(dv, lambda e, L=L, s=s: e.tensor_scalar(out=s[:, 8:12], in0=L[:, 0:4], scalar1=s[:, 0:1], scalar2=-1.0, op0=ALU.is_ge, op1=ALU.add), reads=[Lk, sk], writes=[sk])
                P.op(dv, lambda e, s=s: e.tensor_scalar(out=s[:, 8:12], in0=s[:, 8:12], scalar1=1.0e9, scalar2=None, op0=ALU.mult), reads=[sk], writes=[sk])
                for g in range(4):
                    P.op(dv, lambda e, g=g, L=L, s=s, elm=elm: e.tensor_scalar(out=elm[:, 8 * g:8 * g + 8], in0=L[:, 4 + 8 * g:12 + 8 * g], scalar1=s[:, 8 + g:9 + g], scalar2=None, op0=ALU.add), reads=[Lk, sk, ek], writes=[ek])
                P.op(dv, lambda e, s=s, elm=elm: e.tensor_reduce(out=s[:, 4:5], in_=elm[:], axis=AX.X, op=ALU.max), reads=[ek, sk], writes=[sk])
                P.op(dv, lambda e, s=s, elm=elm, oh1=oh1: e.tensor_scalar(out=oh1[:], in0=elm[:], scalar1=s[:, 4:5], scalar2=None, op0=ALU.is_ge), reads=[ek, sk], writes=[o1k])
                P.op(dv, lambda e, elm=elm, oh1=oh1, elm2=elm2: e.scalar_tensor_tensor(out=elm2[:], in0=oh1[:], scalar=-1.0e9, in1=elm[:], op0=ALU.mult, op1=ALU.add), reads=[o1k, ek], writes=[e2k])
                P.op(dv, lambda e, s=s, elm2=elm2: e.tensor_reduce(out=s[:, 5:6], in_=elm2[:], axis=AX.X, op=ALU.max), reads=[e2k, sk], writes=[sk])
                P.op(dv, lambda e, s=s, elm2=elm2, oh2=oh2: e.tensor_scalar(out=oh2[:], in0=elm2[:], scalar1=s[:, 5:6], scalar2=None, op0=ALU.is_ge), reads=[e2k, sk], writes=[o2k])
                P.op(dv, lambda e, s=s: e.tensor_tensor(out=s[:, 6:7], in0=s[:, 4:5], in1=s[:, 5:6], op=ALU.subtract), reads=[sk], writes=[sk])
                P.op(ac, lambda e, s=s: e.activation(out=s[:, 6:7], in_=s[:, 6:7], func=AF.Sigmoid), reads=[sk], writes=[sk])
                P.op(dv, lambda e, s=s: e.tensor_tensor(out=s[:, 7:8], in0=s[:, 6:7], in1=s[:, 3:4], op=ALU.mult), reads=[sk], writes=[sk])
                P.op(dv, lambda e, s=s: e.tensor_tensor(out=s[:, 12:13], in0=s[:, 3:4], in1=s[:, 7:8], op=ALU.subtract), reads=[sk], writes=[sk])
                P.op(dv, lambda e, s=s, oh1=oh1, t=t: e.tensor_scalar(out=comb[:, t, :], in0=oh1[:], scalar1=s[:, 7:8], scalar2=None, op0=ALU.mult), reads=[o1k, sk], writes=[("comb", t)])
                P.op(dv, lambda e, s=s, oh2=oh2, t=t: e.scalar_tensor_tensor(out=comb[:, t, :], in0=oh2[:], scalar=s[:, 12:13], in1=comb[:, t, :], op0=ALU.mult, op1=ALU.add), reads=[o2k, sk, ("comb", t)], writes=[("comb", t)])
            P.barrier_all()
            if maybe_stop("F0"):
                return nc
        dbg("comb", comb, [128, TOWN, NE])
        with Scope(bump) as stF1:
            Wg_r = Ring(stF1, nc, "Wg", [128, KC, 256], BF16, 2, bump=bump); Wu_r = Ring(stF1, nc, "Wu", [128, KC, 256], BF16, 2, bump=bump)
            Wd_r = Ring(stF1, nc, "Wd", [128, 2, D], BF16, 3, bump=bump)
            sgl_r = Ring(stF1, nc, "sgl", [128, 512], F32, 2, bump=bump)
            ssq = sb("ssqF1", [128, 4], F32, stF1)
            units = [(e_, fh) for e_ in range(NE) for fh in range(2)]
            wt = {}

            def f_load(u):
                e_, fh = units[u]
                Wg, wgk = Wg_r.next(); Wu, wuk = Wu_r.next(); Wd, wdk = Wd_r.next()
                wload(Wg[:], wgk, kchunks(w_eg[e_][:, fh * 256:(fh + 1) * 256]), bar=("F1" not in NOBAR))
                wload(Wu[:], wuk, kchunks(w_eu[e_][:, fh * 256:(fh + 1) * 256]), bar=("F1" not in NOBAR))
                wload(Wd[:], wdk, w_ed[e_][fh * 256:(fh + 1) * 256, :].rearrange("(c p) d -> p c d", p=128), bar=("F1" not in NOBAR))
                wt[u] = (Wg, wgk, Wu, wuk, Wd, wdk)
            class _R6:
                t = PSB.t + [pss, pacc]
                k = PSB.k + ["pss", "pacc"]
                i = 0

                def next(self):
                    j = self.i % 6
                    self.i += 1
                    return self.t[j], self.k[j]
            PS6 = _R6()

            def gateup_steps(Wg, wgk, Wu, wuk, hT, hTk):
                steps = []
                for fc in range(2):
                    for (t0, n) in TGS:
                        def step(fc=fc, t0=t0, n=n):
                            phg, phgk = PS6.next(); phu, phuk = PS6.next()
                            for c in range(KC):
                                P.op(pe, lambda e, c=c: e.matmul(phg[:, 0:n], lhsT=Wg[:, c, fc * 128:(fc + 1) * 128], rhs=hFT[:, c, t0:t0 + n], start=(c == 0), stop=(c == KC - 1)), reads=[wgk], writes=[phgk])
                            for c in range(KC):
                                P.op(pe, lambda e, c=c: e.matmul(phu[:, 0:n], lhsT=Wu[:, c, fc * 128:(fc + 1) * 128], rhs=hFT[:, c, t0:t0 + n], start=(c == 0), stop=(c == KC - 1)), reads=[wuk], writes=[phuk])
                            sgl, sglk = sgl_r.next()
                            P.op(ac, lambda e: e.activation(out=sgl[:, 0:n], in_=phg[:, 0:n], func=AF.Silu), reads=[phgk], writes=[sglk])
                            P.op(dv, lambda e: e.tensor_tensor(out=hT[:, fc, t0:t0 + n], in0=sgl[:, 0:n], in1=phu[:, 0:n], op=ALU.mult), reads=[sglk, phuk, hTk], writes=[hTk])
                        steps.append((n, step))
                return steps

            def down_groups(e_, hT, hTk, Wd, wdk):
                groups = []
                for t in range(TOWN):
                    for cb in range(4):
                        def grp(t=t, cb=cb):
                            py, pyk = PS6.next()
                            for fc in range(2):
                                P.op(pe, lambda e, fc=fc: e.matmul(py[:], lhsT=hT[:, fc, t * 128:(t + 1) * 128], rhs=Wd[:, fc, cb * 512:(cb + 1) * 512], start=(fc == 0), stop=(fc == 1)), reads=[hTk, wdk], writes=[pyk])
                            P.op(dv, lambda e: e.scalar_tensor_tensor(out=yacc[:, t, cb * 512:(cb + 1) * 512], in0=py[:], scalar=comb[:, t, e_:e_ + 1], in1=yacc[:, t, cb * 512:(cb + 1) * 512], op0=ALU.mult, op1=ALU.add), reads=[pyk, ("yacc", t, cb)], writes=[("yacc", t, cb)])
                        groups.append(grp)
                return groups

            f_load(0)
            prev_down = []
            for u, (e_, fh) in enumerate(units):
                if u + 1 < len(units):
                    f_load(u + 1)
                Wg, wgk, Wu, wuk, Wd, wdk = wt.pop(u)
                hT, hTk = hT_v[u % 2], ("hT", u % 2)
                pos = 0
                for (n, step) in gateup_steps(Wg, wgk, Wu, wuk, hT, hTk):
                    step()
                    take = 8 if n == 512 else 2
                    for g in prev_down[pos:pos + take]:
                        g()
                    pos += take
                for g in prev_down[pos:]:
                    g()
                prev_down = down_groups(e_, hT, hTk, Wd, wdk)
            for g in prev_down:
                g()
            P.dma("sp", lambda e: e.dma_start(out=gbc[:], in_=g_final.partition_broadcast(128)), writes=["gbc"])
            for t in range(TOWN):
                yk = [("yacc", t, cb) for cb in range(4)]
                sglj, sgljk = sgl_r.next()
                for cb in range(4):
                    P.op(ac, lambda e, t=t, cb=cb, sglj=sglj: e.activation(out=sglj[:], in_=yacc[:, t, cb * 512:(cb + 1) * 512], func=AF.Square, accum_out=ssq[:, cb:cb + 1]), reads=[("yacc", t, cb), "ssqF"], writes=[sgljk, "ssqF"])
                P.op(dv, lambda e: e.tensor_reduce(out=ssq[:, 0:1], in_=ssq[:, 0:4], axis=AX.X, op=ALU.add), reads=["ssqF"], writes=["ssqF"])
                P.op(ac, lambda e: e.activation(out=ssq[:, 1:2], in_=ssq[:, 0:1], func=AF.Sqrt, scale=1.0 / D, bias=EPS), reads=["ssqF"], writes=["ssqF"])
                P.op(dv, lambda e: e.reciprocal(out=ssq[:, 2:3], in_=ssq[:, 1:2]), reads=["ssqF"], writes=["ssqF"])
                P.op(dv, lambda e, t=t: e.scalar_tensor_tensor(out=yacc[:, t, :], in0=yacc[:, t, :], scalar=ssq[:, 2:3], in1=gbc[:], op0=ALU.mult, op1=ALU.mult), reads=yk + ["ssqF", "gbc"], writes=yk)
                P.dma("sp", lambda e, t=t: e.dma_start(out=y_d[t * 128:(t + 1) * 128, :], in_=yacc[:, t, :]), reads=yk, is_output=True)
            P.finish()
            P.emit()
            _DBG['P'] = P; _DBG['peak'] = bump.peak
    return nc


_NC = {}


def _get_nc(debug=None):
    key = tuple(debug) if debug else ()
    if key not in _NC:
        _NC[key] = build(debug)
    return _NC[key]


def make_in_maps(inp):
    f = lambda a: np.ascontiguousarray(np.asarray(a, dtype=np.float32))
    xpr = f(inp["x_prompt"]); xs = f(inp["x_sample"])
    shared = {
        "g_mix": f(inp["g_mix"][0]), "w_in": f(inp["w_in"][0]), "b_if": f(inp["b_if"][0]), "w_pool": f(inp["w_pool"][0]),
        "pool_scale": f(inp["pool_scale"][0]), "w_pa": f(inp["w_proj_a"][0]), "w_pb": f(inp["w_proj_b"][0]),
        "g_head": f(inp["g_head"][0]), "w_out": f(inp["w_out"][0]), "g_ffn": f(inp["g_ffn"][0]),
        "w_rg": f(inp["w_router_group"][0]), "b_rg": f(inp["b_router_group"][0]), "w_re": f(inp["w_router_expert"][0]),
        "b_re": f(inp["b_router_expert"][0]), "w_eg": f(inp["w_exp_gate"][0]), "w_eu": f(inp["w_exp_up"][0]),
        "w_ed": f(inp["w_exp_down"][0]), "g_final": f(inp["g_final"]),
    }
    spool = f(inp["state_pool"][0]); sC = f(inp["state_C"][0]); sn = f(inp["state_n"][0]); sm = f(inp["state_m"][0])
    maps = []
    for c in range(8):
        b, half = c // 2, c % 2
        xo = np.concatenate([xpr[b, half * 1024:(half + 1) * 1024], xs[16 * c:16 * c + 16].reshape(128, D)], axis=0)
        xp = xpr[b, 0:1024] if half == 1 else np.zeros((1024, D), np.float32)
        rc = np.zeros((4, 16), np.float32)
        for g in range(4):
            for t in range(16):
                rc[g, t] = 1.0 / min(half * 1024 + t + 1, 2 << g)
        m = dict(shared)
        m.update({
            "xo": np.ascontiguousarray(xo), "xp": np.ascontiguousarray(xp),
            "flag": np.full((128, 1), float(half), np.float32), "rc": rc.reshape(64),
            "spool": np.ascontiguousarray(spool[16 * c:16 * c + 16]), "sC": np.ascontiguousarray(sC[16 * c:16 * c + 16]),
            "sn": np.ascontiguousarray(sn[16 * c:16 * c + 16].reshape(64, HD)), "sm": np.ascontiguousarray(sm[16 * c:16 * c + 16]),
        })
        maps.append(m)
    return maps


def assemble(res):
    B = 4
    y_prompt = np.zeros((B, 2048, D), np.float32); y_sample = np.zeros((128, 8, D), np.float32)
    pool_p = np.zeros((1, B, 15, PW), np.float32); C_p = np.zeros((1, B, H, HD, HD), np.float32)
    n_p = np.zeros((1, B, H, HD), np.float32); m_p = np.zeros((1, B, H), np.float32)
    pool_s = np.zeros((1, 128, 15, PW), np.float32); C_s = np.zeros((1, 128, H, HD, HD), np.float32)
    n_s = np.zeros((1, 128, H, HD), np.float32); m_s = np.zeros((1, 128, H), np.float32)
    for c in range(8):
        r = res[c]
        b, half = c // 2, c % 2
        y_prompt[b, half * 1024:(half + 1) * 1024] = r["y"][0:1024]
        y_sample[16 * c:16 * c + 16] = r["y"][1024:].reshape(16, 8, D)
        if half == 1:
            pool_p[0, b] = r["pool_p"]; C_p[0, b] = r["C_p"]; n_p[0, b] = r["n_p"]; m_p[0, b] = r["m_p"][:, 0]
        pool_s[0, 16 * c:16 * c + 16] = r["pool_s"]; C_s[0, 16 * c:16 * c + 16] = r["C_s"]
        n_s[0, 16 * c:16 * c + 16] = r["n_s"].reshape(16, H, HD); m_s[0, 16 * c:16 * c + 16] = r["m_s"]
    return (y_prompt, y_sample, pool_p, C_p, n_p, m_p, pool_s, C_s, n_s, m_s)


def kernel(**inputs):
    nc = _get_nc()
    in_maps = make_in_maps(inputs)
    res = run_bass_kernel_spmd(nc, in_maps, core_ids=list(range(8)))
    return assemble(res.results)
```

```python
import contextlib
import numpy as np
import concourse.bass as bass
import concourse.mybir as mybir
from concourse.bass_utils import run_bass_kernel_spmd

F32 = mybir.dt.float32
BF16 = mybir.dt.bfloat16
AF = mybir.ActivationFunctionType
ALU = mybir.AluOpType
AX = mybir.AxisListType

ENGS = ("pe", "act", "dve", "pool", "sp")
NDMA = 8


class Prog:
    def __init__(self, nc):
        self.nc = nc
        self.lists = {e: [] for e in ENGS}
        self.cnt = {e: 0 for e in ENGS}
        self.dcnt = {e: 0 for e in ENGS}
        self.waited = {e: {} for e in ENGS}
        self.lastw = {}
        self.readers = {}
        self.out_tokens = []

    def _need(self, eng, tok, waits):
        if tok is None:
            return
        sk, v = tok
        if self.waited[eng].get(sk, 0) >= v:
            return
        if sk == "pe" and eng == "pe":
            return
        self.waited[eng][sk] = v
        waits.append((sk, v))

    def _deps(self, eng, reads, writes):
        toks = {}

        def add(t):
            if t is not None and toks.get(t[0], 0) < t[1]:
                toks[t[0]] = t[1]
        for k in reads:
            add(self.lastw.get(k))
        for k in writes:
            add(self.lastw.get(k))
            for t in self.readers.get(k, ()):
                add(t)
        waits = []
        for sk, v in toks.items():
            self._need(eng, (sk, v), waits)
        return waits

    def _commit(self, tok, reads, writes):
        for k in reads:
            self.readers.setdefault(k, []).append(tok)
        for k in writes:
            self.lastw[k] = tok
            self.readers[k] = []

    @staticmethod
    def _norm(reads, writes):
        def nk(k):
            if isinstance(k, str) and k.startswith("pss"):
                return "pss"
            return k
        def is_ps(k):
            return k in ("pss", "pacc") or (isinstance(k, tuple) and k[0] in ("psb", "pst"))
        r2, w2 = [], []
        for k in reads:
            k = nk(k)
            (w2 if is_ps(k) else r2).append(k)
        for k in writes:
            w2.append(nk(k))
        return r2, w2

    def op(self, eng, fn, reads=(), writes=()):
        reads, writes = self._norm(reads, writes)
        waits = self._deps(eng, reads, writes)
        self.cnt[eng] += 1
        tok = (eng, self.cnt[eng])
        self.lists[eng].append(("op", fn, waits, None))
        self._commit(tok, reads, writes)
        return tok

    def dma(self, q, fn, reads=(), writes=(), is_output=False):
        waits = self._deps(q, reads, writes)
        j = self.dcnt[q]
        self.dcnt[q] += 1
        sk = ("dma", q, j % NDMA)
        val = 16 * (j // NDMA + 1)
        if val > 16:
            self._need(q, (sk, val - 16), waits)
        tok = (sk, val)
        if q == "pool":
            if getattr(self, "_last_pool_tok", None) is not None:
                self._need(q, self._last_pool_tok, waits)
            self._last_pool_tok = tok
        self.lists[q].append(("dma", fn, waits, sk))
        self._commit(tok, reads, writes)
        if is_output:
            self.out_tokens.append(tok)
        return tok

    def barrier_all(self):
        toks = [(e, self.cnt[e]) for e in ENGS if self.cnt[e] > 0]
        for q in ENGS:
            for s in range(min(NDMA, self.dcnt[q])):
                n_uses = (self.dcnt[q] - 1 - s) // NDMA + 1
                toks.append((("dma", q, s), 16 * n_uses))
        for e in ENGS:
            waits = []
            for t in toks:
                if t[0] == e:
                    continue
                self._need(e, t, waits)
            if waits:
                self.lists[e].append(("wait", None, waits, None))
        self.lastw.clear()
        self.readers.clear()

    def finish(self):
        waits = []
        for t in self.out_tokens:
            self._need("sp", t, waits)
        self.lists["sp"].append(("wait", None, waits, None))

    def emit(self):
        nc = self.nc
        with contextlib.ExitStack() as st:
            sems = {}
            for e in ENGS:
                sems[e] = st.enter_context(nc.semaphore("s_" + e))
                for s in range(NDMA):
                    sems[("dma", e, s)] = st.enter_context(nc.semaphore("d_%s_%d" % (e, s)))
            block = st.enter_context(nc.Block())

            def run(engname):
                def body(eng):
                    for kind, fn, waits, sk in self.lists[engname]:
                        for (wk, v) in waits:
                            eng.wait_ge(sems[wk], v)
                        if kind == "op":
                            fn(eng).then_inc(sems[engname], 1)
                        elif kind == "dma":
                            fn(eng).then_inc(sems[sk], 16)
                return body

            block.tensor(run("pe"))
            block.scalar(run("act"))
            block.vector(run("dve"))
            block.gpsimd(run("pool"))
            block.sync(run("sp"))


D = 2048
KC = 16
NPRE = 1024
NOWN = 1152
TPRE = 8
TOWN = 9
H = 4
HD = 512
PW = 1024
U0, Q0, K0, V0, O0, IG0, FG0, GA0, GB0, INC = 0, 1024, 3072, 5120, 7168, 9216, 9220, 9224, 11272, 13320
NE = 32
EFF = 512
EPS = 1e-6
NEG = -30000.0
KSCALE = HD ** -0.5
TGS = [(0, 512), (512, 512), (1024, 128)]


class Bump:
    TOTAL = 204800

    def __init__(self, arena, start):
        self.arena = arena
        self.top = start
        self.peak = start

    def view(self, off, shape, dt):
        esz = 2 if dt == BF16 else 4
        n = 1
        for s in shape[1:]:
            n *= s
        nbytes = n * esz
        assert off % 4 == 0 and nbytes % 4 == 0 and off + nbytes <= self.TOTAL, (off, shape, nbytes)
        v = self.arena[0:shape[0], off // 4:(off + nbytes) // 4]
        if dt == BF16:
            v = v.bitcast(BF16)
        if len(shape) == 3:
            v = v.rearrange("p (a b) -> p a b", a=shape[1])
        elif len(shape) == 4:
            v = v.rearrange("p (a b c) -> p a b c", a=shape[1], b=shape[2])
        return v

    def alloc(self, shape, dt):
        esz = 2 if dt == BF16 else 4
        n = 1
        for s in shape[1:]:
            n *= s
        nbytes = (n * esz + 3) // 4 * 4
        shape2 = list(shape)
        if nbytes != n * esz:
            assert len(shape) == 2
            shape2 = [shape[0], nbytes // esz]
        off = (self.top + 63) // 64 * 64
        self.top = off + nbytes
        self.peak = max(self.peak, self.top)
        assert self.top <= self.TOTAL, ("SBUF arena overflow", self.top, shape)
        v = self.view(off, shape2, dt)
        if shape2 != list(shape):
            v = v[:, 0:shape[1]]
        return v


class Scope:
    def __init__(self, bump):
        self.bump = bump

    def __enter__(self):
        self.mark = self.bump.top
        return self

    def __exit__(self, *a):
        self.bump.top = self.mark
        return False


class Ring:
    def __init__(self, st, nc, name, shape, dt, n, psum=False, bump=None):
        if psum:
            self.t = [st.enter_context(nc.psum_tensor("%s%d" % (name, i), shape, dt)) for i in range(n)]
        else:
            self.t = [bump.alloc(shape, dt) for i in range(n)]
        self.k = [(name, i) for i in range(n)]
        self.i = 0

    def next(self):
        j = self.i % len(self.t)
        self.i += 1
        return self.t[j], self.k[j]


_DBG = {}


C2Q = "act"
NOBAR = {"F1", "D0", "D1", "E", "P", "B", "F0", "C1"}


def build(debug=None, stop=None):
    nc = bass.Bass("TRN2", target_bir_lowering=False)
    P = Prog(nc)
    debug = debug or ()

    def maybe_stop(tag):
        if stop == tag:
            P.barrier_all()
            P.finish()
            P.emit()
            _DBG['P'] = P
            return True
        return False

    def din(name, shape):
        return nc.dram_tensor(name, shape, F32, kind="ExternalInput").ap()

    def dout(name, shape):
        return nc.dram_tensor(name, shape, F32, kind="ExternalOutput").ap()

    xo = din("xo", [NOWN, D]); xp = din("xp", [NPRE, D])
    flag_d = din("flag", [128, 1]); rc_d = din("rc", [64])
    spool = din("spool", [16, 15, PW]); sC = din("sC", [16, H, HD, HD]); sn = din("sn", [64, HD]); sm = din("sm", [16, H])
    g_mix = din("g_mix", [D]); w_in = din("w_in", [D, INC]); b_if = din("b_if", [8])
    w_pool = din("w_pool", [4, 256, 256]); pool_scale = din("pool_scale", [PW])
    w_pa = din("w_pa", [PW, D]); w_pb = din("w_pb", [D, D]); g_head = din("g_head", [D]); w_out = din("w_out", [D, D])
    g_ffn = din("g_ffn", [D]); w_rg = din("w_rg", [D, 4]); b_rg = din("b_rg", [4]); w_re = din("w_re", [D, NE]); b_re = din("b_re", [NE])
    w_eg = din("w_eg", [NE, D, EFF]); w_eu = din("w_eu", [NE, D, EFF]); w_ed = din("w_ed", [NE, EFF, D]); g_final = din("g_final", [D])

    y_d = dout("y", [NOWN, D]); poolp_d = dout("pool_p", [15, PW]); Cp_d = dout("C_p", [H, HD, HD]); np_d = dout("n_p", [H, HD]); mp_d = dout("m_p", [H, 1])
    pools_d = dout("pool_s", [16, 15, PW]); Cs_d = dout("C_s", [16, H, HD, HD]); ns_d = dout("n_s", [64, HD]); ms_d = dout("m_s", [16, H])
    cpre_d = dout("scr_cpre", [H, 128, 4 * HD])
    npre_d = dout("scr_npre", [H, 128, 4])

    def kchunks(ap2d):
        return ap2d.rearrange("(c p) n -> p c n", p=128)

    dv, pl, ac, pe = "dve", "pool", "act", "pe"

    with contextlib.ExitStack() as st0:
        Y0, M0, A0, AEND = 0, 73728, 110592, 129024
        arena = st0.enter_context(nc.sbuf_tensor("arena", [128, Bump.TOTAL // 4], F32))
        bump = Bump(arena, AEND)
        view = bump.view

        def sb(name, shape, dt=F32, st=None):
            return bump.alloc(shape, dt)

        hNTo = view(Y0, [128, KC, NOWN], BF16)
        hbT = view(Y0 + 36864, [128, KC, NOWN], BF16)
        yacc = view(Y0, [128, TOWN, D], F32)
        hNTp = view(M0, [128, KC, NPRE], BF16)
        mixT = view(M0, [128, KC, NOWN], BF16)
        hFT = view(M0, [128, KC, NOWN], BF16)
        aT = view(A0, [128, 8, NOWN], BF16)

        PSB = Ring(st0, nc, "psb", [128, 512], F32, 4, psum=True)
        pss = st0.enter_context(nc.psum_tensor("pss", [128, 512], F32))
        pacc = st0.enter_context(nc.psum_tensor("pacc", [128, 512], F32))
        PST = Ring(st0, nc, "pst", [128, 1024], BF16, 2, psum=True)

        identb = sb("identb", [128, 128], BF16); identf = sb("identf", [128, 128])
        maskP = sb("maskP", [128, 128]); maskS = sb("maskS", [128, 128])
        E16 = sb("E16", [16, 128]); blk = sb("blk", [128, 16])
        sel = sb("sel", [4, 4, 128]); ones_b = sb("ones_b", [128, 1], BF16)
        flag = sb("flag", [128, 1]); rcb = sb("rcb", [128, 4, 16])
        gbc = sb("gbc", [128, D])

        QA = Bump.TOTAL // 16
        for qi, eng_ in enumerate([dv, pl, dv, pl]):
            P.op(eng_, lambda e, qi=qi: e.memset(arena[:, qi * QA:(qi + 1) * QA], 0.0), writes=[("arena0", qi)])
        P.barrier_all()
        P.op(pl, lambda e: e.memset(identf[:], 1.0), writes=["identf"])
        P.op(pl, lambda e: e.affine_select(out=identf[:], in_=identf[:], pattern=[[-1, 128]], compare_op=ALU.is_equal, fill=0.0, base=0, channel_multiplier=1), reads=["identf"], writes=["identf"])
        P.op(dv, lambda e: e.tensor_copy(out=identb[:], in_=identf[:]), reads=["identf"], writes=["identb"])
        P.op(pl, lambda e: e.memset(maskP[:], 0.0), writes=["maskP"])
        P.op(pl, lambda e: e.affine_select(out=maskP[:], in_=maskP[:], pattern=[[1, 128]], compare_op=ALU.is_ge, fill=NEG, base=0, channel_multiplier=-1), reads=["maskP"], writes=["maskP"])
        P.op(pl, lambda e: e.memset(E16[:], 1.0), writes=["E16"])
        P.op(pl, lambda e: e.affine_select(out=E16[:], in_=E16[:], pattern=[[1, 128]], compare_op=ALU.is_ge, fill=0.0, base=0, channel_multiplier=-8), reads=["E16"], writes=["E16"])
        P.op(pl, lambda e: e.affine_select(out=E16[:], in_=E16[:], pattern=[[-1, 128]], compare_op=ALU.is_ge, fill=0.0, base=7, channel_multiplier=8), reads=["E16"], writes=["E16"])
        P.op(pe, lambda e: e.matmul(pss[:, 0:128], lhsT=E16[:], rhs=E16[:], start=True, stop=True), reads=["E16"], writes=["pss"])
        P.op(dv, lambda e: e.tensor_scalar(out=maskS[:], in0=pss[:, 0:128], scalar1=-1.0, scalar2=-NEG, op0=ALU.add, op1=ALU.mult), reads=["pss"], writes=["maskS"])
        P.op(dv, lambda e: e.tensor_tensor(out=maskS[:], in0=maskS[:], in1=maskP[:], op=ALU.add), reads=["maskS", "maskP"], writes=["maskS"])
        P.op(pe, lambda e: e.transpose(out=pss[:, 128:144], in_=E16[:], identity=identf[0:16, 0:16]), reads=["E16", "identf", "maskS"], writes=["pss"])
        P.op(dv, lambda e: e.tensor_copy(out=blk[:], in_=pss[:, 128:144]), reads=["pss"], writes=["blk"])
        P.op(pl, lambda e: e.memset(sel[:], 1.0), writes=["sel"])
        P.op(pl, lambda e: e.affine_select(out=sel[:], in_=sel[:], pattern=[[-1, 4], [0, 128]], compare_op=ALU.is_equal, fill=0.0, base=0, channel_multiplier=1), reads=["sel"], writes=["sel"])
        P.op(pl, lambda e: e.memset(ones_b[:], 1.0), writes=["ones_b"])
        P.dma("sp", lambda e: e.dma_start(out=flag[:], in_=flag_d), writes=["flag"])
        P.dma("sp", lambda e: e.dma_start(out=rcb[:].rearrange("p a b -> p (a b)"), in_=rc_d.partition_broadcast(128)), writes=["rcb"])
        P.dma("sp", lambda e: e.dma_start(out=gbc[:], in_=g_mix.partition_broadcast(128)), writes=["gbc"])

        def dbg(name, ap_sb, shape, cast=False):
            if name not in debug:
                return
            d = dout("dbg_" + name, shape)
            P.barrier_all()
            P.dma("pool" if cast else "sp", lambda e: e.dma_start(out=d, in_=ap_sb), is_output=True)
            P.barrier_all()

        def rmsnorm_T(src, dstT, dst_key, tok0, xt_ring, hn_ring, small):
            if isinstance(src, tuple):
                _, xt_ap, xk = src
            else:
                xt, xk = xt_ring.next()
                P.dma("sp", lambda e: e.dma_start(out=xt[:], in_=src), writes=[xk])
                xt_ap = xt[:]
            hn, hk = hn_ring.next()
            ssq, junk = small
            P.op(ac, lambda e: e.activation(out=junk[:], in_=xt_ap, func=AF.Square, accum_out=ssq[:, 0:1]), reads=[xk], writes=["junk", "ssq"])
            P.op(ac, lambda e: e.activation(out=ssq[:, 1:2], in_=ssq[:, 0:1], func=AF.Sqrt, scale=1.0 / D, bias=EPS), reads=["ssq"], writes=["ssq"])
            P.op(dv, lambda e: e.reciprocal(out=ssq[:, 2:3], in_=ssq[:, 1:2]), reads=["ssq"], writes=["ssq"])
            P.op(dv, lambda e: e.scalar_tensor_tensor(out=hn[:], in0=xt_ap, scalar=ssq[:, 2:3], in1=gbc[:], op0=ALU.mult, op1=ALU.mult), reads=[xk, "ssq", "gbc"], writes=[hk])
            for half in range(2):
                pt, pk = PST.next()
                for c in range(8):
                    cc = half * 8 + c
                    P.op(pe, lambda e, c=c, cc=cc, pt=pt: e.transpose(out=pt[:, c * 128:(c + 1) * 128], in_=hn[:, cc * 128:(cc + 1) * 128], identity=identb[:]), reads=[hk, "identb"], writes=[pk])
                if half == 0:
                    P.op(ac, lambda e, pt=pt, half=half: e.activation(out=dstT[:, half * 8:(half + 1) * 8, tok0:tok0 + 128], in_=pt[:].rearrange("p (c t) -> p c t", c=8), func=AF.Copy), reads=[pk], writes=[(dst_key, tok0 // 128, half)])
                else:
                    P.op(dv, lambda e, pt=pt, half=half: e.tensor_copy(out=dstT[:, half * 8:(half + 1) * 8, tok0:tok0 + 128], in_=pt[:].rearrange("p (c t) -> p c t", c=8)), reads=[pk], writes=[(dst_key, tok0 // 128, half)])

        def wload(dst, key, src_ap, slow=False, bar=True):
            if bar:
                P.barrier_all()
            P.dma("pool", lambda e: e.dma_start(out=dst, in_=src_ap, allow_slow_non_contiguous=slow), writes=[key])
            if bar:
                P.barrier_all()

        with Scope(bump) as stMid:
            colq = sb("colq", [128, TPRE + TOWN, 20], F32, stMid)
            WLI = sb("WLI", [4, 32], F32, stMid); wliS = sb("wliS", [128, 32, 4], F32, stMid)
            alpha = sb("alpha", [4, NOWN], F32, stMid)
            uhist = sb("uhist", [128, 8, 16], F32, stMid)
            with Scope(bump) as stA:
                xt_ring = Ring(stA, nc, "xt", [128, D], F32, 2, bump=bump)
                hn_ring = Ring(stA, nc, "hn", [128, D], BF16, 2, bump=bump)
                ssq = sb("ssqA", [128, 4], F32, stA); junk = sb("junkA", [128, D], BF16, stA)
                for i in range(TPRE):
                    rmsnorm_T(xp[i * 128:(i + 1) * 128, :], hNTp, "hNTp", i * 128, xt_ring, hn_ring, (ssq, junk))
                for i in range(TOWN):
                    rmsnorm_T(xo[i * 128:(i + 1) * 128, :], hNTo, "hNTo", i * 128, xt_ring, hn_ring, (ssq, junk))
                P.barrier_all()
                if maybe_stop("A"):
                    return nc
            NT = NPRE + NOWN
            with Scope(bump) as stB:
                R = [sb("row%d" % i, [4, NT], F32, stB) for i in range(6)]
                ig, lf, mrow, brow, arow, beta = R
                Wig = sb("Wig", [128, KC, 4], BF16, stB); Wfg = sb("Wfg", [128, KC, 4], BF16, stB)
                bi = sb("bi", [4, 2], F32, stB); nbf = sb("nbf", [4, 1], F32, stB)
                smT = sb("smT", [4, 16], F32, stB); zrow = sb("zrow", [4, 128], F32, stB)
                mprev = sb("mprev", [4, 16], F32, stB); tmp4 = sb("tmp4", [4, 16], F32, stB)
                with nc.allow_non_contiguous_dma(reason="tiny gate loads"):
                    wload(Wig[:], "Wig", kchunks(w_in[:, IG0:IG0 + 4]), True, bar=("B" not in NOBAR))
                    wload(Wfg[:], "Wfg", kchunks(w_in[:, FG0:FG0 + 4]), True, bar=("B" not in NOBAR))
                    P.dma("sp", lambda e: e.dma_start(out=bi[:], in_=b_if.rearrange("(two h) -> h two", two=2), allow_slow_non_contiguous=True), writes=["bi"])
                    P.dma("sp", lambda e: e.dma_start(out=smT[:], in_=sm.rearrange("j h -> h j"), allow_slow_non_contiguous=True), writes=["smT"])
                P.op(dv, lambda e: e.tensor_scalar(out=nbf[:], in0=bi[:, 1:2], scalar1=-1.0, scalar2=None, op0=ALU.mult), reads=["bi"], writes=["nbf"])
                P.op(dv, lambda e: e.memset(zrow[:], 0.0), writes=["zrow"])
                if maybe_stop("B1"):
                    return nc
                groups = [(hNTp, 0, 512, 0), (hNTp, 512, 512, 512)] + [(hNTo, t0, n, NPRE + t0) for (t0, n) in TGS]
                for (src, t0, n, col0) in groups:
                    pg, pgk = PSB.next(); pf, pfk = PSB.next()
                    for c in range(KC):
                        P.op(pe, lambda e, c=c, pg=pg, src=src, t0=t0, n=n: e.matmul(pg[0:4, 0:n], lhsT=Wig[:, c, :], rhs=src[:, c, t0:t0 + n], start=(c == 0), stop=(c == KC - 1)), reads=["Wig"], writes=[pgk])
                    for c in range(KC):
                        P.op(pe, lambda e, c=c, pf=pf, src=src, t0=t0, n=n: e.matmul(pf[0:4, 0:n], lhsT=Wfg[:, c, :], rhs=src[:, c, t0:t0 + n], start=(c == 0), stop=(c == KC - 1)), reads=["Wfg"], writes=[pfk])
                    P.op(ac, lambda e, pg=pg, col0=col0, n=n: e.activation(out=ig[:, col0:col0 + n], in_=pg[0:4, 0:n], func=AF.Identity, bias=bi[:, 0:1], scale=1.0), reads=[pgk, "bi"], writes=["ig"])
                    P.op(ac, lambda e, pf=pf, col0=col0, n=n: e.activation(out=lf[:, col0:col0 + n], in_=pf[0:4, 0:n], func=AF.Exp, bias=nbf[:, 0:1], scale=-1.0), reads=[pfk, "nbf"], writes=["lf"])
                P.op(ac, lambda e: e.activation(out=lf[:], in_=lf[:], func=AF.Ln, bias=1.0, scale=1.0), reads=["lf"], writes=["lf"])
                P.op(dv, lambda e: e.tensor_scalar(out=lf[:], in0=lf[:], scalar1=-1.0, scalar2=None, op0=ALU.mult), reads=["lf"], writes=["lf"])
                if maybe_stop("B2"):
                    return nc
                P.op(dv, lambda e: e.tensor_tensor_scan(out=mrow[:, 0:NPRE], data0=lf[:, 0:NPRE], data1=ig[:, 0:NPRE], initial=0.0, op0=ALU.add, op1=ALU.max), reads=["lf", "ig"], writes=["mrow"])
                P.op(dv, lambda e: e.memset(mprev[:], 0.0), writes=["mprev"])
                P.op(dv, lambda e: e.tensor_tensor(out=mprev[:, 8:9], in0=mrow[:, NPRE - 1:NPRE], in1=flag[0:4, :], op=ALU.mult), reads=["mrow", "flag", "mprev"], writes=["mprev"])
                P.op(dv, lambda e: e.tensor_tensor_scan(out=mrow[:, NPRE:2048], data0=lf[:, NPRE:2048], data1=ig[:, NPRE:2048], initial=mprev[:, 8:9], op0=ALU.add, op1=ALU.max), reads=["lf", "ig", "mprev", "mrow"], writes=["mrow"])
                for j in range(16):
                    a = 2048 + 8 * j
                    P.op(dv, lambda e, a=a, j=j: e.tensor_tensor_scan(out=mrow[:, a:a + 8], data0=lf[:, a:a + 8], data1=ig[:, a:a + 8], initial=smT[:, j:j + 1], op0=ALU.add, op1=ALU.max), reads=["lf", "ig", "smT", "mrow"], writes=["mrow"])
                    P.op(dv, lambda e, a=a: e.tensor_tensor_scan(out=brow[:, a:a + 8], data0=lf[:, a:a + 8], data1=zrow[:, 0:8], initial=0.0, op0=ALU.add, op1=ALU.add), reads=["lf", "zrow", "brow"], writes=["brow"])
                for i in range(16):
                    a = 128 * i
                    P.op(dv, lambda e, a=a: e.tensor_tensor_scan(out=brow[:, a:a + 128], data0=lf[:, a:a + 128], data1=zrow[:], initial=0.0, op0=ALU.add, op1=ALU.add), reads=["lf", "zrow", "brow"], writes=["brow"])
                P.op(dv, lambda e: e.tensor_copy(out=mprev[:, 1:8], in_=mrow[:, 127:127 + 7 * 128:128]), reads=["mrow", "mprev"], writes=["mprev"])
                P.op(dv, lambda e: e.tensor_copy(out=mprev[:, 9:16], in_=mrow[:, NPRE + 127:NPRE + 127 + 7 * 128:128]), reads=["mrow", "mprev"], writes=["mprev"])
                if maybe_stop("B3"):
                    return nc
                P.dma("sp", lambda e: e.dma_start(out=mp_d, in_=mrow[:, 2047:2048]), reads=["mrow"], is_output=True)
                with nc.allow_non_contiguous_dma(reason="tiny m out"):
                    P.dma("sp", lambda e: e.dma_start(out=ms_d.rearrange("j h -> h j"), in_=mrow[:, 2048 + 7:NT:8], allow_slow_non_contiguous=True), reads=["mrow"], is_output=True)
                P.op(dv, lambda e: e.tensor_tensor(out=arow[:], in0=brow[:], in1=mrow[:], op=ALU.subtract), reads=["brow", "mrow"], writes=["arow"])
                if maybe_stop("B4"):
                    return nc
                P.op(dv, lambda e: e.tensor_tensor(out=beta[:], in0=ig[:], in1=brow[:], op=ALU.subtract), reads=["ig", "brow"], writes=["beta"])
                P.op(dv, lambda e: e.tensor_copy(out=alpha[:], in_=arow[:, NPRE:NT]), reads=["arow"], writes=["alpha"])
                emneg, winter, wl = brow, lf, ig
                P.op(ac, lambda e: e.activation(out=emneg[:], in_=mrow[:], func=AF.Exp, scale=-1.0), reads=["mrow", "brow", "beta", "arow"], writes=["brow"])
                for i in range(16):
                    a = 128 * i
                    P.op(ac, lambda e, a=a, i=i: e.activation(out=winter[:, a:a + 128], in_=arow[:, a:a + 128], func=AF.Exp, bias=mprev[:, i:i + 1], scale=1.0), reads=["arow", "mprev", "lf", "mrow"], writes=["lf"])
                    P.op(ac, lambda e, a=a: e.activation(out=wl[:, a:a + 128], in_=beta[:, a:a + 128], func=AF.Exp, bias=arow[:, a + 127:a + 128], scale=1.0), reads=["beta", "arow", "ig"], writes=["ig"])
                for j in range(16):
                    a = 2048 + 8 * j
                    P.op(ac, lambda e, a=a, j=j: e.activation(out=winter[:, a:a + 8], in_=arow[:, a:a + 8], func=AF.Exp, bias=smT[:, j:j + 1], scale=1.0), reads=["arow", "smT", "lf"], writes=["lf"])
                    P.op(ac, lambda e, a=a: e.activation(out=wl[:, a:a + 8], in_=beta[:, a:a + 8], func=AF.Exp, bias=arow[:, a + 7:a + 8], scale=1.0), reads=["beta", "arow", "ig"], writes=["ig"])
                P.op(dv, lambda e: e.tensor_tensor(out=tmp4[:], in0=arow[:, 127:2048:128], in1=mprev[:], op=ALU.add), reads=["arow", "mprev"], writes=["tmp4"])
                P.op(ac, lambda e: e.activation(out=WLI[:, 0:16], in_=tmp4[:], func=AF.Exp), reads=["tmp4"], writes=["WLI"])
                P.op(dv, lambda e: e.tensor_tensor(out=tmp4[:], in0=arow[:, 2048 + 7:NT:8], in1=smT[:], op=ALU.add), reads=["arow", "smT", "tmp4", "WLI"], writes=["tmp4"])
                P.op(ac, lambda e: e.activation(out=WLI[:, 16:32], in_=tmp4[:], func=AF.Exp), reads=["tmp4", "WLI"], writes=["WLI"])
                if maybe_stop("B5"):
                    return nc
                for i in range(TPRE + TOWN):
                    a = 128 * i
                    for qi, (rowt, rk) in enumerate([(beta, "beta"), (winter, "lf"), (emneg, "brow"), (wl, "ig"), (arow, "arow")]):
                        P.op(pe, lambda e, rowt=rowt, a=a, qi=qi: e.transpose(out=pss[:, 256 + 4 * qi:256 + 4 * qi + 4], in_=rowt[:, a:a + 128], identity=identf[0:4, 0:4]), reads=[rk, "identf"], writes=["pssq"])
                    P.op(dv, lambda e, i=i: e.tensor_copy(out=colq[:, i, :], in_=pss[:, 256:276]), reads=["pssq"], writes=[("colq", i)])
                    if i == 0 and maybe_stop("B5a"):
                        return nc
                    if i == 7 and maybe_stop("B5b"):
                        return nc
                    if i == 16 and maybe_stop("B5c"):
                        return nc
                rowc_r = Ring(stB, nc, "rowc", [4, 128], F32, 2, bump=bump)
                for i in range(32):
                    rowc, rck = rowc_r.next()
                    P.op(ac, lambda e, rowc=rowc, i=i: e.activation(out=rowc[:], in_=zrow[:], func=AF.Identity, bias=WLI[:, i:i + 1], scale=0.0), reads=["WLI", "zrow"], writes=[rck])
                    P.op(pe, lambda e, rowc=rowc, i=i: e.transpose(out=pss[:, 288 + 4 * i:288 + 4 * i + 4], in_=rowc[:], identity=identf[0:4, 0:4]), reads=[rck], writes=["pssw"])
                P.op(dv, lambda e: e.tensor_copy(out=wliS[:].rearrange("p i h -> p (i h)"), in_=pss[:, 288:416]), reads=["pssw"], writes=["wliS"])
                if maybe_stop("B6"):
                    return nc
                wuB = [view(Y0 + 36864 + k * 4096, [128, KC, 128], BF16) for k in range(2)]
                for fc in range(8):
                    wu, wk = wuB[fc % 2], ("wuB", fc % 2)
                    wload(wu, wk, kchunks(w_in[:, U0 + fc * 128:U0 + (fc + 1) * 128]), bar=("B" not in NOBAR))
                    pu, puk = PSB.next()
                    for c in range(KC):
                        P.op(pe, lambda e, c=c, wu=wu, pu=pu: e.matmul(pu[:, 0:16], lhsT=wu[:, c, :], rhs=hNTp[:, c, NPRE - 16:NPRE], start=(c == 0), stop=(c == KC - 1)), reads=[wk], writes=[puk])
                    P.op(ac, lambda e, fc=fc, pu=pu: e.activation(out=uhist[:, fc, :], in_=pu[:, 0:16], func=AF.Copy), reads=[puk], writes=[("uhist", fc)])
                if "rows" in debug:
                    for nm, t in [("ig_wl", ig), ("lf_winter", lf), ("mrow", mrow), ("b_emneg", brow), ("arow", arow), ("beta", beta)]:
                        dbg_ap = t[:]
                        d = dout("dbg_" + nm, [4, NT])
                        P.barrier_all()
                        P.dma("sp", lambda e, d=d, dbg_ap=dbg_ap: e.dma_start(out=d, in_=dbg_ap), is_output=True)
                P.barrier_all()
                if maybe_stop("B"):
                    return nc
            dbg("hNTo", hNTo, [128, KC, NOWN])
            dbg("colq", colq[:], [128, TPRE + TOWN, 20])
            with Scope(bump) as stC0:
                WkC0 = [view(Y0 + 36864 + k * 16384, [128, KC, HD], BF16) for k in range(2)]
                Wv_r = Ring(stC0, nc, "WvC0_", [128, KC, HD], BF16, 2, bump=bump)
                pad0 = sb("pad0", [128, 1024], F32, stC0)
                Cst = sb("Cst0", [128, 4, HD], F32, stC0); nst = sb("nst0", [128, 4], F32, stC0)
                kt_r = Ring(stC0, nc, "ktC0_", [128, HD], BF16, 2, bump=bump); v_r = Ring(stC0, nc, "vC0_", [128, HD], BF16, 2, bump=bump)
                wlib = sb("wlib0", [128, 2], F32, stC0)

                WvA = view(A0, [128, KC, HD], BF16)
                for h in range(H):
                    Wk, wkk = WkC0[1], ("WkC0", 1)
                    Wv, wvk = WvA, "WvA"
                    wload(Wk, wkk, kchunks(w_in[:, K0 + h * HD:K0 + (h + 1) * HD]), bar=("C0" not in NOBAR))
                    wload(Wv, wvk, kchunks(w_in[:, V0 + h * HD:V0 + (h + 1) * HD]), bar=("C0" not in NOBAR))
                    P.op(dv, lambda e: e.memset(Cst[:], 0.0), reads=["Cst"], writes=["Cst"])
                    P.op(dv, lambda e: e.memset(nst[:], 0.0), reads=["nst"], writes=["nst"])
                    for i in range(TPRE):
                        pk_, pkk = PSB.next(); pv_, pvk = PSB.next()
                        for c in range(KC):
                            P.op(pe, lambda e, c=c, pk_=pk_, i=i, Wk=Wk: e.matmul(pk_[:], lhsT=hNTp[:, c, i * 128:(i + 1) * 128], rhs=Wk[:, c, :], start=(c == 0), stop=(c == KC - 1)), reads=[wkk], writes=[pkk])
                        for c in range(KC):
                            P.op(pe, lambda e, c=c, pv_=pv_, i=i, Wv=Wv: e.matmul(pv_[:], lhsT=hNTp[:, c, i * 128:(i + 1) * 128], rhs=Wv[:, c, :], start=(c == 0), stop=(c == KC - 1)), reads=[wvk], writes=[pvk])
                        kt, ktk = kt_r.next(); vt, vk = v_r.next()
                        P.op(dv, lambda e, kt=kt, pk_=pk_, i=i, h=h: e.tensor_scalar(out=kt[:], in0=pk_[:], scalar1=colq[:, i, 12 + h:13 + h], scalar2=KSCALE, op0=ALU.mult, op1=ALU.mult), reads=[pkk], writes=[ktk])
                        P.op(ac, lambda e, vt=vt, pv_=pv_: e.activation(out=vt[:], in_=pv_[:], func=AF.Copy), reads=[pvk], writes=[vk])
                        if h == 0 and i == 0:
                            dbg("c0v", vt[:], [128, HD], cast=True)
                            dbg("c0kt", kt[:], [128, HD], cast=True)
                            dbg("c0wv", Wv[:, 0, :], [128, HD], cast=True)
                            dbg("c0wv15", Wv[:, 15, :], [128, HD], cast=True)
                            dbg("c0hn", hNTp[:, 0, 0:128], [128, 128], cast=True)
                        for c2 in range(4):
                            pkv, pkvk = PSB.next()
                            P.op(pe, lambda e, c2=c2, pkv=pkv, kt=kt, vt=vt: e.matmul(pkv[:], lhsT=kt[:, c2 * 128:(c2 + 1) * 128], rhs=vt[:], start=True, stop=True), reads=[ktk, vk], writes=[pkvk])
                            P.op(dv, lambda e, c2=c2, pkv=pkv, h=h, i=i: e.scalar_tensor_tensor(out=Cst[:, c2, :], in0=Cst[:, c2, :], scalar=wliS[:, i, h:h + 1], in1=pkv[:], op0=ALU.mult, op1=ALU.add), reads=["Cst", pkvk], writes=["Cst"])
                        for c2 in range(4):
                            P.op(pe, lambda e, c2=c2, kt=kt: e.matmul(pss[:, 8 + c2:9 + c2], lhsT=kt[:, c2 * 128:(c2 + 1) * 128], rhs=ones_b[:], start=True, stop=True), reads=[ktk], writes=["pss1"])
                        P.op(dv, lambda e, h=h, i=i: e.scalar_tensor_tensor(out=nst[:], in0=nst[:], scalar=wliS[:, i, h:h + 1], in1=pss[:, 8:12], op0=ALU.mult, op1=ALU.add), reads=["nst", "pss1"], writes=["nst"])
                        P.barrier_all()
                        if h == 0 and i == 0:
                            dbg("c0cst", Cst[:], [128, 4, HD])
                        if h == 1 and i == 0:
                            dbg("wvA", Wv, [128, KC, HD], cast=True)
                        if h == 1 and i == 7:
                            dbg("wvB", Wv, [128, KC, HD], cast=True)
                            dbg("c0cst7b", Cst[:], [128, 4, HD])
                        if h == 0 and i == 7:
                            dbg("c0cst7", Cst[:], [128, 4, HD])
                            dbg("c0v7", vt[:], [128, HD], cast=True)
                            dbg("c0kt7", kt[:], [128, HD], cast=True)
                        if h == 0 and i == 3:
                            dbg("c0cst3", Cst[:], [128, 4, HD])
                    P.op(dv, lambda e: e.tensor_scalar(out=Cst[:], in0=Cst[:], scalar1=flag[:, 0:1], scalar2=None, op0=ALU.mult), reads=["Cst"], writes=["Cst"])
                    P.op(dv, lambda e: e.tensor_scalar(out=nst[:], in0=nst[:], scalar1=flag[:, 0:1], scalar2=None, op0=ALU.mult), reads=["nst"], writes=["nst"])
                    if h == 0:
                        dbg("c0cstF", Cst[:], [128, 4, HD])
                        dbg("wliS", wliS[:], [128, 32, 4])
                        dbg("flag", flag[:], [128, 1])
                    P.dma("sp", lambda e, h=h: e.dma_start(out=cpre_d[h], in_=Cst[:].rearrange("p c v -> p (c v)")), reads=["Cst"], writes=[("cpre", h)])
                    P.dma("sp", lambda e, h=h: e.dma_start(out=npre_d[h], in_=nst[:]), reads=["nst"], writes=[("npre", h)])
                P.barrier_all()
                if maybe_stop("C0"):
                    return nc
            with Scope(bump) as stC:
                Wq = view(M0, [128, KC, HD], BF16); Wk = view(M0 + 16384, [128, KC, HD], BF16); Wv = view(M0 + 32768, [128, KC, HD], BF16)
                Cst = sb("Cst", [128, 4, HD], F32, stC); Cb = sb("Cb", [128, 4, HD], BF16, stC)
                nst = sb("nst", [128, 4], F32, stC); nb = sb("nb", [128, 4], BF16, stC)
                ghb = sb("ghb", [128, HD], F32, stC)
                s_qT = sb("s_qT", [128, H, 4, 128], BF16, stC); s_kt = sb("s_kt", [128, H, HD], BF16, stC)
                s_v = sb("s_v", [128, H, HD], BF16, stC); s_num = sb("s_num", [128, H, HD], F32, stC); s_den = sb("s_den", [128, H], F32, stC)
                qT_r = Ring(stC, nc, "qT", [128, 4, 128], BF16, 2, bump=bump); kT_r = Ring(stC, nc, "kT", [128, 4, 128], BF16, 2, bump=bump)
                kt_r = Ring(stC, nc, "kt", [128, HD], BF16, 2, bump=bump); v_r = Ring(stC, nc, "vt", [128, HD], BF16, 2, bump=bump)
                Mt_r = Ring(stC, nc, "Mt", [128, 128], F32, 2, bump=bump)
                Wt_r = Ring(stC, nc, "Wt", [128, 128], F32, 2, bump=bump); PT_r = Ring(stC, nc, "PT", [128, 128], BF16, 2, bump=bump)
                num_r = Ring(stC, nc, "num", [128, HD], F32, 1, bump=bump); tmpn_r = Ring(stC, nc, "tmpn", [128, HD], F32, 1, bump=bump)
                hbt_r = Ring(stC, nc, "hbt", [128, HD], BF16, 2, bump=bump)
                sm_r = Ring(stC, nc, "smalls", [128, 16], F32, 2, bump=bump)
                junkC = sb("junkC", [128, HD], BF16, stC)

                def finish_tile(h, i, num_ap, numk, den_ap, small, smk):
                    P.op(dv, lambda e: e.tensor_scalar(out=small[:, 4:5], in0=den_ap, scalar1=-1.0, scalar2=None, op0=ALU.mult), reads=[smk], writes=[smk])
                    P.op(dv, lambda e: e.tensor_tensor(out=small[:, 4:5], in0=small[:, 4:5], in1=den_ap, op=ALU.max), reads=[smk], writes=[smk])
                    P.op(dv, lambda e: e.tensor_tensor(out=small[:, 4:5], in0=small[:, 4:5], in1=colq[:, TPRE + i, 8 + h:9 + h], op=ALU.max), reads=[smk], writes=[smk])
                    P.op(dv, lambda e: e.reciprocal(out=small[:, 5:6], in_=small[:, 4:5]), reads=[smk], writes=[smk])
                    P.op(ac, lambda e: e.activation(out=junkC[:], in_=num_ap, func=AF.Square, accum_out=small[:, 6:7]), reads=[numk, smk], writes=["junkC", smk])
                    P.op(dv, lambda e: e.tensor_scalar(out=small[:, 7:8], in0=small[:, 6:7], scalar1=small[:, 5:6], scalar2=small[:, 5:6], op0=ALU.mult, op1=ALU.mult), reads=[smk], writes=[smk])
                    P.op(ac, lambda e: e.activation(out=small[:, 8:9], in_=small[:, 7:8], func=AF.Sqrt, scale=1.0 / HD, bias=EPS), reads=[smk], writes=[smk])
                    P.op(dv, lambda e: e.reciprocal(out=small[:, 9:10], in_=small[:, 8:9]), reads=[smk], writes=[smk])
                    P.op(dv, lambda e: e.tensor_tensor(out=small[:, 10:11], in0=small[:, 9:10], in1=small[:, 5:6], op=ALU.mult), reads=[smk], writes=[smk])
                    hbt, hbk = hbt_r.next()
                    P.op(dv, lambda e: e.scalar_tensor_tensor(out=hbt[:], in0=num_ap, scalar=small[:, 10:11], in1=ghb[:], op0=ALU.mult, op1=ALU.mult), reads=[numk, smk, "ghb"], writes=[hbk])
                    pt, pk = PST.next()
                    for c in range(4):
                        P.op(pe, lambda e, c=c, pt=pt: e.transpose(out=pt[:, c * 128:(c + 1) * 128], in_=hbt[:, c * 128:(c + 1) * 128], identity=identb[:]), reads=[hbk], writes=[pk])
                    P.op(ac, lambda e, pt=pt: e.activation(out=hbT[:, 4 * h:4 * h + 4, i * 128:(i + 1) * 128], in_=pt[:, 0:512].rearrange("p (c t) -> p c t", c=4), func=AF.Copy), reads=[pk], writes=[("hbT", h, i)])

                for h in range(H):
                    wload(Wq, "Wq", kchunks(w_in[:, Q0 + h * HD:Q0 + (h + 1) * HD]), bar=("C1" not in NOBAR))
                    wload(Wk, "Wk", kchunks(w_in[:, K0 + h * HD:K0 + (h + 1) * HD]), bar=("C1" not in NOBAR))
                    wload(Wv, "Wv", kchunks(w_in[:, V0 + h * HD:V0 + (h + 1) * HD]), bar=("C1" not in NOBAR))
                    P.dma("sp", lambda e, h=h: e.dma_start(out=ghb[:], in_=g_head[h * HD:(h + 1) * HD].partition_broadcast(128)), writes=["ghb"])
                    P.dma("sp", lambda e, h=h: e.dma_start(out=Cst[:].rearrange("p c v -> p (c v)"), in_=cpre_d[h]), writes=["Cst"])
                    P.dma("sp", lambda e, h=h: e.dma_start(out=nst[:], in_=npre_d[h]), writes=["nst"])
                    P.op(ac, lambda e: e.activation(out=Cb[:], in_=Cst[:], func=AF.Copy), reads=["Cst"], writes=["Cb"])
                    P.op(ac, lambda e: e.activation(out=nb[:], in_=nst[:], func=AF.Copy), reads=["nst"], writes=["nb"])
                    if h == 0:
                        dbg("cst0", Cst[:], [128, 4, HD])
                    def part1(i, h=h):
                            samp = (i == TOWN - 1)
                            ci = TPRE + i
                            if samp:
                                qT, qk = s_qT[:, h], ("s_qT", h)
                                ktl, ktk = s_kt[:, h], ("s_kt", h)
                                vt, vk = s_v[:, h], ("s_v", h)
                            else:
                                t_, qk = qT_r.next(); qT = t_[:]
                                t_, ktk = kt_r.next(); ktl = t_[:]
                                t_, vk = v_r.next(); vt = t_[:]
                            t_, kTk = kT_r.next(); kT = t_[:]
                            pq, pqk = PSB.next()
                            for cc in range(4):
                                for c in range(KC):
                                    P.op(pe, lambda e, c=c, cc=cc, pq=pq, i=i: e.matmul(pq[:, cc * 128:(cc + 1) * 128], lhsT=Wq[:, c, cc * 128:(cc + 1) * 128], rhs=hNTo[:, c, i * 128:(i + 1) * 128], start=(c == 0), stop=(c == KC - 1)), reads=["Wq"], writes=[pqk])
                            P.op(ac, lambda e, pq=pq, qT=qT: e.activation(out=qT.rearrange("p c t -> p (c t)"), in_=pq[:], func=AF.Copy), reads=[pqk], writes=[qk])
                            pkT, pkTk = PSB.next()
                            for cc in range(4):
                                for c in range(KC):
                                    P.op(pe, lambda e, c=c, cc=cc, pkT=pkT, i=i: e.matmul(pkT[:, cc * 128:(cc + 1) * 128], lhsT=Wk[:, c, cc * 128:(cc + 1) * 128], rhs=hNTo[:, c, i * 128:(i + 1) * 128], start=(c == 0), stop=(c == KC - 1)), reads=["Wk"], writes=[pkTk])
                            P.op(dv, lambda e, pkT=pkT, kT=kT: e.tensor_scalar(out=kT.rearrange("p c t -> p (c t)"), in0=pkT[:], scalar1=KSCALE, scalar2=None, op0=ALU.mult), reads=[pkTk], writes=[kTk])
                            pk_, pkk = PSB.next()
                            for c in range(KC):
                                P.op(pe, lambda e, c=c, pk_=pk_, i=i: e.matmul(pk_[:], lhsT=hNTo[:, c, i * 128:(i + 1) * 128], rhs=Wk[:, c, :], start=(c == 0), stop=(c == KC - 1)), reads=["Wk"], writes=[pkk])
                            P.op(dv, lambda e, pk_=pk_, ktl=ktl, ci=ci, h=h: e.tensor_scalar(out=ktl, in0=pk_[:], scalar1=colq[:, ci, 12 + h:13 + h], scalar2=KSCALE, op0=ALU.mult, op1=ALU.mult), reads=[pkk], writes=[ktk])
                            pv_, pvk = PSB.next()
                            for c in range(KC):
                                P.op(pe, lambda e, c=c, pv_=pv_, i=i: e.matmul(pv_[:], lhsT=hNTo[:, c, i * 128:(i + 1) * 128], rhs=Wv[:, c, :], start=(c == 0), stop=(c == KC - 1)), reads=["Wv"], writes=[pvk])
                            P.op(ac, lambda e, pv_=pv_, vt=vt: e.activation(out=vt, in_=pv_[:], func=AF.Copy), reads=[pvk], writes=[vk])
                            pS, pSk = PSB.next()
                            for c in range(4):
                                P.op(pe, lambda e, c=c, pS=pS, kT=kT, qT=qT: e.matmul(pS[:, 0:128], lhsT=kT[:, c, :], rhs=qT[:, c, :], start=(c == 0), stop=(c == 3)), reads=[kTk, qk], writes=[pSk])
                            Mt, Mtk = Mt_r.next()
                            P.op(ac, lambda e, Mt=Mt, ci=ci, h=h: e.activation(out=Mt[:], in_=maskP[:], func=AF.Identity, bias=colq[:, ci, 16 + h:17 + h], scale=0.0), reads=[], writes=[Mtk])
                            P.op(pe, lambda e, pS=pS, Mt=Mt: e.transpose(out=pS[:, 128:256], in_=Mt[:], identity=identf[:]), reads=[Mtk], writes=[pSk])
                            Wt, Wtk = Wt_r.next(); PT, PTk = PT_r.next()
                            msk = maskS if samp else maskP
                            P.op(dv, lambda e, Wt=Wt, pS=pS, msk=msk: e.tensor_tensor(out=Wt[:], in0=pS[:, 128:256], in1=msk[:], op=ALU.add), reads=[pSk], writes=[Wtk])
                            P.op(ac, lambda e, Wt=Wt, ci=ci, h=h: e.activation(out=Wt[:], in_=Wt[:], func=AF.Exp, bias=colq[:, ci, h:h + 1], scale=1.0), reads=[Wtk], writes=[Wtk])
                            P.op(dv, lambda e, Wt=Wt, PT=PT, pS=pS: e.tensor_tensor(out=PT[:], in0=pS[:, 0:128], in1=Wt[:], op=ALU.mult), reads=[pSk, Wtk], writes=[PTk])
                            return dict(i=i, samp=samp, ci=ci, qT=qT, qk=qk, ktl=ktl, ktk=ktk, vt=vt, vk=vk, PT=PT, PTk=PTk)

                    def part2(cx, h=h):
                            i, samp, ci, qT, qk, ktl, ktk, vt, vk, PT, PTk = (cx[k_] for k_ in ('i', 'samp', 'ci', 'qT', 'qk', 'ktl', 'ktk', 'vt', 'vk', 'PT', 'PTk'))
                            pn, pnk = PSB.next()
                            P.op(pe, lambda e, pn=pn, PT=PT, vt=vt: e.matmul(pn[:], lhsT=PT[:], rhs=vt, start=True, stop=True), reads=[PTk, vk], writes=[pnk])
                            P.op(pe, lambda e, PT=PT: e.matmul(pss[:, 16:17], lhsT=PT[:], rhs=ones_b[:], start=True, stop=True), reads=[PTk], writes=["pssd"])
                            if samp:
                                P.op(ac, lambda e, pn=pn, h=h: e.activation(out=s_num[:, h, :], in_=pn[:], func=AF.Copy), reads=[pnk], writes=[("s_num", h)])
                                P.op(ac, lambda e, h=h: e.activation(out=s_den[:, h:h + 1], in_=pss[:, 16:17], func=AF.Copy), reads=["pssd"], writes=[("s_den", h)])
                                return
                            pi_, pik = PSB.next()
                            for c in range(4):
                                P.op(pe, lambda e, c=c, pi_=pi_, qT=qT: e.matmul(pi_[:], lhsT=qT[:, c, :], rhs=Cb[:, c, :], start=(c == 0), stop=(c == 3)), reads=[qk, "Cb"], writes=[pik])
                            for c in range(4):
                                P.op(pe, lambda e, c=c, qT=qT: e.matmul(pss[:, 17:18], lhsT=qT[:, c, :], rhs=nb[:, c:c + 1], start=(c == 0), stop=(c == 3)), reads=[qk, "nb"], writes=["pssd"])
                            small, smk = sm_r.next()
                            P.op(ac, lambda e, small=small: e.activation(out=small[:, 0:2], in_=pss[:, 16:18], func=AF.Copy), reads=["pssd"], writes=[smk])
                            tmpn, tmpk = tmpn_r.next(); num, numk = num_r.next()
                            P.op(ac, lambda e, tmpn=tmpn, pi_=pi_, ci=ci, h=h: e.activation(out=tmpn[:], in_=pi_[:], func=AF.Copy, scale=colq[:, ci, 4 + h:5 + h]), reads=[pik], writes=[tmpk])
                            P.op(dv, lambda e, num=num, tmpn=tmpn, pn=pn: e.tensor_tensor(out=num[:], in0=tmpn[:], in1=pn[:], op=ALU.add), reads=[tmpk, pnk], writes=[numk])
                            P.op(dv, lambda e, small=small, ci=ci, h=h: e.scalar_tensor_tensor(out=small[:, 3:4], in0=small[:, 1:2], scalar=colq[:, ci, 4 + h:5 + h], in1=small[:, 0:1], op0=ALU.mult, op1=ALU.add), reads=[smk], writes=[smk])
                            finish_tile(h, i, num[:], numk, small[:, 3:4], small, smk)
                            for c2 in range(4):
                                pkv, pkvk = PSB.next()
                                P.op(pe, lambda e, c2=c2, pkv=pkv, ktl=ktl, vt=vt: e.matmul(pkv[:], lhsT=ktl[:, c2 * 128:(c2 + 1) * 128], rhs=vt, start=True, stop=True), reads=[ktk, vk], writes=[pkvk])
                                P.op(dv, lambda e, c2=c2, pkv=pkv, h=h, i=i: e.scalar_tensor_tensor(out=Cst[:, c2, :], in0=Cst[:, c2, :], scalar=wliS[:, 8 + i, h:h + 1], in1=pkv[:], op0=ALU.mult, op1=ALU.add), reads=["Cst", pkvk], writes=["Cst"])
                            for c2 in range(4):
                                P.op(pe, lambda e, c2=c2, ktl=ktl: e.matmul(pss[:, 24 + c2:25 + c2], lhsT=ktl[:, c2 * 128:(c2 + 1) * 128], rhs=ones_b[:], start=True, stop=True), reads=[ktk], writes=["pssn"])
                            P.op(dv, lambda e, h=h, i=i: e.scalar_tensor_tensor(out=nst[:], in0=nst[:], scalar=wliS[:, 8 + i, h:h + 1], in1=pss[:, 24:28], op0=ALU.mult, op1=ALU.add), reads=["nst", "pssn"], writes=["nst"])
                            if i < TOWN - 2:
                                P.op(ac, lambda e: e.activation(out=Cb[:], in_=Cst[:], func=AF.Copy), reads=["Cst"], writes=["Cb"])
                                P.op(ac, lambda e: e.activation(out=nb[:], in_=nst[:], func=AF.Copy), reads=["nst"], writes=["nb"])

                    pend = None
                    for i in range(TOWN):
                        cx = part1(i)
                        if pend is not None:
                            part2(pend)
                        pend = cx
                    part2(pend)
                    P.dma("sp", lambda e, h=h: e.dma_start(out=Cp_d[h].rearrange("(c p) v -> p c v", p=128), in_=Cst[:]), reads=["Cst"], is_output=True)
                    with nc.allow_non_contiguous_dma(reason="n out"):
                        P.dma("sp", lambda e, h=h: e.dma_start(out=np_d[h].rearrange("(c p) -> p c", p=128), in_=nst[:], allow_slow_non_contiguous=True), reads=["nst"], is_output=True)
                P.barrier_all()
                if maybe_stop("C1"):
                    return nc

                Cj_v = [view(M0 + k * 8192, [128, 4, HD], F32) for k in range(3)]
                Z_v = [view(M0 + 24576 + k * 3968, [128, 4, 248], F32) for k in range(2)]
                o2 = M0 + 24576 + 2 * 3968
                ktj_v = [view(o2 + k * 1024, [128, HD], BF16) for k in range(2)]
                n0 = view(o2 + 2048, [128, HD], F32)
                nout = view(o2 + 4096, [128, HD], F32)
                n0T = view(o2 + 6144, [128, 4, 64], F32)
                nnT = view(o2 + 7168, [128, 4, 64], F32)
                P.dma("sp", lambda e: e.dma_start(out=n0[0:64, :], in_=sn), writes=["n0"])
                for c in range(4):
                    P.op(pe, lambda e, c=c: e.transpose(out=pss[:, 64 * c:64 * c + 64], in_=n0[0:64, c * 128:(c + 1) * 128], identity=identf[0:64, 0:64]), reads=["n0"], writes=["pss"])
                P.op(dv, lambda e: e.tensor_copy(out=n0T.rearrange("p c j -> p (c j)"), in_=pss[:, 0:256]), reads=["pss"], writes=["n0T"])
                for k in range(2):
                    P.op(dv, lambda e, k=k: e.memset(Z_v[k], 0.0), writes=[("Z", k)])
                ci = TPRE + TOWN - 1
                seq = [(h, j) for h in range(H) for j in range(16)]

                def c2_load(idx):
                    h, j = seq[idx]
                    P.dma("sp", lambda e: e.dma_start(out=Cj_v[idx % 3], in_=sC[j, h].rearrange("(c p) v -> p c v", p=128)), writes=[("Cj", idx % 3)])
                def c2_prep(idx):
                    h, j = seq[idx]
                    Z, Zk = Z_v[idx % 2], ("Z", idx % 2)
                    ktj, ktjk = ktj_v[idx % 2], ("ktj", idx % 2)
                    P.op(dv, lambda e, Z=Z, j=j, h=h: e.tensor_copy(out=Z[:, :, 120:128], in_=s_qT[:, h, :, 8 * j:8 * j + 8]), reads=[Zk], writes=[Zk])
                    P.op(dv, lambda e, ktj=ktj, j=j, h=h: e.tensor_scalar(out=ktj, in0=s_kt[:, h, :], scalar1=blk[:, j:j + 1], scalar2=None, op0=ALU.mult), reads=[ktjk], writes=[ktjk])
                c2_load(0); c2_load(1)
                c2_prep(0)
                for idx, (h, j) in enumerate(seq):
                    if idx + 2 < len(seq):
                        c2_load(idx + 2)
                    Cj, Cjk = Cj_v[idx % 3], ("Cj", idx % 3)
                    Z, Zk = Z_v[idx % 2], ("Z", idx % 2)
                    for c in range(4):
                        P.op(pe, lambda e, c=c, Z=Z, Cj=Cj, j=j: e.matmul(pacc[:], lhsT=Z[:, c, 120 - 8 * j:248 - 8 * j], rhs=Cj[:, c, :], start=(j == 0 and c == 0), stop=(j == 15 and c == 3)), reads=[Zk, Cjk], writes=["pacc"])
                    for c in range(4):
                        P.op(pe, lambda e, c=c, Z=Z, j=j, h=h: e.matmul(pss[:, 320 + j:321 + j], lhsT=Z[:, c, 120 - 8 * j:248 - 8 * j], rhs=n0T[:, c, 4 * j + h:4 * j + h + 1], start=(c == 0), stop=(c == 3)), reads=[Zk, "n0T"], writes=["pssd2"])
                    ktj, ktjk = ktj_v[idx % 2], ("ktj", idx % 2)
                    if idx + 1 < len(seq):
                        c2_prep(idx + 1)
                    for c2 in range(4):
                        pkv, pkvk = PSB.next()
                        P.op(pe, lambda e, c2=c2, pkv=pkv, ktj=ktj, h=h: e.matmul(pkv[:], lhsT=ktj[:, c2 * 128:(c2 + 1) * 128], rhs=s_v[:, h, :], start=True, stop=True), reads=[ktjk], writes=[pkvk])
                        P.op(dv, lambda e, c2=c2, pkv=pkv, Cj=Cj, j=j, h=h: e.scalar_tensor_tensor(out=Cj[:, c2, :], in0=Cj[:, c2, :], scalar=wliS[:, 16 + j, h:h + 1], in1=pkv[:], op0=ALU.mult, op1=ALU.add), reads=[Cjk, pkvk], writes=[Cjk])
                    for c2 in range(4):
                        P.op(pe, lambda e, c2=c2, ktj=ktj: e.matmul(pss[:, 304 + c2:305 + c2], lhsT=ktj[:, c2 * 128:(c2 + 1) * 128], rhs=ones_b[:], start=True, stop=True), reads=[ktjk], writes=["pssn2"])
                    P.op(dv, lambda e, j=j, h=h: e.scalar_tensor_tensor(out=nnT[:, :, 4 * j + h], in0=n0T[:, :, 4 * j + h], scalar=wliS[:, 16 + j, h:h + 1], in1=pss[:, 304:308], op0=ALU.mult, op1=ALU.add), reads=["n0T", "pssn2", "nnT"], writes=["nnT"])
                    P.dma(C2Q, lambda e, Cj=Cj, j=j, h=h: e.dma_start(out=Cs_d[j, h].rearrange("(c p) v -> p c v", p=128), in_=Cj), reads=[Cjk], is_output=True)
                    if j == 15:
                        small, smk = sm_r.next()
                        P.op(dv, lambda e, small=small: e.tensor_reduce(out=small[:, 1:2], in_=pss[:, 320:336], axis=AX.X, op=ALU.add), reads=["pssd2"], writes=[smk])
                        tmpn, tmpk = tmpn_r.next(); num, numk = num_r.next()
                        P.op(ac, lambda e, tmpn=tmpn, h=h: e.activation(out=tmpn[:], in_=pacc[:], func=AF.Copy, scale=colq[:, ci, 4 + h:5 + h]), reads=["pacc"], writes=[tmpk])
                        P.op(dv, lambda e, num=num, tmpn=tmpn, h=h: e.tensor_tensor(out=num[:], in0=tmpn[:], in1=s_num[:, h, :], op=ALU.add), reads=[tmpk], writes=[numk])
                        P.op(dv, lambda e, small=small, h=h: e.scalar_tensor_tensor(out=small[:, 3:4], in0=small[:, 1:2], scalar=colq[:, ci, 4 + h:5 + h], in1=s_den[:, h:h + 1], op0=ALU.mult, op1=ALU.add), reads=[smk], writes=[smk])
                        P.dma("sp", lambda e, h=h: e.dma_start(out=ghb[:], in_=g_head[h * HD:(h + 1) * HD].partition_broadcast(128)), writes=["ghb"])
                        finish_tile(h, TOWN - 1, num[:], numk, small[:, 3:4], small, smk)
                for c in range(4):
                    P.op(pe, lambda e, c=c: e.transpose(out=pss[0:64, 128 * c:128 * c + 128], in_=nnT[:, c, :], identity=identf[:]), reads=["nnT"], writes=["pss", "pssd2", "pssn2"])
                P.op(dv, lambda e: e.tensor_copy(out=nout[0:64, :], in_=pss[0:64, :]), reads=["pss"], writes=["nout"])
                P.dma("sp", lambda e: e.dma_start(out=ns_d, in_=nout[0:64, :]), reads=["nout"], is_output=True)
                P.barrier_all()
                if maybe_stop("C2"):
                    return nc
            dbg("hbT", hbT, [128, KC, NOWN])
            with Scope(bump) as stP:
                pooledT = view(M0, [128, 8, NOWN], BF16)
                hist = view(M0 + 18432, [128, 8, 240], F32)
                utok = view(M0 + 26112, [128, PW], F32); utoks = view(M0 + 30208, [128, PW], F32)
                hld = Ring(stP, nc, "hld", [120, PW], F32, 1, bump=bump)
                full = sb("full", [128, 1040], F32, stP); wsA = sb("wsA", [128, 1040], F32, stP); wsB = sb("wsB", [128, 1040], F32, stP)
                fulls = sb("fulls", [128, 16, 23], F32, stP); wsAs = sb("wsAs", [128, 16, 23], F32, stP); wsBs = sb("wsBs", [128, 16, 23], F32, stP)
                tmp16 = sb("tmp16", [128, 16], F32, stP)
                pscale = sb("pscale", [128, 8], F32, stP)
                wu_ring = Ring(stP, nc, "wuP", [128, KC, 128], BF16, 2, bump=bump)
                wut_ring = Ring(stP, nc, "wutP", [128, KC, 512], BF16, 1, bump=bump)
                wp_ring = Ring(stP, nc, "wpP", [128, 2, 256], BF16, 2, bump=bump)
                with nc.allow_non_contiguous_dma(reason="pool scale"):
                    P.dma("sp", lambda e: e.dma_start(out=pscale[:], in_=pool_scale.rearrange("(c p) -> p c", p=128), allow_slow_non_contiguous=True), writes=["pscale"])
                for half in range(2):
                    hl, hlk = hld.next()
                    P.dma("sp", lambda e, hl=hl, half=half: e.dma_start(out=hl[:], in_=spool[8 * half:8 * half + 8].rearrange("j r d -> (j r) d")), writes=[hlk])
                    for fc in range(8):
                        pt_, ptk = PSB.next()
                        P.op(pe, lambda e, fc=fc, hl=hl, pt_=pt_: e.transpose(out=pt_[:, 0:120], in_=hl[:, fc * 128:(fc + 1) * 128], identity=identf[0:120, 0:120]), reads=[hlk], writes=[ptk])
                        P.op(ac, lambda e, fc=fc, half=half, pt_=pt_: e.activation(out=hist[:, fc, 120 * half:120 * half + 120], in_=pt_[:, 0:120], func=AF.Copy), reads=[ptk], writes=[("hist", fc, half)])
                for cb in range(2):
                    wut, wutk = wut_ring.next()
                    wload(wut[:], wutk, kchunks(w_in[:, U0 + cb * 512:U0 + (cb + 1) * 512]), bar=("P" not in NOBAR))
                    for (ti, dst, dk) in [(7, utok, "utok"), (8, utoks, "utoks")]:
                        pu, puk = PSB.next()
                        for c in range(KC):
                            P.op(pe, lambda e, c=c, pu=pu, ti=ti, wut=wut: e.matmul(pu[:], lhsT=hNTo[:, c, ti * 128:(ti + 1) * 128], rhs=wut[:, c, :], start=(c == 0), stop=(c == KC - 1)), reads=[wutk], writes=[puk])
                        P.op(ac, lambda e, pu=pu, dst=dst, cb=cb: e.activation(out=dst[:, cb * 512:(cb + 1) * 512], in_=pu[:], func=AF.Copy), reads=[puk], writes=[(dk, cb)])
                P.dma("sp", lambda e: e.dma_start(out=poolp_d, in_=utok[113:128, :]), reads=[("utok", 0), ("utok", 1)], is_output=True)
                for j in range(16):
                    P.dma("sp", lambda e, j=j: e.dma_start(out=pools_d[j, 7:15, :], in_=utoks[8 * j:8 * j + 8, :]), reads=[("utoks", 0), ("utoks", 1)], is_output=True)
                P.dma("sp", lambda e: e.dma_start(out=pools_d[:, 0:7, :], in_=spool[:, 8:15, :]), is_output=True)
                wu_t = {}

                def pool_load(fc):
                    wu, wk = wu_ring.next()
                    wload(wu[:], wk, kchunks(w_in[:, U0 + fc * 128:U0 + (fc + 1) * 128]), bar=("P" not in NOBAR))
                    wu_t[fc] = (wu, wk)
                pool_load(0)
                for fc in range(8):
                    if fc + 1 < 8:
                        pool_load(fc + 1)
                    g = fc // 2
                    w = 2 << g
                    wu, wk = wu_t[fc]
                    P.op(dv, lambda e, fc=fc: e.tensor_copy(out=full[:, 0:16], in_=uhist[:, fc, :]), reads=["full"], writes=["full"])
                    P.op(dv, lambda e, fc=fc: e.tensor_copy(out=fulls[:, :, 0:15], in_=hist[:, fc, :].rearrange("p (j r) -> p j r", r=15)), reads=[("hist", fc, 0), ("hist", fc, 1), "fulls"], writes=["fulls"])
                    for (t0, n) in TGS:
                        pu, puk = PSB.next()
                        for c in range(KC):
                            P.op(pe, lambda e, c=c, pu=pu, t0=t0, n=n, wu=wu: e.matmul(pu[:, 0:n], lhsT=wu[:, c, :], rhs=hNTo[:, c, t0:t0 + n], start=(c == 0), stop=(c == KC - 1)), reads=[wk], writes=[puk])
                        if t0 < 1024:
                            P.op(ac, lambda e, pu=pu, t0=t0, n=n: e.activation(out=full[:, 16 + t0:16 + t0 + n], in_=pu[:, 0:n], func=AF.Copy), reads=[puk, "full"], writes=["full"])
                        else:
                            P.op(ac, lambda e, pu=pu: e.activation(out=fulls[:, :, 15:23], in_=pu[:, 0:128].rearrange("p (j r) -> p j r", r=8), func=AF.Copy), reads=[puk, "fulls"], writes=["fulls"])
                    src, srck, srcs, srcsk = full, "full", fulls, "fulls"
                    bufs = [(wsA, "wsA", wsAs, "wsAs"), (wsB, "wsB", wsBs, "wsBs")]
                    for k in range(g + 1):
                        sh = 1 << k
                        dst, dstk, dsts, dstsk = bufs[k % 2]
                        P.op(dv, lambda e, src=src, dst=dst, sh=sh: e.tensor_tensor(out=dst[:, sh:1040], in0=src[:, sh:1040], in1=src[:, 0:1040 - sh], op=ALU.add), reads=[srck, dstk], writes=[dstk])
                        P.op(dv, lambda e, srcs=srcs, dsts=dsts, sh=sh: e.tensor_tensor(out=dsts[:, :, sh:23], in0=srcs[:, :, sh:23], in1=srcs[:, :, 0:23 - sh], op=ALU.add), reads=[srcsk, dstsk], writes=[dstsk])
                        src, srck, srcs, srcsk = dst, dstk, dsts, dstsk
                    P.op(dv, lambda e, src=src, fc=fc, w=w: e.scalar_tensor_tensor(out=pooledT[:, fc, 0:1024], in0=src[:, 16:1040], scalar=1.0 / w, in1=full[:, 16:1040], op0=ALU.mult, op1=ALU.subtract), reads=[srck, "full"], writes=[("pooledT", fc)])
                    P.op(dv, lambda e, src=src, g=g: e.tensor_tensor(out=tmp16[:], in0=src[:, 16:32], in1=rcb[:, g, :], op=ALU.mult), reads=[srck, "tmp16"], writes=["tmp16"])
                    P.op(dv, lambda e, fc=fc: e.tensor_tensor(out=pooledT[:, fc, 0:16], in0=tmp16[:], in1=full[:, 16:32], op=ALU.subtract), reads=["tmp16", "full", ("pooledT", fc)], writes=[("pooledT", fc)])
                    P.op(dv, lambda e, srcs=srcs, fc=fc, w=w: e.scalar_tensor_tensor(out=pooledT[:, fc, 1024:1152].rearrange("p (j r) -> p j r", r=8), in0=srcs[:, :, 15:23], scalar=1.0 / w, in1=fulls[:, :, 15:23], op0=ALU.mult, op1=ALU.subtract), reads=[srcsk, "fulls", ("pooledT", fc)], writes=[("pooledT", fc)])
                for g in range(4):
                    wp, wpk = wp_ring.next()
                    wload(wp[:], wpk, w_pool[g].rearrange("(c p) d -> p c d", p=128), bar=("P" not in NOBAR))
                    for dc in range(2):
                        for (t0, n) in TGS:
                            pm, pmk = PSB.next()
                            for cc in range(2):
                                P.op(pe, lambda e, cc=cc, pm=pm, wp=wp, dc=dc, g=g, t0=t0, n=n: e.matmul(pm[:, 0:n], lhsT=wp[:, cc, dc * 128:(dc + 1) * 128], rhs=pooledT[:, 2 * g + cc, t0:t0 + n], start=(cc == 0), stop=(cc == 1)), reads=[wpk, ("pooledT", 2 * g), ("pooledT", 2 * g + 1)], writes=[pmk])
                            P.op(ac, lambda e, pm=pm, g=g, dc=dc, t0=t0, n=n: e.activation(out=aT[:, 2 * g + dc, t0:t0 + n], in_=pm[:, 0:n], func=AF.Copy, scale=pscale[:, 2 * g + dc:2 * g + dc + 1]), reads=[pmk, "pscale"], writes=[("aT", 2 * g + dc, t0)])
                P.barrier_all()
                if maybe_stop("Pool"):
                    return nc
        dbg("aT", aT, [128, 8, NOWN])
        with Scope(bump) as stDD:
            wo_r = Ring(stDD, nc, "woD", [128, KC, 128], BF16, 2, bump=bump)
            wga_r = Ring(stDD, nc, "wga", [128, KC, 128], BF16, 2, bump=bump); wgb_r = Ring(stDD, nc, "wgb", [128, KC, 128], BF16, 2, bump=bump)
            wpa_r = Ring(stDD, nc, "wpa", [128, 8, 128], BF16, 2, bump=bump); wpb_r = Ring(stDD, nc, "wpb", [128, KC, 128], BF16, 2, bump=bump)
            sg_r = Ring(stDD, nc, "sg", [128, 512], F32, 3, bump=bump); t1_r = Ring(stDD, nc, "t1", [128, 512], F32, 2, bump=bump); t2_r = Ring(stDD, nc, "t2", [128, 512], F32, 2, bump=bump)
            wo_t = {}

            def d0_load(oc):
                wo, wok = wo_r.next()
                wload(wo[:], wok, kchunks(w_in[:, O0 + oc * 128:O0 + (oc + 1) * 128]), bar=("D0" not in NOBAR))
                wo_t[oc] = (wo, wok)
            d0_load(0)
            for oc in range(KC):
                if oc + 1 < KC:
                    d0_load(oc + 1)
                wo, wok = wo_t[oc]
                for (t0, n) in TGS:
                    po, pok = PSB.next()
                    for c in range(KC):
                        P.op(pe, lambda e, c=c, po=po, wo=wo, t0=t0, n=n: e.matmul(po[:, 0:n], lhsT=wo[:, c, :], rhs=hNTo[:, c, t0:t0 + n], start=(c == 0), stop=(c == KC - 1)), reads=[wok], writes=[pok])
                    sg, sgk = sg_r.next()
                    P.op(ac, lambda e, sg=sg, po=po, n=n: e.activation(out=sg[:, 0:n], in_=po[:, 0:n], func=AF.Sigmoid), reads=[pok], writes=[sgk])
                    P.op(dv, lambda e, sg=sg, oc=oc, t0=t0, n=n: e.tensor_tensor(out=hbT[:, oc, t0:t0 + n], in0=hbT[:, oc, t0:t0 + n], in1=sg[:, 0:n], op=ALU.mult), reads=[sgk], writes=[("hbTg", oc, t0)])
            P.barrier_all()
            if maybe_stop("D0"):
                return nc
            w_t = {}

            def d1_load(cb):
                wga, wgak = wga_r.next(); wgb, wgbk = wgb_r.next(); wpa, wpak = wpa_r.next(); wpb, wpbk = wpb_r.next()
                wload(wga[:], wgak, kchunks(w_in[:, GA0 + cb * 128:GA0 + (cb + 1) * 128]), bar=("D1" not in NOBAR))
                wload(wpa[:], wpak, kchunks(w_pa[:, cb * 128:(cb + 1) * 128]), bar=("D1" not in NOBAR))
                wload(wgb[:], wgbk, kchunks(w_in[:, GB0 + cb * 128:GB0 + (cb + 1) * 128]), bar=("D1" not in NOBAR))
                wload(wpb[:], wpbk, kchunks(w_pb[:, cb * 128:(cb + 1) * 128]), bar=("D1" not in NOBAR))
                w_t[cb] = (wga, wgak, wgb, wgbk, wpa, wpak, wpb, wpbk)
            d1_load(0)
            for cb in range(KC):
                if cb + 1 < KC:
                    d1_load(cb + 1)
                wga, wgak, wgb, wgbk, wpa, wpak, wpb, wpbk = w_t[cb]
                for (t0, n) in TGS:
                    pga, pgak = PSB.next()
                    for c in range(KC):
                        P.op(pe, lambda e, c=c, pga=pga, wga=wga, t0=t0, n=n: e.matmul(pga[:, 0:n], lhsT=wga[:, c, :], rhs=hNTo[:, c, t0:t0 + n], start=(c == 0), stop=(c == KC - 1)), reads=[wgak], writes=[pgak])
                    pa_, pak = PSB.next()
                    for c in range(8):
                        P.op(pe, lambda e, c=c, pa_=pa_, wpa=wpa, t0=t0, n=n: e.matmul(pa_[:, 0:n], lhsT=wpa[:, c, :], rhs=aT[:, c, t0:t0 + n], start=(c == 0), stop=(c == 7)), reads=[wpak], writes=[pak])
                    sga, sgak = sg_r.next()
                    P.op(ac, lambda e, sga=sga, pga=pga, n=n: e.activation(out=sga[:, 0:n], in_=pga[:, 0:n], func=AF.Sigmoid), reads=[pgak], writes=[sgak])
                    t1, t1k = t1_r.next()
                    P.op(dv, lambda e, t1=t1, sga=sga, pa_=pa_, n=n: e.tensor_tensor(out=t1[:, 0:n], in0=sga[:, 0:n], in1=pa_[:, 0:n], op=ALU.mult), reads=[sgak, pak], writes=[t1k])
                    pgb, pgbk = PSB.next()
                    for c in range(KC):
                        P.op(pe, lambda e, c=c, pgb=pgb, wgb=wgb, t0=t0, n=n: e.matmul(pgb[:, 0:n], lhsT=wgb[:, c, :], rhs=hNTo[:, c, t0:t0 + n], start=(c == 0), stop=(c == KC - 1)), reads=[wgbk], writes=[pgbk])
                    pb_, pbk = PSB.next()
                    for c in range(KC):
                        P.op(pe, lambda e, c=c, pb_=pb_, wpb=wpb, t0=t0, n=n: e.matmul(pb_[:, 0:n], lhsT=wpb[:, c, :], rhs=hbT[:, c, t0:t0 + n], start=(c == 0), stop=(c == KC - 1)), reads=[wpbk], writes=[pbk])
                    sgb, sgbk = sg_r.next()
                    P.op(ac, lambda e, sgb=sgb, pgb=pgb, n=n: e.activation(out=sgb[:, 0:n], in_=pgb[:, 0:n], func=AF.Sigmoid), reads=[pgbk], writes=[sgbk])
                    t2, t2k = t2_r.next()
                    P.op(dv, lambda e, t2=t2, sgb=sgb, pb_=pb_, n=n: e.tensor_tensor(out=t2[:, 0:n], in0=sgb[:, 0:n], in1=pb_[:, 0:n], op=ALU.mult), reads=[sgbk, pbk], writes=[t2k])
                    P.op(dv, lambda e, t1=t1, t2=t2, cb=cb, t0=t0, n=n: e.tensor_tensor(out=mixT[:, cb, t0:t0 + n], in0=t1[:, 0:n], in1=t2[:, 0:n], op=ALU.add), reads=[t1k, t2k], writes=[("mixT", cb, t0)])
            P.barrier_all()
            if maybe_stop("D"):
                return nc
        dbg("mixT", mixT, [128, KC, NOWN])
        with Scope(bump) as stE:
            woE = Ring(stE, nc, "woE", [128, KC, 512], BF16, 2, bump=bump)
            P.dma("sp", lambda e: e.dma_start(out=yacc, in_=xo.rearrange("(t p) d -> p t d", p=128)), writes=["yacc"])
            we_t = {}

            def e_load(cb):
                wo, wok = woE.next()
                wload(wo[:], wok, kchunks(w_out[:, cb * 512:(cb + 1) * 512]), bar=("E" not in NOBAR))
                we_t[cb] = (wo, wok)
            e_load(0)
            for cb in range(4):
                if cb + 1 < 4:
                    e_load(cb + 1)
                wo, wok = we_t[cb]
                for t in range(TOWN):
                    px, pxk = PSB.next()
                    for c in range(KC):
                        P.op(pe, lambda e, c=c, px=px, wo=wo, t=t: e.matmul(px[:], lhsT=mixT[:, c, t * 128:(t + 1) * 128], rhs=wo[:, c, :], start=(c == 0), stop=(c == KC - 1)), reads=[wok], writes=[pxk])
                    P.op(dv, lambda e, px=px, t=t, cb=cb: e.tensor_tensor(out=yacc[:, t, cb * 512:(cb + 1) * 512], in0=yacc[:, t, cb * 512:(cb + 1) * 512], in1=px[:], op=ALU.add), reads=[pxk, "yacc"], writes=[("yacc", t, cb)])
            P.barrier_all()
            if maybe_stop("E"):
                return nc
        dbg("x1", yacc, [128, TOWN, D])
        comb = view(A0 + 9216, [128, TOWN, NE], F32)
        hT_v = [view(A0 + k * 4608, [128, 2, NOWN], BF16) for k in range(2)]
        with Scope(bump) as stF0:
            hn_ring = Ring(stF0, nc, "hnF", [128, D], BF16, 2, bump=bump)
            ssq = sb("ssqF", [128, 4], F32, stF0); junk = sb("junkF", [128, D], BF16, stF0)
            Wr = sb("Wr", [128, KC, 36], BF16, stF0); bbc = sb("bbc", [128, 36], F32, stF0)
            L_r = Ring(stF0, nc, "Lr", [128, 36], F32, 2, bump=bump); elm_r = Ring(stF0, nc, "elm", [128, 32], F32, 2, bump=bump)
            elm2_r = Ring(stF0, nc, "elm2", [128, 32], F32, 2, bump=bump); oh1_r = Ring(stF0, nc, "oh1", [128, 32], F32, 2, bump=bump); oh2_r = Ring(stF0, nc, "oh2", [128, 32], F32, 2, bump=bump)
            s_r = Ring(stF0, nc, "rs", [128, 16], F32, 2, bump=bump); j4 = sb("j4", [128, 4], F32, stF0)
            P.dma("sp", lambda e: e.dma_start(out=gbc[:], in_=g_ffn.partition_broadcast(128)), writes=["gbc"])
            with nc.allow_non_contiguous_dma(reason="router weights"):
                wload(Wr[:, :, 0:4], "Wr0", kchunks(w_rg), True, bar=("F0" not in NOBAR))
                wload(Wr[:, :, 4:36], "Wr1", kchunks(w_re), True, bar=("F0" not in NOBAR))
            P.dma("sp", lambda e: e.dma_start(out=bbc[:, 0:4], in_=b_rg.partition_broadcast(128)), writes=["bbc0"])
            P.dma("sp", lambda e: e.dma_start(out=bbc[:, 4:36], in_=b_re.partition_broadcast(128)), writes=["bbc1"])
            for t in range(TOWN):
                rmsnorm_T(("sb", yacc[:, t, :], ("yacc_t", t)), hFT, "hFT", t * 128, None, hn_ring, (ssq, junk))
            for t in range(TOWN):
                plg, plk = PSB.next()
                for c in range(KC):
                    P.op(pe, lambda e, c=c, plg=plg, t=t: e.matmul(plg[:, 0:36], lhsT=hFT[:, c, t * 128:(t + 1) * 128], rhs=Wr[:, c, :], start=(c == 0), stop=(c == KC - 1)), reads=["Wr0", "Wr1", ("hFT", t, 0), ("hFT", t, 1)], writes=[plk])
                L, Lk = L_r.next(); elm, ek = elm_r.next(); elm2, e2k = elm2_r.next(); oh1, o1k = oh1_r.next(); oh2, o2k = oh2_r.next(); s, sk = s_r.next()
                P.op(dv, lambda e, L=L, plg=plg: e.tensor_tensor(out=L[:], in0=plg[:, 0:36], in1=bbc[:], op=ALU.add), reads=[plk, "bbc0", "bbc1"], writes=[Lk])
                P.op(dv, lambda e, L=L, s=s: e.tensor_reduce(out=s[:, 0:1], in_=L[:, 0:4], axis=AX.X, op=ALU.max), reads=[Lk], writes=[sk])
                P.op(dv, lambda e, s=s: e.tensor_scalar(out=s[:, 1:2], in0=s[:, 0:1], scalar1=-1.0, scalar2=None, op0=ALU.mult), reads=[sk], writes=[sk])
                P.op(ac, lambda e, L=L, s=s: e.activation(out=j4[:], in_=L[:, 0:4], func=AF.Exp, bias=s[:, 1:2], scale=1.0, accum_out=s[:, 2:3]), reads=[Lk, sk], writes=["j4", sk])
                P.op(dv, lambda e, s=s: e.reciprocal(out=s[:, 3:4], in_=s[:, 2:3]), reads=[sk], writes=[sk])
                P.op(dv, lambda e, L=L, s=s: e.tensor_scalar(out=s[:, 8:12], in0=L[:, 0:4], scalar1=s[:, 0:1], scalar2=-1.0, op0=ALU.is_ge, op1=ALU.add), reads=[Lk, sk], writes=[sk])
                P.op(dv, lambda e, s=s: e.tensor_scalar(out=s[:, 8:12], in0=s[:, 8:12], scalar1=1.0e9, scalar2=None, op0=ALU.mult), reads=[sk], writes=[sk])
                for g in range(4):
                    P.op(dv, lambda e, g=g, L=L, s=s, elm=elm: e.tensor_scalar(out=elm[:, 8 * g:8 * g + 8], in0=L[:, 4 + 8 * g:12 + 8 * g], scalar1=s[:, 8 + g:9 + g], scalar2=None, op0=ALU.add), reads=[Lk, sk, ek], writes=[ek])
                P.op(dv, lambda e, s=s, elm=elm: e.tensor_reduce(out=s[:, 4:5], in_=elm[:], axis=AX.X, op=ALU.max), reads=[ek, sk], writes=[sk])
                P.op(dv, lambda e, s=s, elm=elm, oh1=oh1: e.tensor_scalar(out=oh1[:], in0=elm[:], scalar1=s[:, 4:5], scalar2=None, op0=ALU.is_ge), reads=[ek, sk], writes=[o1k])
                P.op(dv, lambda e, elm=elm, oh1=oh1, elm2=elm2: e.scalar_tensor_tensor(out=elm2[:], in0=oh1[:], scalar=-1.0e9, in1=elm[:], op0=ALU.mult, op1=ALU.add), reads=[o1k, ek], writes=[e2k])
                P.op(dv, lambda e, s=s, elm2=elm2: e.tensor_reduce(out=s[:, 5:6], in_=elm2[:], axis=AX.X, op=ALU.max), reads=[e2k, sk], writes=[sk])
                P.op(dv, lambda e, s=s, elm2=elm2, oh2=oh2: e.tensor_scalar(out=oh2[:], in0=elm2[:], scalar1=s[:, 5:6], scalar2=None, op0=ALU.is_ge), reads=[e2k, sk], writes=[o2k])
                P.op(dv, lambda e, s=s: e.tensor_tensor(out=s[:, 6:7], in0=s[:, 4:5], in1=s[:, 5:6], op=ALU.subtract), reads=[sk], writes=[sk])
                P.op(ac, lambda e, s=s: e.activation(out=s[:, 6:7], in_=s[:, 6:7], func=AF.Sigmoid), reads=[sk], writes=[sk])
                P.op(dv, lambda e, s=s: e.tensor_tensor(out=s[:, 7:8], in0=s[:, 6:7], in1=s[:, 3:4], op=ALU.mult), reads=[sk], writes=[sk])
                P.op(dv, lambda e, s=s: e.tensor_tensor(out=s[:, 12:13], in0=s[:, 3:4], in1=s[:, 7:8], op=ALU.subtract), reads=[sk], writes=[sk])
                P.op(dv, lambda e, s=s, oh1=oh1, t=t: e.tensor_scalar(out=comb[:, t, :], in0=oh1[:], scalar1=s[:, 7:8], scalar2=None, op0=ALU.mult), reads=[o1k, sk], writes=[("comb", t)])
                P.op(dv, lambda e, s=s, oh2=oh2, t=t: e.scalar_tensor_tensor(out=comb[:, t, :], in0=oh2[:], scalar=s[:, 12:13], in1=comb[:, t, :], op0=ALU.mult, op1=ALU.add), reads=[o2k, sk, ("comb", t)], writes=[("comb", t)])
            P.barrier_all()
            if maybe_stop("F0"):
                return nc
        dbg("comb", comb, [128, TOWN, NE])
        with Scope(bump) as stF1:
            Wg_r = Ring(stF1, nc, "Wg", [128, KC, 256], BF16, 2, bump=bump); Wu_r = Ring(stF1, nc, "Wu", [128, KC, 256], BF16, 2, bump=bump)
            Wd_r = Ring(stF1, nc, "Wd", [128, 2, D], BF16, 3, bump=bump)
            sgl_r = Ring(stF1, nc, "sgl", [128, 512], F32, 2, bump=bump)
            ssq = sb("ssqF1", [128, 4], F32, stF1)
            units = [(e_, fh) for e_ in range(NE) for fh in range(2)]
            wt = {}

            def f_load(u):
                e_, fh = units[u]
                Wg, wgk = Wg_r.next(); Wu, wuk = Wu_r.next(); Wd, wdk = Wd_r.next()
                wload(Wg[:], wgk, kchunks(w_eg[e_][:, fh * 256:(fh + 1) * 256]), bar=("F1" not in NOBAR))
                wload(Wu[:], wuk, kchunks(w_eu[e_][:, fh * 256:(fh + 1) * 256]), bar=("F1" not in NOBAR))
                wload(Wd[:], wdk, w_ed[e_][fh * 256:(fh + 1) * 256, :].rearrange("(c p) d -> p c d", p=128), bar=("F1" not in NOBAR))
                wt[u] = (Wg, wgk, Wu, wuk, Wd, wdk)
            class _R6:
                t = PSB.t + [pss, pacc]
                k = PSB.k + ["pss", "pacc"]
                i = 0

                def next(self):
                    j = self.i % 6
                    self.i += 1
                    return self.t[j], self.k[j]
            PS6 = _R6()

            def gateup_steps(Wg, wgk, Wu, wuk, hT, hTk):
                steps = []
                for fc in range(2):
                    for (t0, n) in TGS:
                        def step(fc=fc, t0=t0, n=n):
                            phg, phgk = PS6.next(); phu, phuk = PS6.next()
                            for c in range(KC):
                                P.op(pe, lambda e, c=c: e.matmul(phg[:, 0:n], lhsT=Wg[:, c, fc * 128:(fc + 1) * 128], rhs=hFT[:, c, t0:t0 + n], start=(c == 0), stop=(c == KC - 1)), reads=[wgk], writes=[phgk])
                            for c in range(KC):
                                P.op(pe, lambda e, c=c: e.matmul(phu[:, 0:n], lhsT=Wu[:, c, fc * 128:(fc + 1) * 128], rhs=hFT[:, c, t0:t0 + n], start=(c == 0), stop=(c == KC - 1)), reads=[wuk], writes=[phuk])
                            sgl, sglk = sgl_r.next()
                            P.op(ac, lambda e: e.activation(out=sgl[:, 0:n], in_=phg[:, 0:n], func=AF.Silu), reads=[phgk], writes=[sglk])
                            P.op(dv, lambda e: e.tensor_tensor(out=hT[:, fc, t0:t0 + n], in0=sgl[:, 0:n], in1=phu[:, 0:n], op=ALU.mult), reads=[sglk, phuk, hTk], writes=[hTk])
                        steps.append((n, step))
                return steps

            def down_groups(e_, hT, hTk, Wd, wdk):
                groups = []
                for t in range(TOWN):
                    for cb in range(4):
                        def grp(t=t, cb=cb):
                            py, pyk = PS6.next()
                            for fc in range(2):
                                P.op(pe, lambda e, fc=fc: e.matmul(py[:], lhsT=hT[:, fc, t * 128:(t + 1) * 128], rhs=Wd[:, fc, cb * 512:(cb + 1) * 512], start=(fc == 0), stop=(fc == 1)), reads=[hTk, wdk], writes=[pyk])
                            P.op(dv, lambda e: e.scalar_tensor_tensor(out=yacc[:, t, cb * 512:(cb + 1) * 512], in0=py[:], scalar=comb[:, t, e_:e_ + 1], in1=yacc[:, t, cb * 512:(cb + 1) * 512], op0=ALU.mult, op1=ALU.add), reads=[pyk, ("yacc", t, cb)], writes=[("yacc", t, cb)])
                        groups.append(grp)
                return groups

            f_load(0)
            prev_down = []
            for u, (e_, fh) in enumerate(units):
                if u + 1 < len(units):
                    f_load(u + 1)
                Wg, wgk, Wu, wuk, Wd, wdk = wt.pop(u)
                hT, hTk = hT_v[u % 2], ("hT", u % 2)
                pos = 0
                for (n, step) in gateup_steps(Wg, wgk, Wu, wuk, hT, hTk):
                    step()
                    take = 8 if n == 512 else 2
                    for g in prev_down[pos:pos + take]:
                        g()
                    pos += take
                for g in prev_down[pos:]:
                    g()
                prev_down = down_groups(e_, hT, hTk, Wd, wdk)
            for g in prev_down:
                g()
            P.dma("sp", lambda e: e.dma_start(out=gbc[:], in_=g_final.partition_broadcast(128)), writes=["gbc"])
            for t in range(TOWN):
                yk = [("yacc", t, cb) for cb in range(4)]
                sglj, sgljk = sgl_r.next()
                for cb in range(4):
                    P.op(ac, lambda e, t=t, cb=cb, sglj=sglj: e.activation(out=sglj[:], in_=yacc[:, t, cb * 512:(cb + 1) * 512], func=AF.Square, accum_out=ssq[:, cb:cb + 1]), reads=[("yacc", t, cb), "ssqF"], writes=[sgljk, "ssqF"])
                P.op(dv, lambda e: e.tensor_reduce(out=ssq[:, 0:1], in_=ssq[:, 0:4], axis=AX.X, op=ALU.add), reads=["ssqF"], writes=["ssqF"])
                P.op(ac, lambda e: e.activation(out=ssq[:, 1:2], in_=ssq[:, 0:1], func=AF.Sqrt, scale=1.0 / D, bias=EPS), reads=["ssqF"], writes=["ssqF"])
                P.op(dv, lambda e: e.reciprocal(out=ssq[:, 2:3], in_=ssq[:, 1:2]), reads=["ssqF"], writes=["ssqF"])
                P.op(dv, lambda e, t=t: e.scalar_tensor_tensor(out=yacc[:, t, :], in0=yacc[:, t, :], scalar=ssq[:, 2:3], in1=gbc[:], op0=ALU.mult, op1=ALU.mult), reads=yk + ["ssqF", "gbc"], writes=yk)
                P.dma("sp", lambda e, t=t: e.dma_start(out=y_d[t * 128:(t + 1) * 128, :], in_=yacc[:, t, :]), reads=yk, is_output=True)
            P.finish()
            P.emit()
            _DBG['P'] = P; _DBG['peak'] = bump.peak
    return nc


_NC = {}


def _get_nc(debug=None):
    key = tuple(debug) if debug else ()
    if key not in _NC:
        _NC[key] = build(debug)
    return _NC[key]


def make_in_maps(inp):
    f = lambda a: np.ascontiguousarray(np.asarray(a, dtype=np.float32))
    xpr = f(inp["x_prompt"]); xs = f(inp["x_sample"])
    shared = {
        "g_mix": f(inp["g_mix"][0]), "w_in": f(inp["w_in"][0]), "b_if": f(inp["b_if"][0]), "w_pool": f(inp["w_pool"][0]),
        "pool_scale": f(inp["pool_scale"][0]), "w_pa": f(inp["w_proj_a"][0]), "w_pb": f(inp["w_proj_b"][0]),
        "g_head": f(inp["g_head"][0]), "w_out": f(inp["w_out"][0]), "g_ffn": f(inp["g_ffn"][0]),
        "w_rg": f(inp["w_router_group"][0]), "b_rg": f(inp["b_router_group"][0]), "w_re": f(inp["w_router_expert"][0]),
        "b_re": f(inp["b_router_expert"][0]), "w_eg": f(inp["w_exp_gate"][0]), "w_eu": f(inp["w_exp_up"][0]),
        "w_ed": f(inp["w_exp_down"][0]), "g_final": f(inp["g_final"]),
    }
    spool = f(inp["state_pool"][0]); sC = f(inp["state_C"][0]); sn = f(inp["state_n"][0]); sm = f(inp["state_m"][0])
    maps = []
    for c in range(8):
        b, half = c // 2, c % 2
        xo = np.concatenate([xpr[b, half * 1024:(half + 1) * 1024], xs[16 * c:16 * c + 16].reshape(128, D)], axis=0)
        xp = xpr[b, 0:1024] if half == 1 else np.zeros((1024, D), np.float32)
        rc = np.zeros((4, 16), np.float32)
        for g in range(4):
            for t in range(16):
                rc[g, t] = 1.0 / min(half * 1024 + t + 1, 2 << g)
        m = dict(shared)
        m.update({
            "xo": np.ascontiguousarray(xo), "xp": np.ascontiguousarray(xp),
            "flag": np.full((128, 1), float(half), np.float32), "rc": rc.reshape(64),
            "spool": np.ascontiguousarray(spool[16 * c:16 * c + 16]), "sC": np.ascontiguousarray(sC[16 * c:16 * c + 16]),
            "sn": np.ascontiguousarray(sn[16 * c:16 * c + 16].reshape(64, HD)), "sm": np.ascontiguousarray(sm[16 * c:16 * c + 16]),
        })
        maps.append(m)
    return maps


def assemble(res):
    B = 4
    y_prompt = np.zeros((B, 2048, D), np.float32); y_sample = np.zeros((128, 8, D), np.float32)
    pool_p = np.zeros((1, B, 15, PW), np.float32); C_p = np.zeros((1, B, H, HD, HD), np.float32)
    n_p = np.zeros((1, B, H, HD), np.float32); m_p = np.zeros((1, B, H), np.float32)
    pool_s = np.zeros((1, 128, 15, PW), np.float32); C_s = np.zeros((1, 128, H, HD, HD), np.float32)
    n_s = np.zeros((1, 128, H, HD), np.float32); m_s = np.zeros((1, 128, H), np.float32)
    for c in range(8):
        r = res[c]
        b, half = c // 2, c % 2
        y_prompt[b, half * 1024:(half + 1) * 1024] = r["y"][0:1024]
        y_sample[16 * c:16 * c + 16] = r["y"][1024:].reshape(16, 8, D)
        if half == 1:
            pool_p[0, b] = r["pool_p"]; C_p[0, b] = r["C_p"]; n_p[0, b] = r["n_p"]; m_p[0, b] = r["m_p"][:, 0]
        pool_s[0, 16 * c:16 * c + 16] = r["pool_s"]; C_s[0, 16 * c:16 * c + 16] = r["C_s"]
        n_s[0, 16 * c:16 * c + 16] = r["n_s"].reshape(16, H, HD); m_s[0, 16 * c:16 * c + 16] = r["m_s"]
    return (y_prompt, y_sample, pool_p, C_p, n_p, m_p, pool_s, C_s, n_s, m_s)


def kernel(**inputs):
    nc = _get_nc()
    in_maps = make_in_maps(inputs)
    res = run_bass_kernel_spmd(nc, in_maps, core_ids=list(range(8)))
    return assemble(res.results)
```
